# Optimizing a Trainium2 kernel written in Bass

```python
import math
import jax, jax.numpy as jnp
from jax import lax
import numpy as np

D_MODEL = 1024
BATCH = 8
SEQ = 4096
DEPTH = 1

EPS = 1e-6
NEG = -1e30
A_HEADS = 8
A_KV_HEADS = 2
A_HEAD_DIM = 64
WINDOW = 128
BLOCK = 128
N_BUCKETS = 32
MAX_DISTANCE = 128
B_HEADS = 8
Q_LORA = 384
KV_LORA = 256
NOPE_DIM = 64
ROPE_DIM = 32
V_DIM = 64
ROPE_THETA = 10000.0
A_WIDTH = A_HEADS * A_HEAD_DIM
B_WIDTH = B_HEADS * V_DIM
MIX_WIDTH = A_WIDTH + B_WIDTH
IN_SPLITS = (A_WIDTH, A_KV_HEADS * A_HEAD_DIM, A_KV_HEADS * A_HEAD_DIM, Q_LORA, KV_LORA, ROPE_DIM)
IN_WIDTH = sum(IN_SPLITS)
N_EXPERTS = 16
EXPERT_FF = 1024
CAPACITY_FACTOR = 2

kernel_name = "hybrid_swa_mla_expert_choice_block"


def rms_norm(t, g):
    tf = t.astype(jnp.float32)
    y = tf * lax.rsqrt(jnp.mean(tf * tf, axis=-1, keepdims=True) + EPS)
    return (y * g.astype(jnp.float32)).astype(t.dtype)


def t5_bucket(rel):
    half = N_BUCKETS // 2
    max_exact = half // 2
    base = jnp.where(rel > 0, half, 0)
    n = jnp.abs(rel)
    nf = jnp.maximum(n, 1).astype(jnp.float32)
    large = max_exact + (jnp.log(nf / max_exact) / math.log(MAX_DISTANCE / max_exact)
                         * (half - max_exact)).astype(jnp.int32)
    large = jnp.minimum(large, half - 1)
    return base + jnp.where(n < max_exact, n, large)


def rope(t, cos, sin):
    half = ROPE_DIM // 2
    t1, t2 = t[..., :half], t[..., half:]
    c, s = cos.astype(t.dtype), sin.astype(t.dtype)
    return jnp.concatenate([t1 * c - t2 * s, t1 * s + t2 * c], axis=-1)


def window_gqa(q, k, v, rel_bias, sink):
    bsz, seq = q.shape[0], q.shape[1]
    nb = seq // BLOCK
    g, r = A_KV_HEADS, A_HEADS // A_KV_HEADS
    qb = q.reshape(bsz, nb, BLOCK, g, r, A_HEAD_DIM)

    def band(t):
        tp = jnp.pad(t, ((0, 0), (BLOCK, BLOCK), (0, 0), (0, 0)))
        tp = tp.reshape(bsz, nb + 2, BLOCK, g, A_HEAD_DIM)
        return jnp.concatenate([tp[:, :-2], tp[:, 1:-1], tp[:, 2:]], axis=2)

    kb, vb = band(k), band(v)
    s = jnp.einsum('bnqgrd,bnkgd->bngrqk', qb, kb).astype(jnp.float32) * (A_HEAD_DIM ** -0.5)
    qi = jnp.arange(BLOCK)[:, None]
    kj = jnp.arange(3 * BLOCK)[None, :]
    rel = (kj - BLOCK) - qi
    bias = rel_bias[t5_bucket(rel)]
    bias = bias.transpose(2, 0, 1).reshape(g, r, BLOCK, 3 * BLOCK).astype(jnp.float32)
    kpos = jnp.arange(nb)[:, None] * BLOCK - BLOCK + jnp.arange(3 * BLOCK)[None, :]
    valid = (jnp.abs(rel) <= WINDOW)[None] & ((kpos >= 0) & (kpos < seq))[:, None, :]
    s = jnp.where(valid[None, :, None, None], s + bias, NEG)
    sink_logit = jnp.broadcast_to(sink.astype(jnp.float32).reshape(g, r, 1, 1), s.shape[:-1] + (1,))
    p = jax.nn.softmax(jnp.concatenate([s, sink_logit], axis=-1), axis=-1)[..., :-1]
    o = jnp.einsum('bngrqk,bnkgd->bnqgrd', p.astype(v.dtype), vb)
    return o.reshape(bsz, seq, A_WIDTH)


def latent_attention(c_q, c_kv, k_rope, cq_norm_g, w_qb, ckv_norm_g, w_kvb,
                     qn_g, qr_g, kn_g, kr_g):
    bsz, seq = c_q.shape[0], c_q.shape[1]
    nb = seq // BLOCK
    pos = jnp.arange(seq, dtype=jnp.float32)
    freqs = ROPE_THETA ** (-jnp.arange(0, ROPE_DIM, 2, dtype=jnp.float32) / ROPE_DIM)
    ang = pos[:, None] * freqs[None, :]
    cos, sin = jnp.cos(ang), jnp.sin(ang)

    q = jnp.einsum('bsc,cf->bsf', rms_norm(c_q, cq_norm_g), w_qb)
    q = q.reshape(bsz, seq, B_HEADS, NOPE_DIM + ROPE_DIM)
    q_nope = rms_norm(q[..., :NOPE_DIM], qn_g)
    q_rot = rope(rms_norm(q[..., NOPE_DIM:], qr_g), cos[:, None], sin[:, None])
    kv = jnp.einsum('bsc,cf->bsf', rms_norm(c_kv, ckv_norm_g), w_kvb)
    kv = kv.reshape(bsz, seq, B_HEADS, NOPE_DIM + V_DIM)
    k_nope = rms_norm(kv[..., :NOPE_DIM], kn_g)
    v = kv[..., NOPE_DIM:]
    k_rot = rope(rms_norm(k_rope, kr_g), cos, sin)
    scale = (NOPE_DIM + ROPE_DIM) ** -0.5

    qn_b = q_nope.reshape(bsz, nb, BLOCK, B_HEADS, NOPE_DIM).swapaxes(0, 1)
    qr_b = q_rot.reshape(bsz, nb, BLOCK, B_HEADS, ROPE_DIM).swapaxes(0, 1)

    def attend(blk):
        qn, qr = blk
        s = (jnp.einsum('bqhd,bkhd->bhqk', qn, k_nope)
             + jnp.einsum('bqhr,bkr->bhqk', qr, k_rot)).astype(jnp.float32) * scale
        p = jax.nn.softmax(s, axis=-1)
        return jnp.einsum('bhqk,bkhd->bqhd', p.astype(v.dtype), v)

    o = lax.map(attend, (qn_b, qr_b))
    return o.swapaxes(0, 1).reshape(bsz, seq, B_WIDTH)


def expert_choice_ffn(h, w_router, w_gate, w_up, w_down):
    bsz, seq, d = h.shape
    cap = CAPACITY_FACTOR * seq // N_EXPERTS
    aff = jax.nn.softmax(jnp.einsum('bsd,de->bse', h, w_router).astype(jnp.float32), axis=-1)
    gate, idx = lax.top_k(aff.transpose(0, 2, 1), cap)
    xe = jax.vmap(lambda hb, ib: hb[ib])(h, idx)
    a = jax.nn.silu(jnp.einsum('becd,edf->becf', xe, w_gate)) * jnp.einsum('becd,edf->becf', xe, w_up)
    ye = jnp.einsum('becf,efd->becd', a, w_down) * gate[..., None].astype(h.dtype)
    y = jax.vmap(lambda yb, ib: jnp.zeros((seq, d), yb.dtype).at[ib.reshape(-1)].add(yb.reshape(-1, d)))(ye, idx)
    return y


def setup_inputs(seed: int = 0) -> dict:
    key = jax.random.key(seed)
    ks = jax.random.split(key, 32)
    f32 = jnp.float32

    def w(k, shape, fan_in):
        return jax.random.normal(k, shape, f32) * (fan_in ** -0.5)

    def gain(k, shape):
        return 1.0 + 0.1 * jax.random.normal(k, shape, f32)

    L = DEPTH
    return {
        'x': jax.random.normal(ks[0], (BATCH, SEQ, D_MODEL), f32),
        'rel_bias': 0.3 * jax.random.normal(ks[1], (N_BUCKETS, A_HEADS), f32),
        'ln1_g': gain(ks[2], (L, D_MODEL)),
        'w_in': w(ks[3], (L, D_MODEL, IN_WIDTH), D_MODEL),
        'a_q_norm_g': gain(ks[4], (L, A_HEAD_DIM)),
        'a_k_norm_g': gain(ks[5], (L, A_HEAD_DIM)),
        'a_sink': 0.5 * jax.random.normal(ks[6], (L, A_HEADS), f32),
        'cq_norm_g': gain(ks[7], (L, Q_LORA)),
        'w_qb': w(ks[8], (L, Q_LORA, B_HEADS * (NOPE_DIM + ROPE_DIM)), Q_LORA),
        'ckv_norm_g': gain(ks[9], (L, KV_LORA)),
        'w_kvb': w(ks[10], (L, KV_LORA, B_HEADS * (NOPE_DIM + V_DIM)), KV_LORA),
        'b_qn_g': gain(ks[11], (L, NOPE_DIM)),
        'b_qr_g': gain(ks[12], (L, ROPE_DIM)),
        'b_kn_g': gain(ks[13], (L, NOPE_DIM)),
        'b_kr_g': gain(ks[14], (L, ROPE_DIM)),
        'out_a_g': gain(ks[15], (L, A_WIDTH)),
        'out_b_g': gain(ks[16], (L, B_WIDTH)),
        'w_o': w(ks[17], (L, MIX_WIDTH, D_MODEL), MIX_WIDTH),
        'ln2_g': gain(ks[18], (L, D_MODEL)),
        'w_router': w(ks[19], (L, D_MODEL, N_EXPERTS), D_MODEL),
        'w_gate': w(ks[20], (L, N_EXPERTS, D_MODEL, EXPERT_FF), D_MODEL),
        'w_up': w(ks[21], (L, N_EXPERTS, D_MODEL, EXPERT_FF), D_MODEL),
        'w_down': w(ks[22], (L, N_EXPERTS, EXPERT_FF, D_MODEL), EXPERT_FF),
    }


def reference(x, rel_bias, ln1_g, w_in, a_q_norm_g, a_k_norm_g, a_sink, cq_norm_g, w_qb,
              ckv_norm_g, w_kvb, b_qn_g, b_qr_g, b_kn_g, b_kr_g, out_a_g, out_b_g, w_o,
              ln2_g, w_router, w_gate, w_up, w_down):
    bsz, seq = x.shape[0], x.shape[1]
    offsets = [int(o) for o in np.cumsum(IN_SPLITS)[:-1]]
    for l in range(DEPTH):
        h = rms_norm(x, ln1_g[l])
        z = jnp.einsum('bsd,df->bsf', h, w_in[l])
        q_a, k_a, v_a, c_q, c_kv, k_rope = jnp.split(z, offsets, axis=-1)
        q_a = rms_norm(q_a.reshape(bsz, seq, A_HEADS, A_HEAD_DIM), a_q_norm_g[l])
        k_a = rms_norm(k_a.reshape(bsz, seq, A_KV_HEADS, A_HEAD_DIM), a_k_norm_g[l])
        v_a = v_a.reshape(bsz, seq, A_KV_HEADS, A_HEAD_DIM)
        o_a = window_gqa(q_a, k_a, v_a, rel_bias, a_sink[l])
        o_b = latent_attention(c_q, c_kv, k_rope, cq_norm_g[l], w_qb[l], ckv_norm_g[l], w_kvb[l],
                               b_qn_g[l], b_qr_g[l], b_kn_g[l], b_kr_g[l])
        o = jnp.concatenate([rms_norm(o_a, out_a_g[l]), rms_norm(o_b, out_b_g[l])], axis=-1)
        x = x + jnp.einsum('bsf,fd->bsd', o, w_o[l])
        h2 = rms_norm(x, ln2_g[l])
        x = x + expert_choice_ffn(h2, w_router[l], w_gate[l], w_up[l], w_down[l])
    return x
```

```python
import math
import os
from contextlib import ExitStack

import numpy as np
import ml_dtypes

import concourse.bass as bass
import concourse.mybir as mybir
from concourse.bass_utils import run_bass_kernel_spmd

F32 = mybir.dt.float32
BF16 = mybir.dt.bfloat16
F16 = mybir.dt.float16
I32 = mybir.dt.int32
ALU = mybir.AluOpType
AF = mybir.ActivationFunctionType

S = 4096
D = 1024
NT = 32
NG = 8
EPS = 1e-6
NE = 16
CAP = 512
SB_BASE = 16512
SB_END = 229376

GC_Q2, GC_K2, GC_CQ, GC_CKV, GC_KR, GC_QB, GC_KN, GC_LN1, GC_OA, GC_OB, GC_EPS = 0, 1, 2, 5, 7, 8, 9, 10, 18, 22, 26
NGC = 28


class Op:
    __slots__ = ("eng", "fn", "deps", "signal", "sem", "val", "is_dma", "key", "idx")

    def __init__(self, eng, fn, deps, is_dma, key):
        self.eng, self.fn, self.deps, self.is_dma, self.key = eng, fn, deps, is_dma, key
        self.signal = False
        self.sem = None
        self.val = 0


class Prog:
    ENGS = ("pe", "act", "dve", "pool", "sp")
    LIMIT = 30000

    def __init__(self, nc, sems):
        self.nc = nc
        self.free_sems = list(sems)
        self.ops = {e: [] for e in self.ENGS}
        self.lastw = {}
        self.readers = {}
        self.eng_sem = {e: None for e in self.ENGS}
        self.eng_cnt = {e: 0 for e in self.ENGS}
        self.dma_sem = {}
        self.dma_cnt = {}
        self.waited = {e: {} for e in self.ENGS}
        self.phase_dma = []

    def op(self, eng, fn, reads=(), writes=(), dma=False, key=None, deps=()):
        d = []
        for r in reads:
            w = self.lastw.get(r)
            if w is not None:
                d.append(w)
        for w_ in writes:
            w = self.lastw.get(w_)
            if w is not None:
                d.append(w)
            d.extend(self.readers.get(w_, ()))
        d.extend(deps)
        self.nops = getattr(self, "nops", 0) + 1
        if self.nops > int(os.environ.get("P_MAXOPS", 10 ** 9)) and fn is not None:
            return Op(eng, None, [], False, None)
        o = Op(eng, fn, d, dma, key)
        for r in reads:
            self.readers.setdefault(r, []).append(o)
        for w_ in writes:
            self.lastw[w_] = o
            self.readers[w_] = []
        self.ops[eng].append(o)
        if dma:
            if key not in self.dma_sem:
                self.dma_sem[key] = self.free_sems.pop()
                self.dma_cnt[key] = 0
            self.dma_cnt[key] += 16
            o.sem, o.val = self.dma_sem[key], self.dma_cnt[key]
            self.phase_dma.append(o)
        return o

    def flush(self, blk):
        if self.phase_dma:
            last = {}
            for o in self.phase_dma:
                last[o.sem] = o
            self.op("sp", None, deps=list(last.values()))
        for e in self.ENGS:
            for o in self.ops[e]:
                for dd in o.deps:
                    if dd.is_dma:
                        continue
                    if dd.eng == o.eng and o.eng == "pe":
                        continue
                    dd.signal = True
        for e in self.ENGS:
            for o in self.ops[e]:
                if o.is_dma or not o.signal:
                    continue
                if self.eng_sem[e] is None or self.eng_cnt[e] >= self.LIMIT:
                    self.eng_sem[e] = self.free_sems.pop()
                    self.eng_cnt[e] = 0
                self.eng_cnt[e] += 1
                o.sem, o.val = self.eng_sem[e], self.eng_cnt[e]
        starters = {"pe": blk.tensor, "act": blk.scalar, "dve": blk.vector, "pool": blk.gpsimd, "sp": blk.sync}
        for e in self.ENGS:
            ops = self.ops[e]
            if not ops:
                continue

            def body(h, ops=ops, e=e):
                waited = self.waited[e]
                for o in ops:
                    need = {}
                    for dd in o.deps:
                        if not dd.is_dma and dd.eng == e and e == "pe":
                            continue
                        if dd.sem is None:
                            continue
                        if need.get(dd.sem, (0, None))[0] < dd.val:
                            need[dd.sem] = (dd.val, dd.sem)
                    for sem, (val, _) in need.items():
                        if waited.get(sem, 0) < val:
                            h.wait_ge(sem, val)
                            waited[sem] = val
                    if o.fn is None:
                        continue
                    ins = o.fn(h)
                    if o.is_dma:
                        ins.then_inc(o.sem, 16)
                    elif o.signal:
                        ins.then_inc(o.sem, 1)

            starters[e](body)
        self.ops = {e: [] for e in self.ENGS}
        self.lastw = {}
        self.readers = {}
        self.phase_dma = []


class Arena:
    def __init__(self, nc):
        self.nc = nc
        self.n = 0
        self.lo = SB_BASE

    def at(self, name, shape, dtype, off):
        self.n += 1
        return self.nc.alloc_sbuf_tensor_at(f"{name}_{self.n}", list(shape), dtype, offset=off)

    def tmp(self, name, shape, dtype, limit):
        nbytes = int(np.prod(shape[1:])) * mybir.dt.size(dtype)
        nbytes = (nbytes + 31) // 32 * 32
        off = self.lo
        self.lo += nbytes
        assert self.lo <= limit, f"SBUF overflow allocating {name}: {self.lo} > {limit}"
        return self.at(name, shape, dtype, off)


def KB(x):
    return SB_BASE + int(x * 1024)


def build_program(debug=(), stop_after=99):
    nc = bass.Bass("TRN2", target_bir_lowering=False)
    dbg = {}

    def din(name, shape, dt=F32):
        return nc.dram_tensor(name, list(shape), dt, kind="ExternalInput").ap()

    xT = din("xT", [D, S])
    x = din("x", [S, D])
    w_aug = din("w_aug", [D, 1664])
    gcols_d = din("gcols", [128, NGC])
    cmat_d = din("cmat", [128, 6, 128])
    csk_d = din("csk", [128, S])
    csq_d = din("csq", [128, S])
    ohrel_d = din("ohrel", [128, 640])
    maskadd_d = din("maskadd", [128, 640])
    relb_d = din("relb_rep", [128, 8, 128])
    sink_d = din("a_sink", [1, 8])
    wqb_d = din("wqb_aug", [384, 1024])
    wkvb_d = din("wkvb_aug", [256, 1024])
    wo_d = din("wo_p", [D, D])
    ln2_d = din("ln2_g", [1, D])
    wr_d = din("w_router", [D, 128])
    wg_d = din("w_gate", [NE, D, D])
    wu_d = din("w_up", [NE, D, D])
    wd_d = din("w_down", [NE, D, D])
    ccol_d = din("ccol", [128, 4])
    out = nc.dram_tensor("out", [S, D], F32, kind="ExternalOutput").ap()
    trep = nc.dram_tensor("trep", [8, 128, 640], F32, kind="ExternalOutput")
    h2_dram = nc.dram_tensor("h2_dram", [S, D], BF16, kind="ExternalOutput").ap()
    aff_dram = nc.dram_tensor("aff_dram", [S, NE], F32, kind="ExternalOutput").ap()
    cum_dram = nc.dram_tensor("cum_dram", [NE, S], F16, kind="ExternalOutput").ap()
    w16 = nc.dram_tensor("w16", [3 * NE, D, D], BF16, kind="ExternalOutput").ap()
    wsrc = (wg_d, wu_d, wd_d)
    wc_state = [0]


    def dump(P, name, t, eng="sp"):
        if name not in debug:
            return
        shp = list(t.shape)
        d = nc.dram_tensor("dbg_" + name, shp, t.dtype, kind="ExternalOutput").ap()
        dbg[name] = d
        lastops = [P.ops[en][-1] for en in ("pe", "act", "dve", "pool") if P.ops[en]]
        lo_p = 64 if name.startswith("KT") else 0
        P.op(eng, lambda e: e.dma_start(out=d[lo_p:], in_=t[lo_p:]), deps=lastops + list(P.phase_dma), dma=True, key="dbg_" + name)

    with ExitStack() as es:
        sems = [es.enter_context(nc.semaphore(f"s{i}")) for i in range(100)]
        P = Prog(nc, sems)
        A = Arena(nc)

        def wcast(n):
            for _ in range(n):
                i = wc_state[0]
                if i >= 3 * NE or stop_after < 6:
                    return
                wc_state[0] += 1
                P.op("pool", lambda e, i=i: e.dma_start(out=w16[i], in_=wsrc[i % 3][i // 3]), dma=True, key=f"wc{i % 4}")
        psum = [es.enter_context(nc.psum_tensor(f"ps{i}", [128, 512], F32)) for i in range(8)]

        cmat = A.at("cmat", [128, 6, 128], BF16, KB(0))
        identf = A.at("identf", [128, 128], F32, KB(1.5))
        gcols = A.at("gcols", [128, NGC], F32, KB(2))
        esink = A.at("esink", [128, 8], F32, KB(2.25))
        ccol = A.at("ccol", [128, 4], F32, KB(2.5))
        CONST_END = 4
        ident_bf = cmat[:, 0, :]
        ones_bf = cmat[:, 1, :]
        bd64 = cmat[:, 2, :]
        bdq = cmat[:, 3, :]
        bdk32 = cmat[:, 4, :]
        fold = cmat[:, 5, :]

        R_C = CONST_END
        R_A = R_C + 56
        R_OA = R_A + 56
        cqn = A.at("cqn", [128, 3, S], BF16, KB(R_C))
        ckvn = A.at("ckvn", [128, 2, S], BF16, KB(R_C + 24))
        KT = [A.at(f"KT{i}", [128, S], BF16, KB(R_C + 40 + 8 * i)) for i in range(2)]
        qA = A.at("qA", [128, 4, S], BF16, KB(R_A))
        kA = A.at("kA", [128, S], BF16, KB(R_A + 32))
        VA = A.at("VA", [128, NT, 2, 128], BF16, KB(R_A + 40))
        oA = A.at("oA", [128, 4, S], BF16, KB(R_OA))

        if stop_after >= 1:
            A.lo = KB(R_OA)
            LIM = SB_END
            Wbf = A.tmp("Wbf", [128, 8, 1664], BF16, LIM)
            Wst = A.tmp("Wst", [128, 1664], F32, LIM)
            xst = [A.tmp(f"xst{i}", [128, 512], F32, LIM) for i in range(3)]
            xbf = [A.tmp(f"xbf{i}", [128, 8, 512], BF16, LIM) for i in range(2)]
            xsq = A.tmp("xsq", [128, 8, 512], BF16, LIM)
            zt = [A.tmp(f"zt{i}", [128, 512], F32, LIM) for i in range(4)]
            sqb = [A.tmp(f"sqb{i}", [128, 512], BF16, LIM) for i in range(3)]
            rs = [A.tmp(f"rs{i}", [128, 512], F32, LIM) for i in range(2)]
            rinv = [A.tmp(f"rinv{i}", [128, 512], F32, LIM) for i in range(2)]
            rstd_bc = [A.tmp(f"rstdbc{i}", [128, 512], F32, LIM) for i in range(2)]
            rsc = A.tmp("rsc", [128, 4], F32, LIM)
            rstd_col = [A.tmp(f"rstdcol{i}", [128, 4], F32, LIM) for i in range(2)]
            wk = A.tmp("wk", [128, 512], BF16, LIM)
            csk = [A.tmp(f"csk{i}", [128, 512], F32, LIM) for i in range(2)]
            sinkt = A.tmp("sinkt", [128, 8], F32, LIM)

            with nc.Block() as blk:
                P.op("sp", lambda e: e.dma_start(out=Wst[:, 0:768], in_=cmat_d.rearrange("p a b -> p (a b)")), writes=["Wst"], dma=True, key="wst")
                P.op("sp", lambda e: e.dma_start(out=identf[:], in_=cmat_d[:, 0, :]), writes=["identf"], dma=True, key="c11")
                P.op("sp", lambda e: e.dma_start(out=gcols[:], in_=gcols_d), writes=["gcols"], dma=True, key="c12")
                P.op("sp", lambda e: e.dma_start(out=ccol[:], in_=ccol_d), writes=["ccol"], dma=True, key="c13")
                P.op("sp", lambda e: e.dma_start(out=sinkt[:], in_=sink_d.partition_broadcast(128)), writes=["sinkt"], dma=True, key="c14")
                P.op("dve", lambda e: e.tensor_copy(out=cmat[:].rearrange("p a b -> p (a b)"), in_=Wst[:, 0:768]), reads=["Wst"], writes=["cmat"])
                P.op("act", lambda e: e.activation(out=esink[:], in_=sinkt[:], func=AF.Exp), reads=["sinkt"], writes=["esink"])
                P.op("pool", lambda e: e.memset(VA[:, :, 0, 64:128], 1.0), writes=["VAones0"])
                P.op("pool", lambda e: e.memset(VA[:, :, 1, 0:64], 1.0), writes=["VAones1"])
                wcast(12)
                for k in range(8):
                    P.op("sp", lambda e, k=k: e.dma_start(out=Wst[:], in_=w_aug[k * 128:(k + 1) * 128, :]),
                         writes=["Wst"], dma=True, key="wst")
                    P.op("dve", lambda e, k=k: e.tensor_scalar(out=Wbf[:, k, :], in0=Wst[:], scalar1=gcols[:, GC_LN1 + k:GC_LN1 + k + 1],
                                                               scalar2=None, op0=ALU.mult),
                         reads=["Wst", "gcols"], writes=[("Wbf", k)])
                WB = [("Wbf", k) for k in range(8)]

                def stage(G):
                    xb = xbf[G % 2]
                    P.op("sp", lambda e: e.dma_start(out=csk[G % 2][:], in_=csk_d[:, G * 512:(G + 1) * 512]),
                         writes=[("csk", G % 2)], dma=True, key=f"csk{G % 2}")
                    for k in range(8):
                        r = k % 3
                        P.op("sp", lambda e, k=k, r=r: e.dma_start(out=xst[r][:], in_=xT[k * 128:(k + 1) * 128, G * 512:(G + 1) * 512]),
                             writes=[("xst", r)], dma=True, key=f"xst{r}")
                        P.op("act", lambda e, k=k, r=r: e.activation(out=xsq[:, k, :], in_=xst[r][:], func=AF.Square),
                             reads=[("xst", r)], writes=[("xsq", k)])
                        P.op("dve", lambda e, k=k, r=r: e.tensor_copy(out=xb[:, k, :], in_=xst[r][:]),
                             reads=[("xst", r)], writes=[("xbf", G % 2, k)])

                def stats(G):
                    ss = psum[0]
                    for k in range(8):
                        P.op("pe", lambda e, k=k: e.matmul(ss[:], lhsT=ones_bf, rhs=xsq[:, k, :], start=(k == 0), stop=(k == 7)),
                             reads=[("xsq", k), "cmat"], writes=["ps0"])
                    P.op("act", lambda e: e.activation(out=rs[0][:], in_=ss[:], func=AF.Ln, scale=1.0 / D, bias=gcols[:, GC_EPS:GC_EPS + 1]),
                         reads=["ps0", "gcols"], writes=["rs0"])
                    P.op("act", lambda e: e.activation(out=rstd_bc[G % 2][:], in_=rs[0][:], func=AF.Exp, scale=-0.5), reads=["rs0"], writes=[("rstdbc", G % 2)])
                    sc = psum[1]
                    for t in range(4):
                        for k in range(8):
                            P.op("pe", lambda e, k=k, t=t: e.matmul(sc[:, t * 128:(t + 1) * 128], lhsT=xsq[:, k, t * 128:(t + 1) * 128], rhs=ones_bf,
                                                                   start=(k == 0), stop=(k == 7)),
                                 reads=[("xsq", k), "cmat"], writes=["ps1"])
                    P.op("act", lambda e: e.activation(out=rsc[:], in_=sc[:, 0:512:128], func=AF.Ln, scale=1.0 / D, bias=gcols[:, GC_EPS:GC_EPS + 1]),
                         reads=["ps1", "gcols"], writes=["rsc"])
                    P.op("act", lambda e: e.activation(out=rstd_col[G % 2][:], in_=rsc[:], func=AF.Exp, scale=-0.5), reads=["rsc"], writes=[("rstdcol", G % 2)])

                zring = [0]

                def proj(G, c):
                    slot = zring[0] % 4
                    zring[0] += 1
                    pb = 2 + (slot % 3)
                    zp = psum[pb]
                    xb = xbf[G % 2]
                    for k in range(8):
                        P.op("pe", lambda e, k=k: e.matmul(zp[:], lhsT=Wbf[:, k, c * 128:(c + 1) * 128], rhs=xb[:, k, :], start=(k == 0), stop=(k == 7)),
                             reads=[("Wbf", k), ("xbf", G % 2, k)], writes=[f"ps{pb}"])
                    P.op("dve", lambda e: e.tensor_tensor(out=zt[slot][:], in0=zp[:], in1=rstd_bc[G % 2][:], op=ALU.mult),
                         reads=[f"ps{pb}", ("rstdbc", G % 2)], writes=[("zt", slot)])
                    return slot

                nring = [0]

                def norm_rinv(slots, bd, nrows, scale):
                    j = nring[0] % 2
                    nring[0] += 1
                    msp = psum[5 + j]
                    for i, sl in enumerate(slots):
                        sb_ = sqb[(nring[0] * 3 + i) % 3] if False else sqb[i]
                        P.op("act", lambda e, sl=sl, sb_=sb_: e.activation(out=sb_[0:nrows, :], in_=zt[sl][0:nrows, :], func=AF.Square),
                             reads=[("zt", sl)], writes=[("sqb", i)])
                        P.op("pe", lambda e, sb_=sb_, i=i: e.matmul(msp[0:nrows, :], lhsT=bd[0:nrows, 0:nrows], rhs=sb_[0:nrows, :],
                                                                   start=(i == 0), stop=(i == len(slots) - 1)),
                             reads=[("sqb", i), "cmat"], writes=[f"ps{5 + j}"])
                    P.op("act", lambda e: e.activation(out=rs[1][0:nrows, :], in_=msp[0:nrows, :], func=AF.Ln, scale=scale, bias=gcols[0:nrows, GC_EPS:GC_EPS + 1]),
                         reads=[f"ps{5 + j}", "gcols"], writes=["rs1"])
                    P.op("act", lambda e: e.activation(out=rinv[j][0:nrows, :], in_=rs[1][0:nrows, :], func=AF.Exp, scale=-0.5), reads=["rs1"], writes=[("rinv", j)])
                    return j

                def finish(slot, j, gc, dst, dkey, nrows=128):
                    P.op("dve", lambda e: e.scalar_tensor_tensor(out=dst, in0=zt[slot][0:nrows, :], scalar=gcols[0:nrows, gc:gc + 1],
                                                                 in1=rinv[j][0:nrows, :], op0=ALU.mult, op1=ALU.mult),
                         reads=[("zt", slot), ("rinv", j), "gcols"], writes=[dkey])

                def group(G):
                    gs = slice(G * 512, (G + 1) * 512)
                    stats(G)

                    def fin_q(c):
                        return lambda sls, j: finish(sls[0], j, GC_Q2, qA[:, c, gs], ("qA", c, G))

                    def fin_k(sls, j):
                        finish(sls[0], j, GC_K2, kA[:, gs], ("kA", G))

                    def fin_cq(sls, j):
                        for i in range(3):
                            finish(sls[i], j, GC_CQ + i, cqn[:, i, gs], ("cqn", i, G))

                    def fin_ckv(sls, j):
                        for i in range(2):
                            finish(sls[i], j, GC_CKV + i, ckvn[:, i, gs], ("ckvn", i, G))

                    def fin_kr(sls, j):
                        sl = sls[0]
                        finish(sl, j, GC_KR, zt[sl][:, :], ("zt", sl))
                        P.op("dve", lambda e: e.tensor_tensor(out=wk[:], in0=zt[sl][:, :], in1=csk[G % 2][:], op=ALU.mult),
                             reads=[("zt", sl), ("csk", G % 2)], writes=["wk"])
                        kp = psum[7]
                        P.op("pe", lambda e: e.matmul(kp[:], lhsT=fold, rhs=wk[:], start=True, stop=True),
                             reads=["wk", "cmat"], writes=["ps7"])
                        P.op("dve", lambda e: e.tensor_copy(out=KT[0][64:128, gs], in_=kp[64:128, :]), reads=["ps7"], writes=[("KT0r", G)])
                        P.op("dve", lambda e: e.tensor_copy(out=KT[1][64:128, gs], in_=kp[64:128, :]), reads=["ps7"], writes=[("KT1r", G)])

                    descs = [([c], bd64, 1.0, fin_q(c)) for c in range(4)]
                    descs.append(([4], bd64, 1.0, fin_k))
                    descs.append(([5, 6, 7], ones_bf, 1.0 / 384, fin_cq))
                    descs.append(([10], bdk32, 1.0, fin_kr))
                    descs.append(([8, 9], ones_bf, 1.0 / 256, fin_ckv))
                    pending = None
                    for di, (chs, bd_, sc_, fin_) in enumerate(descs):
                        sls = [proj(G, c) for c in chs]
                        if pending is not None:
                            p_sls, p_bd, p_sc, p_fin = pending
                            p_fin(p_sls, norm_rinv(p_sls, p_bd, 128, p_sc))
                        pending = (sls, bd_, sc_, fin_)
                        if di == 4 and G + 1 < int(os.environ.get("P1_GROUPS", NG)):
                            stage(G + 1)
                    p_sls, p_bd, p_sc, p_fin = pending
                    p_fin(p_sls, norm_rinv(p_sls, p_bd, 128, p_sc))
                    for t in range(4):
                        tile_i = G * 4 + t
                        vp = psum[7]
                        xb = xbf[G % 2]
                        for k in range(8):
                            P.op("pe", lambda e, k=k, t=t: e.matmul(vp[:, 0:128], lhsT=xb[:, k, t * 128:(t + 1) * 128], rhs=Wbf[:, k, 1536:1664],
                                                                   start=(k == 0), stop=(k == 7)),
                                 reads=[("Wbf", k), ("xbf", G % 2, k)], writes=["ps7"])
                        P.op("dve", lambda e, t=t, tile_i=tile_i: e.tensor_scalar(out=VA[:, tile_i, 0, 0:64], in0=vp[:, 0:64], scalar1=rstd_col[G % 2][:, t:t + 1],
                                                                                 scalar2=None, op0=ALU.mult),
                             reads=["ps7", ("rstdcol", G % 2)], writes=[("VA0", tile_i)])
                        P.op("dve", lambda e, t=t, tile_i=tile_i: e.tensor_scalar(out=VA[:, tile_i, 1, 64:128], in0=vp[:, 64:128], scalar1=rstd_col[G % 2][:, t:t + 1],
                                                                                 scalar2=None, op0=ALU.mult),
                             reads=["ps7", ("rstdcol", G % 2)], writes=[("VA1", tile_i)])

                NGRP = int(os.environ.get("P1_GROUPS", NG))
                if NGRP > 0:
                    stage(0)
                for G in range(NGRP):
                    group(G)
                for nm, t in (("qA", qA), ("kA", kA), ("VA", VA), ("cqn", cqn), ("ckvn", ckvn), ("KT0", KT[0])):
                    dump(P, nm, t)
                P.flush(blk)


        if stop_after >= 2:
            A.lo = KB(R_OA + 32)
            LIM = SB_END
            kAm = [A.tmp(f"kAm{i}", [128, S], BF16, LIM) for i in range(2)]
            relrep = A.tmp("relrep", [128, 8, 128], F32, LIM)
            ohp = A.tmp("ohp", [128, 640], F32, LIM)
            mka = A.tmp("mka", [128, 640], F32, LIM)
            rep = A.tmp("rep", [128, 640], F32, LIM)
            BTf = A.tmp("BTf", [128, 384], F32, LIM)
            BTb = [A.tmp(f"BTb{i}", [128, 384], BF16, LIM) for i in range(8)]
            pTa = [A.tmp(f"pTa{i}", [128, 384], BF16, LIM) for i in range(3)]
            NB = 8
            raw = [A.tmp(f"raw{i}", [128, NB, 256], F32, LIM) for i in range(2)]
            dsh2 = A.tmp("dsh2", [128, NB, 128], F32, LIM)
            with nc.Block() as blk:
                P.op("sp", lambda e: e.dma_start(out=relrep[:], in_=relb_d), writes=["relrep"], dma=True, key="p2a")
                P.op("sp", lambda e: e.dma_start(out=ohp[:], in_=ohrel_d), writes=["ohp"], dma=True, key="p2b")
                P.op("sp", lambda e: e.dma_start(out=mka[:], in_=maskadd_d), writes=["mka"], dma=True, key="p2c")
                P.op("pool", lambda e: e.memset(kAm[0][:], 0.0), writes=["kAm0"])
                P.op("pool", lambda e: e.memset(kAm[1][:], 0.0), writes=["kAm1"])
                wcast(6)
                P.op("dve", lambda e: e.tensor_copy(out=kAm[0][0:64, :], in_=kA[0:64, :]), reads=["kAm0"], writes=["kAm0"])
                P.op("dve", lambda e: e.tensor_copy(out=kAm[1][64:128, :], in_=kA[64:128, :]), reads=["kAm1"], writes=["kAm1"])
                for h in range(8):
                    for hf in range(2):
                        P.op("pe", lambda e, h=h, hf=hf: e.matmul(psum[hf][:, 0:320], lhsT=relrep[:, h, :], rhs=ohp[:, hf * 320:(hf + 1) * 320], start=True, stop=True),
                             reads=["relrep", "ohp"], writes=[f"ps{hf}"])
                        P.op("dve", lambda e, hf=hf: e.tensor_tensor(out=rep[:, hf * 320:(hf + 1) * 320], in0=psum[hf][:, 0:320], in1=mka[:, hf * 320:(hf + 1) * 320], op=ALU.add),
                             reads=[f"ps{hf}", "mka"], writes=[("rep", hf)])
                    P.op("sp", lambda e, h=h: e.dma_start(out=trep.ap()[h], in_=rep[:]), reads=[("rep", 0), ("rep", 1)], writes=[("trep", h)], dma=True, key="trepw")
                    P.op("sp", lambda e, h=h: e.dma_start(out=BTf[:], in_=bass.AP(tensor=trep, offset=h * 128 * 640 + 127, ap=[[639, 128], [1, 384]])),
                         reads=[("trep", h)], writes=["BTf"], dma=True, key="btf")
                    P.op("dve", lambda e, h=h: e.tensor_scalar(out=BTb[h][:], in0=BTf[:], scalar1=8.0, scalar2=None, op0=ALU.mult), reads=["BTf"], writes=[("BTb", h)])
                it = [0]
                for c in range(4):
                    items = [(n, hh) for n in range(NT) for hh in range(2)]
                    rmap = {}

                    def qk(n, hh, c=c):
                        head = c + 4 * hh
                        r = it[0] % 3
                        it[0] += 1
                        rmap[(n, hh)] = r
                        spb = psum[r]
                        ms_ = [(yi, m) for yi, m in enumerate((n + 1, n, n - 1)) if 0 <= m < NT]
                        y0, y1 = ms_[0][0] * 128, ms_[-1][0] * 128 + 128
                        P.op("pe", lambda e: e.matmul(spb[:, y0:y1], lhsT=ident_bf, rhs=BTb[head][:, y0:y1], start=True, stop=False),
                             reads=[("BTb", head)], writes=[f"ps{r}"])
                        for i_, (yi, m) in enumerate(ms_):
                            P.op("pe", lambda e, yi=yi, m=m, i_=i_, L=len(ms_): e.matmul(spb[:, yi * 128:(yi + 1) * 128], lhsT=kAm[hh][:, m * 128:(m + 1) * 128],
                                                                                         rhs=qA[:, c, n * 128:(n + 1) * 128], start=False, stop=(i_ == L - 1)),
                                 reads=[f"kAm{hh}"], writes=[f"ps{r}"])
                        P.op("act", lambda e: e.activation(out=pTa[r][:, y0:y1], in_=spb[:, y0:y1], func=AF.Exp, scale=0.125),
                             reads=[f"ps{r}"], writes=[("pTa", r)])

                    def pv(n, hh, c=c):
                        r = rmap[(n, hh)]
                        ab = 3 + n % 2
                        acc = psum[ab]
                        ms_ = [(yi, m) for yi, m in enumerate((n + 1, n, n - 1)) if 0 <= m < NT]
                        for i_, (yi, m) in enumerate(ms_):
                            P.op("pe", lambda e, yi=yi, m=m, i_=i_, L=len(ms_): e.matmul(acc[:, hh * 128:(hh + 1) * 128], lhsT=VA[:, m, hh, :], rhs=pTa[r][:, yi * 128:(yi + 1) * 128],
                                                                                         start=(i_ == 0), stop=(i_ == L - 1)),
                                 reads=[("pTa", r)], writes=[f"ps{ab}"])
                        if hh == 1:
                            nb_, ni = n // NB, n % NB
                            bi = (c * (NT // NB) + nb_) % 2
                            rw, rk = raw[bi], ("raw", bi)
                            P.op("dve", lambda e: e.tensor_copy(out=rw[:, ni, :], in_=acc[:, 0:256]), reads=[f"ps{ab}"], writes=[rk])
                            if ni == NB - 1:
                                ns = slice(nb_ * NB * 128, (nb_ + 1) * NB * 128)
                                P.op("pool", lambda e: e.tensor_copy(out=dsh2[0:64, :, :], in_=rw[64:128, :, 0:128]), reads=[rk], writes=["dsh2a"])
                                P.op("dve", lambda e: e.tensor_copy(out=dsh2[64:128, :, :], in_=rw[0:64, :, 128:256]), reads=[rk], writes=["dsh2b"])
                                P.op("dve", lambda e: e.tensor_scalar(out=dsh2[0:64, :, :], in0=dsh2[0:64, :, :], scalar1=esink[0:64, c:c + 1], scalar2=None, op0=ALU.add), reads=["dsh2a"], writes=["dsh2a"])
                                P.op("dve", lambda e: e.tensor_scalar(out=dsh2[64:128, :, :], in0=dsh2[64:128, :, :], scalar1=esink[64:128, c + 4:c + 5], scalar2=None, op0=ALU.add), reads=["dsh2b"], writes=["dsh2b"])
                                P.op("act", lambda e: e.activation(out=dsh2[:], in_=dsh2[:], func=AF.Ln), reads=["dsh2a", "dsh2b"], writes=["dsh2a", "dsh2b"])
                                P.op("act", lambda e: e.activation(out=dsh2[:], in_=dsh2[:], func=AF.Exp, scale=-1.0), reads=["dsh2a", "dsh2b"], writes=["dsh2a", "dsh2b"])
                                P.op("dve", lambda e: e.tensor_tensor(out=oA[0:64, c, ns].rearrange("p (a b) -> p a b", a=NB), in0=rw[0:64, :, 0:128], in1=dsh2[0:64, :, :], op=ALU.mult),
                                     reads=[rk, "dsh2a"], writes=[("oA", c, nb_, 0)])
                                P.op("dve", lambda e: e.tensor_tensor(out=oA[64:128, c, ns].rearrange("p (a b) -> p a b", a=NB), in0=rw[64:128, :, 128:256], in1=dsh2[64:128, :, :], op=ALU.mult),
                                     reads=[rk, "dsh2b"], writes=[("oA", c, nb_, 1)])

                    qk(*items[0])
                    for i in range(len(items)):
                        if i + 1 < len(items):
                            qk(*items[i + 1])
                        pv(*items[i])
                dump(P, "oA", oA)
                P.flush(blk)

        R_OB = R_OA + 32
        oB = A.at("oB", [128, 4, S], BF16, KB(R_OB))
        if stop_after >= 3:
            A.lo = KB(R_OB + 32)
            A2 = Arena(nc)
            A2.n = 5000
            A2.lo = KB(R_A)
            LIM2 = KB(R_OA)
            LIM = SB_END
            QT = [A2.tmp(f"QT{i}", [128, S], BF16, LIM2) for i in range(2)]
            VO = [A2.tmp(f"VO{i}", [128, NT, 128], BF16, LIM2) for i in range(2)]
            CSG = A2.tmp("CSG", [128, S], F32, LIM2)
            Wqb = A2.tmp("Wqb", [128, 3, 1024], BF16, LIM2)
            Wkv = A.tmp("Wkv", [128, 2, 1024], BF16, SB_END)
            pTb = [A.tmp(f"pTb{i}", [128, 512], BF16, LIM) for i in range(4)]
            sq3 = [A.tmp(f"sq3{i}", [128, 512], BF16, LIM) for i in range(3)]
            ri3 = [A.tmp(f"ri3{i}", [128, 512], F32, LIM) for i in range(3)]
            u3 = [A.tmp(f"u3{i}", [128, 512], F32, LIM) for i in range(2)]
            kt3 = A.tmp("kt3", [128, 512], BF16, LIM)
            dshb = [A.tmp(f"dshb{i}", [128, 512], F32, LIM) for i in range(2)]
            SCALE_B = 96.0 ** -0.5
            with nc.Block() as blk:
                P.op("sp", lambda e: e.dma_start(out=CSG[:], in_=csq_d), writes=["CSG"], dma=True, key="p3a")
                P.op("pool", lambda e: e.dma_start(out=Wqb[:], in_=wqb_d.rearrange("(k p) f -> p k f", p=128)), writes=["Wqb"], dma=True, key="p3b")
                P.op("pool", lambda e: e.dma_start(out=Wkv[:], in_=wkvb_d.rearrange("(k p) f -> p k f", p=128)), writes=["Wkv"], dma=True, key="p3c")
                P.op("dve", lambda e: e.tensor_scalar(out=CSG[:], in0=CSG[:], scalar1=gcols[:, GC_QB:GC_QB + 1], scalar2=None, op0=ALU.mult), reads=["CSG"], writes=["CSG"])
                P.op("pool", lambda e: e.memset(VO[0][:, :, 64:128], 1.0), writes=["VO0ones"])
                P.op("pool", lambda e: e.memset(VO[1][:, :, 0:64], 1.0), writes=["VO1ones"])
                for hp in range(4):
                    for G in range(NG):
                        gs = slice(G * 512, (G + 1) * 512)
                        kp, qp0, qp1, vp = psum[5], psum[0], psum[1], psum[7]
                        for i in range(2):
                            P.op("pe", lambda e, i=i, gs=gs, hp=hp, kp=kp: e.matmul(kp[:], lhsT=Wkv[:, i, hp * 128:(hp + 1) * 128], rhs=ckvn[:, i, gs], start=(i == 0), stop=(i == 1)),
                                 reads=["Wkv"], writes=["ps5"])
                        for hh, qp in ((0, qp0), (1, qp1)):
                            h = 2 * hp + hh
                            for i in range(3):
                                P.op("pe", lambda e, i=i, gs=gs, h=h, qp=qp: e.matmul(qp[:], lhsT=Wqb[:, i, h * 128:(h + 1) * 128], rhs=cqn[:, i, gs], start=(i == 0), stop=(i == 2)),
                                     reads=["Wqb"], writes=[f"ps{hh}"])
                        for t in range(4):
                            ti = G * 4 + t
                            for i in range(2):
                                P.op("pe", lambda e, i=i, ti=ti, t=t, hp=hp, vp=vp: e.matmul(vp[:, t * 128:(t + 1) * 128], lhsT=ckvn[:, i, ti * 128:(ti + 1) * 128], rhs=Wkv[:, i, 512 + hp * 128:512 + (hp + 1) * 128],
                                                                               start=(i == 0), stop=(i == 1)),
                                     reads=["Wkv"], writes=["ps7"])
                        srcs = ((kp, "ps5", bd64, psum[6], "ps6"), (qp0, "ps0", bdq, psum[2], "ps2"), (qp1, "ps1", bdq, psum[3], "ps3"))
                        for z_, (pp, pk, bd_, mp, mk) in enumerate(srcs):
                            P.op("act", lambda e, pp=pp, z_=z_: e.activation(out=sq3[z_][:], in_=pp[:], func=AF.Square), reads=[pk], writes=[("sq3", z_)])
                        for z_, (pp, pk, bd_, mp, mk) in enumerate(srcs):
                            P.op("pe", lambda e, bd_=bd_, mp=mp, z_=z_: e.matmul(mp[:], lhsT=bd_, rhs=sq3[z_][:], start=True, stop=True), reads=[("sq3", z_)], writes=[mk])
                        for z_, (pp, pk, bd_, mp, mk) in enumerate(srcs):
                            P.op("act", lambda e, mp=mp, z_=z_: e.activation(out=ri3[z_][:], in_=mp[:], func=AF.Ln, bias=gcols[:, GC_EPS:GC_EPS + 1]), reads=[mk], writes=[("ri3", z_)])
                            P.op("act", lambda e, z_=z_: e.activation(out=ri3[z_][:], in_=ri3[z_][:], func=AF.Exp, scale=-0.5), reads=[("ri3", z_)], writes=[("ri3", z_)])
                        vp3 = vp[:].rearrange("p (a b) -> p a b", a=4)
                        P.op("dve", lambda e, G=G, vp3=vp3: e.tensor_copy(out=VO[0][:, G * 4:(G + 1) * 4, 0:64], in_=vp3[:, :, 0:64]), reads=["ps7"], writes=[("VO0", G)])
                        P.op("dve", lambda e, G=G, vp3=vp3: e.tensor_copy(out=VO[1][:, G * 4:(G + 1) * 4, 64:128], in_=vp3[:, :, 64:128]), reads=["ps7"], writes=[("VO1", G)])
                        P.op("dve", lambda e, gs=gs, kp=kp: e.scalar_tensor_tensor(out=KT[0][0:64, gs], in0=kp[0:64, :], scalar=gcols[0:64, GC_KN:GC_KN + 1], in1=ri3[0][0:64, :], op0=ALU.mult, op1=ALU.mult),
                             reads=["ps5", ("ri3", 0)], writes=[("KT0n", G)])
                        P.op("dve", lambda e, kp=kp: e.scalar_tensor_tensor(out=kt3[64:128, :], in0=kp[64:128, :], scalar=gcols[64:128, GC_KN:GC_KN + 1], in1=ri3[0][64:128, :], op0=ALU.mult, op1=ALU.mult),
                             reads=["ps5", ("ri3", 0)], writes=["kt3"])
                        P.op("pool", lambda e, gs=gs: e.tensor_copy(out=KT[1][0:64, gs], in_=kt3[64:128, :]), reads=["kt3"], writes=[("KT1n", G)])
                        for hh, qp in ((0, qp0), (1, qp1)):
                            P.op("pool", lambda e, gs=gs, hh=hh: e.tensor_tensor(out=u3[hh][:], in0=ri3[1 + hh][:], in1=CSG[:, gs], op=ALU.mult), reads=[("ri3", 1 + hh), "CSG"], writes=[("u3", hh)])
                            P.op("dve", lambda e, gs=gs, hh=hh, qp=qp: e.tensor_tensor(out=QT[hh][:, gs], in0=qp[:], in1=u3[hh][:], op=ALU.mult), reads=[f"ps{hh}", ("u3", hh)], writes=[("QT", hh, G)])
                    wcast((8, 8, 7, 7)[hp])
                    for hh in range(2):
                        nr = slice(0, 64) if hh == 0 else slice(64, 128)
                        dr = slice(64, 128) if hh == 0 else slice(0, 64)
                        kdeps = [("KT0n" if hh == 0 else "KT1n", G) for G in range(NG)]
                        for qg in range(NG):
                            qs = slice(qg * 512, (qg + 1) * 512)
                            ab = 3 + qg % 2
                            acc = psum[ab]

                            def QK(kt, hh=hh, qs=qs, qg=qg):
                                b = kt % 3
                                P.op("pe", lambda e, b=b, kt=kt: e.matmul(psum[b][:], lhsT=KT[hh][:, kt * 128:(kt + 1) * 128], rhs=QT[hh][:, qs], start=True, stop=True),
                                     reads=[("KT0n" if hh == 0 else "KT1n", kt // 4), ("QT", hh, qg)], writes=[f"ps{b}"])
                                P.op("act", lambda e, b=b, kt=kt: e.activation(out=pTb[kt % 4][:], in_=psum[b][:], func=AF.Exp, scale=SCALE_B),
                                     reads=[f"ps{b}"], writes=[("pTb", kt % 4)])

                            def PV(kt, hh=hh, acc=acc, ab=ab):
                                P.op("pe", lambda e, kt=kt: e.matmul(acc[:], lhsT=VO[hh][:, kt, :], rhs=pTb[kt % 4][:], start=(kt == 0), stop=(kt == NT - 1)),
                                     reads=[("pTb", kt % 4), (f"VO{hh}", kt // 4), f"VO{hh}ones"], writes=[f"ps{ab}"])
                            QK(0)
                            QK(1)
                            for kt in range(NT):
                                if kt + 2 < NT:
                                    QK(kt + 2)
                                PV(kt)
                            d_ = dshb[qg % 2]
                            P.op("dve", lambda e, acc=acc, d_=d_, nr=nr, dr=dr: e.tensor_copy(out=d_[nr, :], in_=acc[dr, :]), reads=[f"ps{ab}"], writes=[("dshb", qg % 2)])
                            P.op("dve", lambda e, d_=d_, nr=nr: e.reciprocal(out=d_[nr, :], in_=d_[nr, :]), reads=[("dshb", qg % 2)], writes=[("dshb", qg % 2)])
                            P.op("dve", lambda e, acc=acc, d_=d_, nr=nr, qs=qs, hp=hp: e.tensor_tensor(out=oB[nr, hp, qs], in0=acc[nr, :], in1=d_[nr, :], op=ALU.mult),
                                 reads=[f"ps{ab}", ("dshb", qg % 2)], writes=[("oB", hp, qg, hh)])
                dump(P, "oB", oB)
                P.flush(blk)


        R_AF = R_C
        affT = A.at("affT", [128, S], F32, KB(R_AF))
        if stop_after >= 4:
            A.lo = KB(R_AF + 16)
            LIM = KB(R_OA)
            Wo = A.tmp("Wo", [128, 8, 1024], BF16, LIM)
            g2bc = A.tmp("g2bc", [128, 1024], F32, LIM)
            wr = A.tmp("wr", [128, 8, 128], F32, LIM)
            xt = [A.tmp(f"xt{i}", [128, 1024], F32, LIM) for i in range(2)]
            x1 = [A.tmp(f"x1{i}", [128, 1024], F32, LIM) for i in range(2)]
            h2f = [A.tmp(f"h2f{i}", [128, 1024], F32, LIM) for i in range(2)]
            h2b = [A.tmp(f"h2b{i}", [128, 1024], BF16, LIM) for i in range(2)]
            h2Tb_ = [A.tmp(f"h2T{i}", [128, 8, 128], F32, LIM) for i in range(2)]
            sqA = A.tmp("sqA", [128, 4, 128], BF16, LIM)
            sqB = A.tmp("sqB", [128, 4, 128], BF16, LIM)
            afp = [A.tmp(f"afp{i}", [128, 128], F32, LIM) for i in range(2)]
            sm = A.tmp("sm", [128, 16], F32, LIM)
            with nc.Block() as blk:
                P.op("pool", lambda e: e.dma_start(out=Wo[:], in_=wo_d.rearrange("(k p) f -> p k f", p=128)), writes=["Wo"], dma=True, key="p4a")
                P.op("sp", lambda e: e.dma_start(out=g2bc[:], in_=ln2_d.partition_broadcast(128)), writes=["g2bc"], dma=True, key="p4b")
                P.op("sp", lambda e: e.dma_start(out=wr[:], in_=wr_d.rearrange("(k p) f -> p k f", p=128)), writes=["wr"], dma=True, key="p4c")
                for c in range(8):
                    P.op("dve", lambda e, c=c: e.tensor_scalar(out=Wo[:, c, :], in0=Wo[:, c, :], scalar1=gcols[:, GC_OA + c:GC_OA + c + 1], scalar2=None, op0=ALU.mult),
                         reads=["Wo"], writes=["Wo"])
                P.op("pool", lambda e: e.memset(afp[0][:], 0.0), writes=[("afp", 0)])
                P.op("pool", lambda e: e.memset(afp[1][:], 0.0), writes=[("afp", 1)])
                def stA(t):
                    ts_ = slice(t * 128, (t + 1) * 128)
                    b = t % 2
                    P.op("sp", lambda e, b=b, ts_=ts_: e.dma_start(out=xt[b][:], in_=x[ts_, :]), writes=[("xt", b)], dma=True, key=f"xt{b}")
                    P.op("act", lambda e, ts_=ts_: e.activation(out=sqA[:], in_=oA[:, :, ts_], func=AF.Square), writes=["sqA"])
                    P.op("act", lambda e, ts_=ts_: e.activation(out=sqB[:], in_=oB[:, :, ts_], func=AF.Square), writes=["sqB"])
                    ss = psum[6]
                    for c in range(4):
                        P.op("pe", lambda e, c=c: e.matmul(ss[:, 0:128], lhsT=sqA[:, c, :], rhs=ones_bf, start=(c == 0), stop=(c == 3)), reads=["sqA"], writes=["ps6"])
                    for c in range(4):
                        P.op("pe", lambda e, c=c: e.matmul(ss[:, 128:256], lhsT=sqB[:, c, :], rhs=ones_bf, start=(c == 0), stop=(c == 3)), reads=["sqB"], writes=["ps6"])
                    P.op("act", lambda e: e.activation(out=sm[:, 0:2], in_=ss[:, 0:256:128], func=AF.Sqrt, scale=1.0 / 512, bias=gcols[:, GC_EPS:GC_EPS + 1]), reads=["ps6"], writes=["sm01"])
                    P.op("dve", lambda e: e.reciprocal(out=sm[:, 2:4], in_=sm[:, 0:2]), reads=["sm01"], writes=["sm23"])
                    for hf in range(2):
                        hs = slice(hf * 512, (hf + 1) * 512)
                        for c in range(4):
                            P.op("pe", lambda e, c=c, hs=hs, hf=hf, ts_=ts_: e.matmul(psum[hf][:], lhsT=oA[:, c, ts_], rhs=Wo[:, c, hs], start=(c == 0), stop=(c == 3)), reads=["Wo"], writes=[f"ps{hf}"])
                        for c in range(4):
                            P.op("pe", lambda e, c=c, hs=hs, hf=hf, ts_=ts_: e.matmul(psum[2 + hf][:], lhsT=oB[:, c, ts_], rhs=Wo[:, 4 + c, hs], start=(c == 0), stop=(c == 3)), reads=["Wo"], writes=[f"ps{2 + hf}"])
                        P.op("dve", lambda e, hs=hs, hf=hf, b=b: e.scalar_tensor_tensor(out=x1[b][:, hs], in0=psum[hf][:], scalar=sm[:, 2:3], in1=xt[b][:, hs], op0=ALU.mult, op1=ALU.add),
                             reads=[f"ps{hf}", "sm23", ("xt", b)], writes=[("x1", b, hf)])
                        P.op("dve", lambda e, hs=hs, hf=hf, b=b: e.scalar_tensor_tensor(out=x1[b][:, hs], in0=psum[2 + hf][:], scalar=sm[:, 3:4], in1=x1[b][:, hs], op0=ALU.mult, op1=ALU.add),
                             reads=[f"ps{2 + hf}", "sm23", ("x1", b, hf)], writes=[("x1", b, hf)])
                    X1 = [("x1", b, 0), ("x1", b, 1)]
                    P.op("sp", lambda e, b=b, ts_=ts_: e.dma_start(out=out[ts_, :], in_=x1[b][:]), reads=X1, dma=True, key=f"o{b}")

                def stB1(t):
                    ts_ = slice(t * 128, (t + 1) * 128)
                    b = t % 2
                    h2T = h2Tb_[b]
                    X1 = [("x1", b, 0), ("x1", b, 1)]
                    P.op("dve", lambda e, b=b: e.tensor_tensor(out=h2f[b][:], in0=x1[b][:], in1=x1[b][:], op=ALU.mult), reads=X1, writes=[("h2f", b)])
                    P.op("dve", lambda e, b=b: e.reduce_sum(out=sm[:, 4:5], in_=h2f[b][:], axis=mybir.AxisListType.X), reads=[("h2f", b)], writes=["sm4"])
                    P.op("act", lambda e: e.activation(out=sm[:, 5:6], in_=sm[:, 4:5], func=AF.Sqrt, scale=1.0 / D, bias=gcols[:, GC_EPS:GC_EPS + 1]), reads=["sm4"], writes=["sm5"])
                    P.op("dve", lambda e: e.reciprocal(out=sm[:, 6:7], in_=sm[:, 5:6]), reads=["sm5"], writes=["sm6"])
                    P.op("dve", lambda e, b=b: e.scalar_tensor_tensor(out=h2f[b][:], in0=x1[b][:], scalar=sm[:, 6:7], in1=g2bc[:], op0=ALU.mult, op1=ALU.mult),
                         reads=X1 + ["sm6", "g2bc", ("h2f", b)], writes=[("h2f", b)])
                    P.op("pool", lambda e, b=b: e.tensor_copy(out=h2b[b][:], in_=h2f[b][:]), reads=[("h2f", b)], writes=[("h2b", b)])
                    P.op("sp", lambda e, b=b, ts_=ts_: e.dma_start(out=h2_dram[ts_, :], in_=h2b[b][:]), reads=[("h2b", b)], dma=True, key=f"h2o{b}")
                    for k in range(8):
                        pb = 4 + k // 4
                        P.op("pe", lambda e, k=k, pb=pb, b=b: e.transpose(psum[pb][:, (k % 4) * 128:(k % 4 + 1) * 128], h2f[b][:, k * 128:(k + 1) * 128], identf[:]),
                             reads=[("h2f", b), "identf"], writes=[f"ps{pb}"])
                    P.op("dve", lambda e: e.tensor_copy(out=h2T[:, 0:4, :], in_=psum[4][:].rearrange("p (a b) -> p a b", a=4)), reads=["ps4"], writes=[("h2Ta", b)])
                    P.op("dve", lambda e: e.tensor_copy(out=h2T[:, 4:8, :], in_=psum[5][:].rearrange("p (a b) -> p a b", a=4)), reads=["ps5"], writes=[("h2Tb", b)])

                def stB2(t):
                    ts_ = slice(t * 128, (t + 1) * 128)
                    b = t % 2
                    h2T = h2Tb_[b]
                    lg = psum[7]
                    for k in range(8):
                        P.op("pe", lambda e, k=k: e.matmul(lg[:, 0:128], lhsT=h2T[:, k, :], rhs=wr[:, k, :], start=(k == 0), stop=(k == 7)),
                             reads=[("h2Ta", b), ("h2Tb", b), "wr"], writes=["ps7"])
                    af = afp[b]
                    P.op("dve", lambda e: e.reduce_max(out=sm[:, 7:8], in_=lg[:, 0:16], axis=mybir.AxisListType.X), reads=["ps7"], writes=["sm7"])
                    P.op("dve", lambda e: e.tensor_scalar(out=sm[:, 8:9], in0=sm[:, 7:8], scalar1=-1.0, scalar2=None, op0=ALU.mult), reads=["sm7"], writes=["sm8"])
                    P.op("act", lambda e, af=af: e.activation(out=af[:, 0:16], in_=lg[:, 0:16], func=AF.Exp, bias=sm[:, 8:9]), reads=["ps7", "sm8"], writes=[("afp", b)])
                    P.op("dve", lambda e, af=af: e.reduce_sum(out=sm[:, 9:10], in_=af[:, 0:16], axis=mybir.AxisListType.X), reads=[("afp", b)], writes=["sm9"])
                    P.op("dve", lambda e: e.reciprocal(out=sm[:, 10:11], in_=sm[:, 9:10]), reads=["sm9"], writes=["sm10"])
                    P.op("dve", lambda e, af=af: e.tensor_scalar(out=af[:, 0:16], in0=af[:, 0:16], scalar1=sm[:, 10:11], scalar2=None, op0=ALU.mult), reads=["sm10", ("afp", b)], writes=[("afp", b)])
                    P.op("sp", lambda e, af=af, ts_=ts_: e.dma_start(out=aff_dram[ts_, :], in_=af[:, 0:16]), reads=[("afp", b)], dma=True, key=f"afo{b}")
                    P.op("pe", lambda e, af=af: e.transpose(lg[:, 128:256], af[:], identf[:]), reads=[("afp", b)], writes=["ps7"])
                    P.op("dve", lambda e, ts_=ts_: e.tensor_copy(out=affT[:, ts_], in_=lg[:, 128:256]), reads=["ps7"], writes=[("affT", t)])


                for t in range(NT):
                    stA(t)
                    if t >= 1:
                        stB1(t - 1)
                    if t >= 2:
                        stB2(t - 2)
                stB1(NT - 1)
                stB2(NT - 2)
                stB2(NT - 1)
                dump(P, "affT", affT)
                P.flush(blk)

        NWB = 7
        WM = [A.at(f"WM{i}", [128, 8, 1024], BF16, KB(20 + 16 * i)) for i in range(NWB)]
        def wload_mat(i):
            if i >= 3 * NE:
                return
            sl = i % NWB
            P.op("sp", lambda e: e.dma_start(out=WM[sl][:], in_=w16[i].rearrange("(k p) f -> p k f", p=128)), writes=[("WM", sl)], dma=True, key=f"wm{sl}")

        if stop_after >= 5:
            A.lo = KB(132)
            LIM = SB_END
            junk = A.tmp("junk", [128, S], BF16, LIM)
            mask = A.tmp("mask", [128, S], F32, LIM)
            cum = A.tmp("cum", [128, S], F32, LIM)
            cumh = A.tmp("cumh", [128, S], F16, LIM)
            bs = A.tmp("bs", [128, 8], F32, LIM)
            with nc.Block() as blk:
                if stop_after >= 6:
                    for i in range(NWB):
                        wload_mat(i)
                P.op("dve", lambda e: e.memset(bs[:, 0:1], 0.0), writes=["bs"])
                P.op("dve", lambda e: e.memset(bs[:, 1:2], 1.0), reads=["bs"], writes=["bs"])
                P.op("dve", lambda e: e.memset(bs[:, 2:3], 0.5), reads=["bs"], writes=["bs"])
                for it_ in range(30):
                    hn = 2.0 ** -(it_ + 2)
                    P.op("dve", lambda e: e.tensor_tensor(out=bs[:, 5:6], in0=bs[:, 2:3], in1=bs[:, 0:1], op=ALU.subtract), reads=["bs"], writes=["bs"])
                    P.op("dve", lambda e: e.tensor_scalar(out=junk[:], in0=affT[:], scalar1=bs[:, 2:3], scalar2=None, op0=ALU.is_ge, op1=ALU.add, accum_out=bs[:, 3:4]),
                         reads=["bs"], writes=["bs", "junk"])
                    P.op("dve", lambda e: e.tensor_single_scalar(out=bs[:, 4:5], in_=bs[:, 3:4], scalar=CAP - 0.5, op=ALU.is_ge), reads=["bs"], writes=["bs"])
                    P.op("dve", lambda e: e.scalar_tensor_tensor(out=bs[:, 0:1], in0=bs[:, 5:6], scalar=bs[:, 4:5], in1=bs[:, 0:1], op0=ALU.mult, op1=ALU.add), reads=["bs"], writes=["bs"])
                    P.op("dve", lambda e, hn=hn: e.tensor_scalar(out=bs[:, 2:3], in0=bs[:, 0:1], scalar1=hn, scalar2=None, op0=ALU.add), reads=["bs"], writes=["bs"])
                P.op("dve", lambda e: e.tensor_scalar(out=mask[:], in0=affT[:], scalar1=bs[:, 0:1], scalar2=None, op0=ALU.is_ge), reads=["bs"], writes=["mask"])
                P.op("dve", lambda e: e.tensor_tensor_scan(out=cum[:], data0=mask[:], data1=mask[:], initial=0.0, op0=ALU.add, op1=ALU.max), reads=["mask"], writes=["cum"])
                P.op("dve", lambda e: e.tensor_scalar(out=cumh[:], in0=cum[:], scalar1=1000.0, scalar2=None, op0=ALU.min), reads=["cum"], writes=["cumh"])
                P.op("sp", lambda e: e.dma_start(out=cum_dram, in_=cumh[0:16, :]), reads=["cumh"], dma=True, key="p5a")
                dump(P, "cum", cum)
                P.flush(blk)

        if stop_after >= 6:
            A.lo = KB(132)
            LIM = SB_END
            cbc = A.tmp("cbc", [128, S], F16, LIM)
            junk6 = A.tmp("junk6", [128, S], BF16, LIM)
            xg = [[A.tmp(f"xg{b}{i}", [128, 1024], BF16, LIM) for i in range(4)] for b in range(2)]
            xeT = [A.tmp(f"xeT{b}", [128, 8, 512], BF16, LIM) for b in range(2)]
            actT = A.tmp("actT", [128, 8, 512], BF16, LIM)
            sg = [A.tmp(f"sg{i}", [128, 512], F32, LIM) for i in range(2)]
            ye = [A.tmp(f"ye{i}", [128, 1024], F32, LIM) for i in range(2)]
            gt = [[A.tmp(f"gt{b}{i}", [128, 16], F32, LIM) for i in range(4)] for b in range(2)]
            idxf = A.tmp("idxf", [128, 4], F32, LIM)
            idxi = [A.tmp(f"idxi{i}", [128, 4], I32, LIM) for i in range(2)]
            with nc.Block() as blk:
                prev_sc = [[]]

                def s1_cbc(ex):
                    P.op("sp", lambda e: e.dma_start(out=cbc[:], in_=cum_dram[ex:ex + 1, :].partition_broadcast(128)), writes=["cbc"], dma=True, key="cbc")

                def s1_cmp(ex, j):
                    P.op("dve", lambda e: e.tensor_scalar(out=junk6[:], in0=cbc[:], scalar1=ccol[:, j:j + 1], scalar2=None, op0=ALU.is_le, op1=ALU.add, accum_out=idxf[:, j:j + 1]),
                         reads=["cbc"], writes=["junk6", ("idxf", j)])

                def s1_gather(ex):
                    b = ex % 2
                    ii = idxi[b]
                    IDF = [("idxf", j) for j in range(4)]
                    P.op("dve", lambda e: e.tensor_scalar(out=idxf[:], in0=idxf[:], scalar1=float(S - 1), scalar2=None, op0=ALU.min), reads=IDF, writes=IDF)
                    P.op("dve", lambda e: e.tensor_copy(out=ii[:], in_=idxf[:]), reads=IDF, writes=[("idxi", b)])
                    for j in range(4):
                        P.op("pool", lambda e, j=j: e.indirect_dma_start(out=xg[b][j][:], out_offset=None, in_=h2_dram, in_offset=bass.IndirectOffsetOnAxis(ap=ii[:, j:j + 1], axis=0)),
                             reads=[("idxi", b)], writes=[("xg", b, j)], dma=True, key=f"xg{b}{j}")
                        P.op("pool", lambda e, j=j: e.indirect_dma_start(out=gt[b][j][:], out_offset=None, in_=aff_dram, in_offset=bass.IndirectOffsetOnAxis(ap=ii[:, j:j + 1], axis=0)),
                             reads=[("idxi", b)], writes=[("gt", b, j)], dma=True, key=f"gt{b}{j}")

                def s1_tr(ex):
                    b = ex % 2
                    for j in range(4):
                        tpb = psum[j % 2][:].bitcast(BF16)
                        for k in range(8):
                            P.op("pe", lambda e, j=j, k=k, tpb=tpb: e.transpose(tpb[:, k * 128:(k + 1) * 128], xg[b][j][:, k * 128:(k + 1) * 128], ident_bf),
                                 reads=[("xg", b, j)], writes=[f"ps{j % 2}"])
                        P.op("dve", lambda e, j=j, tpb=tpb: e.tensor_copy(out=xeT[b][:, :, j * 128:(j + 1) * 128], in_=tpb.rearrange("p (a b) -> p a b", a=8)),
                             reads=[f"ps{j % 2}"], writes=[("xeT", b, j)])

                def s2_gu(ex, fc):
                    b = ex % 2
                    sg_, su_ = (3 * ex) % NWB, (3 * ex + 1) % NWB
                    XE = [("xeT", b, j) for j in range(4)]
                    gb, ub = 2 + fc % 2, 4 + fc % 2
                    for k in range(8):
                        P.op("pe", lambda e, k=k: e.matmul(psum[gb][:], lhsT=WM[sg_][:, k, fc * 128:(fc + 1) * 128], rhs=xeT[b][:, k, :], start=(k == 0), stop=(k == 7)),
                             reads=XE + [("WM", sg_)], writes=[f"ps{gb}"])
                    for k in range(8):
                        P.op("pe", lambda e, k=k: e.matmul(psum[ub][:], lhsT=WM[su_][:, k, fc * 128:(fc + 1) * 128], rhs=xeT[b][:, k, :], start=(k == 0), stop=(k == 7)),
                             reads=XE + [("WM", su_)], writes=[f"ps{ub}"])
                    P.op("act", lambda e: e.activation(out=sg[fc % 2][:], in_=psum[gb][:], func=AF.Silu), reads=[f"ps{gb}"], writes=[("sg", fc % 2)])
                    P.op("dve", lambda e: e.tensor_tensor(out=actT[:, fc, :], in0=psum[ub][:], in1=sg[fc % 2][:], op=ALU.mult),
                         reads=[f"ps{ub}", ("sg", fc % 2)], writes=[("actT", fc)])

                def s2_down(ex):
                    b = ex % 2
                    sd_ = (3 * ex + 2) % NWB
                    ii = idxi[b]
                    AC = [("actT", fc) for fc in range(8)]
                    for j in range(4):
                        yb_ = ye[j % 2]
                        for hf in range(2):
                            pb = 6 + hf
                            for fc in range(8):
                                P.op("pe", lambda e, fc=fc, j=j, hf=hf, pb=pb: e.matmul(psum[pb][:], lhsT=actT[:, fc, j * 128:(j + 1) * 128], rhs=WM[sd_][:, fc, hf * 512:(hf + 1) * 512],
                                                                                      start=(fc == 0), stop=(fc == 7)),
                                     reads=AC + [("WM", sd_)], writes=[f"ps{pb}"])
                            P.op("dve", lambda e, j=j, hf=hf, pb=pb, yb_=yb_: e.tensor_scalar(out=yb_[:, hf * 512:(hf + 1) * 512], in0=psum[pb][:], scalar1=gt[b][j][:, ex:ex + 1], scalar2=None, op0=ALU.mult),
                                 reads=[f"ps{pb}", ("gt", b, j)], writes=[("ye", j % 2, hf)])
                        o_ = P.op("pool", lambda e, j=j, yb_=yb_: e.indirect_dma_start(out=out, out_offset=bass.IndirectOffsetOnAxis(ap=ii[:, j:j + 1], axis=0), in_=yb_[:], in_offset=None, compute_op=ALU.add),
                                  reads=[("ye", j % 2, 0), ("ye", j % 2, 1), ("idxi", b)], deps=list(prev_sc[0]), dma=True, key=f"sc{j % 2}")
                        prev_sc[0] = [o_]

                s1_cbc(0)
                for j in range(4):
                    s1_cmp(0, j)
                s1_gather(0)
                s1_tr(0)
                for ex in range(NE):
                    nx = ex + 1
                    if nx < NE:
                        s1_cbc(nx)
                    for fc in range(8):
                        s2_gu(ex, fc)
                        if nx < NE and fc < 4:
                            s1_cmp(nx, fc)
                        if nx < NE and fc == 3:
                            s1_gather(nx)
                    wload_mat(3 * ex + NWB)
                    wload_mat(3 * ex + 1 + NWB)
                    if nx < NE:
                        s1_tr(nx)
                    s2_down(ex)
                    wload_mat(3 * ex + 2 + NWB)
                P.flush(blk)

    return nc, dbg


def t5_bucket_np(rel):
    half, max_exact = 16, 8
    base = np.where(rel > 0, half, 0)
    n = np.abs(rel)
    nf = np.maximum(n, 1).astype(np.float32)
    large = max_exact + (np.log(nf / max_exact) / math.log(128 / max_exact) * (half - max_exact)).astype(np.int32)
    large = np.minimum(large, half - 1)
    return base + np.where(n < max_exact, n, large)


def host_constants():
    c = {}
    cm = np.zeros((128, 6, 128), np.float32)
    cm[:, 0, :] = np.eye(128)
    cm[:, 1, :] = 1.0
    cm[0:64, 2, 0:64] = 1 / 64
    cm[64:128, 2, 64:128] = 1 / 64
    cm[0:64, 3, 0:64] = 1 / 64
    cm[64:96, 3, 64:96] = 1 / 32
    cm[96:128, 3, 96:128] = 1 / 32
    cm[0:32, 4, 0:32] = 1 / 32
    cm[32:64, 4, 32:64] = 1 / 32
    for j in range(64):
        for i in range(64, 128):
            if (j % 32) == ((i - 64) % 32):
                cm[j, 5, i] = 1.0
    c["cmat"] = cm
    pos = np.arange(S, dtype=np.float32)
    freqs = (np.float32(10000.0) ** (-np.arange(0, 32, 2, dtype=np.float32) / np.float32(32))).astype(np.float32)
    ang = pos[:, None] * freqs[None, :]
    cos, sin = np.cos(ang).astype(np.float32).T, np.sin(ang).astype(np.float32).T
    cs_main = np.concatenate([cos, cos], 0)
    cs_sw = np.concatenate([-sin, sin], 0)
    c["csk"] = np.ascontiguousarray(np.concatenate([cs_main, cs_sw, np.zeros((64, S), np.float32)], 0))
    c["csq"] = np.ascontiguousarray(np.concatenate([np.ones((64, S), np.float32), cs_main, cs_sw], 0))
    i = np.arange(640)
    rel = 255 - i
    valid = np.abs(rel) <= 128
    bk = t5_bucket_np(rel)
    oh = np.zeros((128, 640), np.float32)
    oh[bk[valid], i[valid]] = 1.0
    c["ohrel"] = oh
    c["maskadd"] = np.tile(np.where(valid, 0.0, -30000.0).astype(np.float32)[None, :], (128, 1))
    c["ccol"] = (np.arange(4)[None, :] * 128 + np.arange(128)[:, None]).astype(np.float32)
    return c


def host_layout(inp):
    f = lambda a: np.ascontiguousarray(np.asarray(a, dtype=np.float32))
    w_in = f(inp["w_in"])[0]
    sw = np.concatenate([np.arange(16, 32), np.arange(0, 16)])
    qperm = np.concatenate([np.concatenate([np.arange(c * 64, c * 64 + 64), np.arange((c + 4) * 64, (c + 4) * 64 + 64)]) for c in range(4)])
    cols = [w_in[:, 0:512][:, qperm], w_in[:, 512:640], w_in[:, 768:1152], w_in[:, 1152:1408],
            w_in[:, 1408:1440], w_in[:, 1408:1440][:, sw], np.zeros((D, 64), np.float32), np.zeros((D, 128), np.float32),
            w_in[:, 640:768]]
    w_aug = np.concatenate(cols, 1)
    assert w_aug.shape[1] == 1664, w_aug.shape
    g = np.zeros((128, NGC), np.float32)
    g[:, GC_Q2] = np.tile(f(inp["a_q_norm_g"])[0], 2)
    g[:, GC_K2] = np.tile(f(inp["a_k_norm_g"])[0], 2)
    g[:, GC_CQ:GC_CQ + 3] = f(inp["cq_norm_g"])[0].reshape(3, 128).T
    g[:, GC_CKV:GC_CKV + 2] = f(inp["ckv_norm_g"])[0].reshape(2, 128).T
    kr = f(inp["b_kr_g"])[0]
    g[0:32, GC_KR] = kr
    g[32:64, GC_KR] = kr[sw]
    qr = f(inp["b_qr_g"])[0]
    g[:, GC_QB] = np.concatenate([f(inp["b_qn_g"])[0], qr, qr[sw]])
    g[:, GC_KN] = np.tile(f(inp["b_kn_g"])[0], 2)
    g[:, GC_LN1:GC_LN1 + 8] = f(inp["ln1_g"])[0].reshape(8, 128).T
    g[:, GC_OA:GC_OA + 4] = f(inp["out_a_g"])[0][qperm].reshape(4, 128).T
    g[:, GC_OB:GC_OB + 4] = f(inp["out_b_g"])[0].reshape(4, 128).T
    g[:, GC_EPS] = EPS
    w_qb = f(inp["w_qb"])[0]
    wq_cols = []
    for h in range(8):
        b = h * 96
        wq_cols += [w_qb[:, b:b + 64], w_qb[:, b + 64:b + 96], w_qb[:, b + 64:b + 96][:, sw]]
    w_kvb = f(inp["w_kvb"])[0]
    kn = np.concatenate([w_kvb[:, h * 128:h * 128 + 64] for h in range(8)], 1)
    vv = np.concatenate([w_kvb[:, h * 128 + 64:h * 128 + 128] for h in range(8)], 1)
    w_o = f(inp["w_o"])[0]
    wo_p = np.concatenate([w_o[0:512][qperm], w_o[512:1024]], 0)
    shared = {
        "w_aug": np.ascontiguousarray(w_aug), "gcols": g,
        "relb_rep": np.ascontiguousarray(np.concatenate([np.repeat(f(inp["rel_bias"])[:, :, None], 128, axis=2), np.zeros((96, 8, 128), np.float32)], 0)), "a_sink": f(inp["a_sink"]),
        "wqb_aug": np.ascontiguousarray(np.concatenate(wq_cols, 1)),
        "wkvb_aug": np.ascontiguousarray(np.concatenate([kn, vv], 1)),
        "wo_p": np.ascontiguousarray(wo_p), "ln2_g": f(inp["ln2_g"]),
        "w_router": np.ascontiguousarray(np.concatenate([f(inp["w_router"])[0], np.zeros((D, 112), np.float32)], 1)),
        "w_gate": f(inp["w_gate"])[0], "w_up": f(inp["w_up"])[0], "w_down": f(inp["w_down"])[0],
    }
    shared.update(host_constants())
    return shared


_CACHE = {}


def kernel(**inputs):
    x = np.asarray(inputs["x"], dtype=np.float32)
    shared = host_layout(inputs)
    if "nc" not in _CACHE:
        _CACHE["nc"] = build_program()[0]
    nc = _CACHE["nc"]
    in_maps = []
    for b in range(8):
        m = dict(shared)
        m["x"] = np.ascontiguousarray(x[b])
        m["xT"] = np.ascontiguousarray(x[b].T)
        in_maps.append(m)
    res = run_bass_kernel_spmd(nc, in_maps, core_ids=list(range(8)))
    return np.stack([np.asarray(r["out"], dtype=np.float32) for r in res.results], 0)
```

```python
import math
import os
from contextlib import ExitStack

import numpy as np
import ml_dtypes

import concourse.bass as bass
import concourse.mybir as mybir
from concourse.bass_utils import run_bass_kernel_spmd

F32 = mybir.dt.float32
BF16 = mybir.dt.bfloat16
F16 = mybir.dt.float16
I32 = mybir.dt.int32
ALU = mybir.AluOpType
AF = mybir.ActivationFunctionType

S = 4096
D = 1024
NT = 32
NG = 8
EPS = 1e-6
NE = 16
CAP = 512
SB_BASE = 16512
SB_END = 229376

GC_Q2, GC_K2, GC_CQ, GC_CKV, GC_KR, GC_QB, GC_KN, GC_LN1, GC_OA, GC_OB, GC_EPS = 0, 1, 2, 5, 7, 8, 9, 10, 18, 22, 26
NGC = 28


class Op:
    __slots__ = ("eng", "fn", "deps", "signal", "sem", "val", "is_dma", "key", "idx")

    def __init__(self, eng, fn, deps, is_dma, key):
        self.eng, self.fn, self.deps, self.is_dma, self.key = eng, fn, deps, is_dma, key
        self.signal = False
        self.sem = None
        self.val = 0


class Prog:
    ENGS = ("pe", "act", "dve", "pool", "sp")
    LIMIT = 30000

    def __init__(self, nc, sems):
        self.nc = nc
        self.free_sems = list(sems)
        self.ops = {e: [] for e in self.ENGS}
        self.lastw = {}
        self.readers = {}
        self.eng_sem = {e: None for e in self.ENGS}
        self.eng_cnt = {e: 0 for e in self.ENGS}
        self.dma_sem = {}
        self.dma_cnt = {}
        self.waited = {e: {} for e in self.ENGS}
        self.phase_dma = []

    def op(self, eng, fn, reads=(), writes=(), dma=False, key=None, deps=()):
        d = []
        for r in reads:
            w = self.lastw.get(r)
            if w is not None:
                d.append(w)
        for w_ in writes:
            w = self.lastw.get(w_)
            if w is not None:
                d.append(w)
            d.extend(self.readers.get(w_, ()))
        d.extend(deps)
        self.nops = getattr(self, "nops", 0) + 1
        if self.nops > int(os.environ.get("P_MAXOPS", 10 ** 9)) and fn is not None:
            return Op(eng, None, [], False, None)
        o = Op(eng, fn, d, dma, key)
        for r in reads:
            self.readers.setdefault(r, []).append(o)
        for w_ in writes:
            self.lastw[w_] = o
            self.readers[w_] = []
        self.ops[eng].append(o)
        if dma:
            if key not in self.dma_sem:
                self.dma_sem[key] = self.free_sems.pop()
                self.dma_cnt[key] = 0
            self.dma_cnt[key] += 16
            o.sem, o.val = self.dma_sem[key], self.dma_cnt[key]
            self.phase_dma.append(o)
        return o

    def flush(self, blk):
        if self.phase_dma:
            last = {}
            for o in self.phase_dma:
                last[o.sem] = o
            self.op("sp", None, deps=list(last.values()))
        for e in self.ENGS:
            for o in self.ops[e]:
                for dd in o.deps:
                    if dd.is_dma:
                        continue
                    if dd.eng == o.eng and o.eng == "pe":
                        continue
                    dd.signal = True
        for e in self.ENGS:
            for o in self.ops[e]:
                if o.is_dma or not o.signal:
                    continue
                if self.eng_sem[e] is None or self.eng_cnt[e] >= self.LIMIT:
                    self.eng_sem[e] = self.free_sems.pop()
                    self.eng_cnt[e] = 0
                self.eng_cnt[e] += 1
                o.sem, o.val = self.eng_sem[e], self.eng_cnt[e]
        starters = {"pe": blk.tensor, "act": blk.scalar, "dve": blk.vector, "pool": blk.gpsimd, "sp": blk.sync}
        for e in self.ENGS:
            ops = self.ops[e]
            if not ops:
                continue

            def body(h, ops=ops, e=e):
                waited = self.waited[e]
                for o in ops:
                    need = {}
                    for dd in o.deps:
                        if not dd.is_dma and dd.eng == e and e == "pe":
                            continue
                        if dd.sem is None:
                            continue
                        if need.get(dd.sem, (0, None))[0] < dd.val:
                            need[dd.sem] = (dd.val, dd.sem)
                    for sem, (val, _) in need.items():
                        if waited.get(sem, 0) < val:
                            h.wait_ge(sem, val)
                            waited[sem] = val
                    if o.fn is None:
                        continue
                    ins = o.fn(h)
                    if o.is_dma:
                        ins.then_inc(o.sem, 16)
                    elif o.signal:
                        ins.then_inc(o.sem, 1)

            starters[e](body)
        self.ops = {e: [] for e in self.ENGS}
        self.lastw = {}
        self.readers = {}
        self.phase_dma = []


class Arena:
    def __init__(self, nc):
        self.nc = nc
        self.n = 0
        self.lo = SB_BASE

    def at(self, name, shape, dtype, off):
        self.n += 1
        return self.nc.alloc_sbuf_tensor_at(f"{name}_{self.n}", list(shape), dtype, offset=off)

    def tmp(self, name, shape, dtype, limit):
        nbytes = int(np.prod(shape[1:])) * mybir.dt.size(dtype)
        nbytes = (nbytes + 31) // 32 * 32
        off = self.lo
        self.lo += nbytes
        assert self.lo <= limit, f"SBUF overflow allocating {name}: {self.lo} > {limit}"
        return self.at(name, shape, dtype, off)


def KB(x):
    return SB_BASE + int(x * 1024)


def build_program(debug=(), stop_after=99):
    nc = bass.Bass("TRN2", target_bir_lowering=False)
    dbg = {}

    def din(name, shape, dt=F32):
        return nc.dram_tensor(name, list(shape), dt, kind="ExternalInput").ap()

    xT = din("xT", [D, S])
    x = din("x", [S, D])
    w_aug = din("w_aug", [D, 1664])
    gcols_d = din("gcols", [128, NGC])
    cmat_d = din("cmat", [128, 6, 128])
    csk_d = din("csk", [128, S])
    csq_d = din("csq", [128, S])
    ohrel_d = din("ohrel", [128, 640])
    maskadd_d = din("maskadd", [128, 640])
    relb_d = din("relb_rep", [128, 8, 128])
    sink_d = din("a_sink", [1, 8])
    wqb_d = din("wqb_aug", [384, 1024])
    wkvb_d = din("wkvb_aug", [256, 1024])
    wo_d = din("wo_p", [D, D])
    ln2_d = din("ln2_g", [1, D])
    wr_d = din("w_router", [D, 128])
    wg_d = din("w_gate", [NE, D, D])
    wu_d = din("w_up", [NE, D, D])
    wd_d = din("w_down", [NE, D, D])
    ccol_d = din("ccol", [128, 4])
    out = nc.dram_tensor("out", [S, D], F32, kind="ExternalOutput").ap()
    trep = nc.dram_tensor("trep", [8, 128, 640], F32, kind="ExternalOutput")
    h2_dram = nc.dram_tensor("h2_dram", [S, D], BF16, kind="ExternalOutput").ap()
    aff_dram = nc.dram_tensor("aff_dram", [S, NE], F32, kind="ExternalOutput").ap()
    cum_dram = nc.dram_tensor("cum_dram", [NE, S], F16, kind="ExternalOutput").ap()

    def dump(P, name, t, eng="sp"):
        if name not in debug:
            return
        shp = list(t.shape)
        d = nc.dram_tensor("dbg_" + name, shp, t.dtype, kind="ExternalOutput").ap()
        dbg[name] = d
        lastops = [P.ops[en][-1] for en in ("pe", "act", "dve", "pool") if P.ops[en]]
        lo_p = 64 if name.startswith("KT") else 0
        P.op(eng, lambda e: e.dma_start(out=d[lo_p:], in_=t[lo_p:]), deps=lastops + list(P.phase_dma), dma=True, key="dbg_" + name)

    with ExitStack() as es:
        sems = [es.enter_context(nc.semaphore(f"s{i}")) for i in range(100)]
        P = Prog(nc, sems)
        A = Arena(nc)
        psum = [es.enter_context(nc.psum_tensor(f"ps{i}", [128, 512], F32)) for i in range(8)]

        cmat = A.at("cmat", [128, 6, 128], BF16, KB(0))
        identf = A.at("identf", [128, 128], F32, KB(1.5))
        gcols = A.at("gcols", [128, NGC], F32, KB(2))
        esink = A.at("esink", [128, 8], F32, KB(2.25))
        ccol = A.at("ccol", [128, 4], F32, KB(2.5))
        CONST_END = 4
        ident_bf = cmat[:, 0, :]
        ones_bf = cmat[:, 1, :]
        bd64 = cmat[:, 2, :]
        bdq = cmat[:, 3, :]
        bdk32 = cmat[:, 4, :]
        fold = cmat[:, 5, :]

        R_C = CONST_END
        R_A = R_C + 56
        R_OA = R_A + 56
        cqn = A.at("cqn", [128, 3, S], BF16, KB(R_C))
        ckvn = A.at("ckvn", [128, 2, S], BF16, KB(R_C + 24))
        KT = [A.at(f"KT{i}", [128, S], BF16, KB(R_C + 40 + 8 * i)) for i in range(2)]
        qA = A.at("qA", [128, 4, S], BF16, KB(R_A))
        kA = A.at("kA", [128, S], BF16, KB(R_A + 32))
        VA = A.at("VA", [128, NT, 2, 128], BF16, KB(R_A + 40))
        oA = A.at("oA", [128, 4, S], BF16, KB(R_OA))

        if stop_after >= 1:
            A.lo = KB(R_OA)
            LIM = SB_END
            Wbf = A.tmp("Wbf", [128, 8, 1664], BF16, LIM)
            Wst = A.tmp("Wst", [128, 1664], F32, LIM)
            xst = [A.tmp(f"xst{i}", [128, 512], F32, LIM) for i in range(3)]
            xbf = [A.tmp(f"xbf{i}", [128, 8, 512], BF16, LIM) for i in range(2)]
            xsq = A.tmp("xsq", [128, 8, 512], BF16, LIM)
            zt = [A.tmp(f"zt{i}", [128, 512], F32, LIM) for i in range(4)]
            sqb = [A.tmp(f"sqb{i}", [128, 512], BF16, LIM) for i in range(3)]
            rs = [A.tmp(f"rs{i}", [128, 512], F32, LIM) for i in range(2)]
            rinv = [A.tmp(f"rinv{i}", [128, 512], F32, LIM) for i in range(2)]
            rstd_bc = [A.tmp(f"rstdbc{i}", [128, 512], F32, LIM) for i in range(2)]
            rsc = A.tmp("rsc", [128, 4], F32, LIM)
            rstd_col = [A.tmp(f"rstdcol{i}", [128, 4], F32, LIM) for i in range(2)]
            wk = A.tmp("wk", [128, 512], BF16, LIM)
            csk = [A.tmp(f"csk{i}", [128, 512], F32, LIM) for i in range(2)]
            sinkt = A.tmp("sinkt", [128, 8], F32, LIM)

            with nc.Block() as blk:
                P.op("sp", lambda e: e.dma_start(out=Wst[:, 0:768], in_=cmat_d.rearrange("p a b -> p (a b)")), writes=["Wst"], dma=True, key="wst")
                P.op("sp", lambda e: e.dma_start(out=identf[:], in_=cmat_d[:, 0, :]), writes=["identf"], dma=True, key="c11")
                P.op("sp", lambda e: e.dma_start(out=gcols[:], in_=gcols_d), writes=["gcols"], dma=True, key="c12")
                P.op("sp", lambda e: e.dma_start(out=ccol[:], in_=ccol_d), writes=["ccol"], dma=True, key="c13")
                P.op("sp", lambda e: e.dma_start(out=sinkt[:], in_=sink_d.partition_broadcast(128)), writes=["sinkt"], dma=True, key="c14")
                P.op("dve", lambda e: e.tensor_copy(out=cmat[:].rearrange("p a b -> p (a b)"), in_=Wst[:, 0:768]), reads=["Wst"], writes=["cmat"])
                P.op("act", lambda e: e.activation(out=esink[:], in_=sinkt[:], func=AF.Exp), reads=["sinkt"], writes=["esink"])
                P.op("pool", lambda e: e.memset(VA[:, :, 0, 64:128], 1.0), writes=["VAones0"])
                P.op("pool", lambda e: e.memset(VA[:, :, 1, 0:64], 1.0), writes=["VAones1"])
                for k in range(8):
                    P.op("sp", lambda e, k=k: e.dma_start(out=Wst[:], in_=w_aug[k * 128:(k + 1) * 128, :]),
                         writes=["Wst"], dma=True, key="wst")
                    P.op("dve", lambda e, k=k: e.tensor_scalar(out=Wbf[:, k, :], in0=Wst[:], scalar1=gcols[:, GC_LN1 + k:GC_LN1 + k + 1],
                                                               scalar2=None, op0=ALU.mult),
                         reads=["Wst", "gcols"], writes=[("Wbf", k)])
                WB = [("Wbf", k) for k in range(8)]

                def stage(G):
                    xb = xbf[G % 2]
                    P.op("sp", lambda e: e.dma_start(out=csk[G % 2][:], in_=csk_d[:, G * 512:(G + 1) * 512]),
                         writes=[("csk", G % 2)], dma=True, key=f"csk{G % 2}")
                    for k in range(8):
                        r = k % 3
                        P.op("sp", lambda e, k=k, r=r: e.dma_start(out=xst[r][:], in_=xT[k * 128:(k + 1) * 128, G * 512:(G + 1) * 512]),
                             writes=[("xst", r)], dma=True, key=f"xst{r}")
                        P.op("act", lambda e, k=k, r=r: e.activation(out=xsq[:, k, :], in_=xst[r][:], func=AF.Square),
                             reads=[("xst", r)], writes=[("xsq", k)])
                        P.op("dve", lambda e, k=k, r=r: e.tensor_copy(out=xb[:, k, :], in_=xst[r][:]),
                             reads=[("xst", r)], writes=[("xbf", G % 2, k)])

                def stats(G):
                    ss = psum[0]
                    for k in range(8):
                        P.op("pe", lambda e, k=k: e.matmul(ss[:], lhsT=ones_bf, rhs=xsq[:, k, :], start=(k == 0), stop=(k == 7)),
                             reads=[("xsq", k), "cmat"], writes=["ps0"])
                    P.op("act", lambda e: e.activation(out=rs[0][:], in_=ss[:], func=AF.Ln, scale=1.0 / D, bias=gcols[:, GC_EPS:GC_EPS + 1]),
                         reads=["ps0", "gcols"], writes=["rs0"])
                    P.op("act", lambda e: e.activation(out=rstd_bc[G % 2][:], in_=rs[0][:], func=AF.Exp, scale=-0.5), reads=["rs0"], writes=[("rstdbc", G % 2)])
                    sc = psum[1]
                    for t in range(4):
                        for k in range(8):
                            P.op("pe", lambda e, k=k, t=t: e.matmul(sc[:, t * 128:(t + 1) * 128], lhsT=xsq[:, k, t * 128:(t + 1) * 128], rhs=ones_bf,
                                                                   start=(k == 0), stop=(k == 7)),
                                 reads=[("xsq", k), "cmat"], writes=["ps1"])
                    P.op("act", lambda e: e.activation(out=rsc[:], in_=sc[:, 0:512:128], func=AF.Ln, scale=1.0 / D, bias=gcols[:, GC_EPS:GC_EPS + 1]),
                         reads=["ps1", "gcols"], writes=["rsc"])
                    P.op("act", lambda e: e.activation(out=rstd_col[G % 2][:], in_=rsc[:], func=AF.Exp, scale=-0.5), reads=["rsc"], writes=[("rstdcol", G % 2)])

                zring = [0]

                def proj(G, c):
                    slot = zring[0] % 4
                    zring[0] += 1
                    pb = 2 + (slot % 3)
                    zp = psum[pb]
                    xb = xbf[G % 2]
                    for k in range(8):
                        P.op("pe", lambda e, k=k: e.matmul(zp[:], lhsT=Wbf[:, k, c * 128:(c + 1) * 128], rhs=xb[:, k, :], start=(k == 0), stop=(k == 7)),
                             reads=[("Wbf", k), ("xbf", G % 2, k)], writes=[f"ps{pb}"])
                    P.op("dve", lambda e: e.tensor_tensor(out=zt[slot][:], in0=zp[:], in1=rstd_bc[G % 2][:], op=ALU.mult),
                         reads=[f"ps{pb}", ("rstdbc", G % 2)], writes=[("zt", slot)])
                    return slot

                nring = [0]

                def norm_rinv(slots, bd, nrows, scale):
                    j = nring[0] % 2
                    nring[0] += 1
                    msp = psum[5 + j]
                    for i, sl in enumerate(slots):
                        sb_ = sqb[(nring[0] * 3 + i) % 3] if False else sqb[i]
                        P.op("act", lambda e, sl=sl, sb_=sb_: e.activation(out=sb_[0:nrows, :], in_=zt[sl][0:nrows, :], func=AF.Square),
                             reads=[("zt", sl)], writes=[("sqb", i)])
                        P.op("pe", lambda e, sb_=sb_, i=i: e.matmul(msp[0:nrows, :], lhsT=bd[0:nrows, 0:nrows], rhs=sb_[0:nrows, :],
                                                                   start=(i == 0), stop=(i == len(slots) - 1)),
                             reads=[("sqb", i), "cmat"], writes=[f"ps{5 + j}"])
                    P.op("act", lambda e: e.activation(out=rs[1][0:nrows, :], in_=msp[0:nrows, :], func=AF.Ln, scale=scale, bias=gcols[0:nrows, GC_EPS:GC_EPS + 1]),
                         reads=[f"ps{5 + j}", "gcols"], writes=["rs1"])
                    P.op("act", lambda e: e.activation(out=rinv[j][0:nrows, :], in_=rs[1][0:nrows, :], func=AF.Exp, scale=-0.5), reads=["rs1"], writes=[("rinv", j)])
                    return j

                def finish(slot, j, gc, dst, dkey, nrows=128):
                    P.op("dve", lambda e: e.scalar_tensor_tensor(out=dst, in0=zt[slot][0:nrows, :], scalar=gcols[0:nrows, gc:gc + 1],
                                                                 in1=rinv[j][0:nrows, :], op0=ALU.mult, op1=ALU.mult),
                         reads=[("zt", slot), ("rinv", j), "gcols"], writes=[dkey])

                def group(G):
                    gs = slice(G * 512, (G + 1) * 512)
                    stats(G)

                    def fin_q(c):
                        return lambda sls, j: finish(sls[0], j, GC_Q2, qA[:, c, gs], ("qA", c, G))

                    def fin_k(sls, j):
                        finish(sls[0], j, GC_K2, kA[:, gs], ("kA", G))

                    def fin_cq(sls, j):
                        for i in range(3):
                            finish(sls[i], j, GC_CQ + i, cqn[:, i, gs], ("cqn", i, G))

                    def fin_ckv(sls, j):
                        for i in range(2):
                            finish(sls[i], j, GC_CKV + i, ckvn[:, i, gs], ("ckvn", i, G))

                    def fin_kr(sls, j):
                        sl = sls[0]
                        finish(sl, j, GC_KR, zt[sl][:, :], ("zt", sl))
                        P.op("dve", lambda e: e.tensor_tensor(out=wk[:], in0=zt[sl][:, :], in1=csk[G % 2][:], op=ALU.mult),
                             reads=[("zt", sl), ("csk", G % 2)], writes=["wk"])
                        kp = psum[7]
                        P.op("pe", lambda e: e.matmul(kp[:], lhsT=fold, rhs=wk[:], start=True, stop=True),
                             reads=["wk", "cmat"], writes=["ps7"])
                        P.op("dve", lambda e: e.tensor_copy(out=KT[0][64:128, gs], in_=kp[64:128, :]), reads=["ps7"], writes=[("KT0r", G)])
                        P.op("dve", lambda e: e.tensor_copy(out=KT[1][64:128, gs], in_=kp[64:128, :]), reads=["ps7"], writes=[("KT1r", G)])

                    descs = [([c], bd64, 1.0, fin_q(c)) for c in range(4)]
                    descs.append(([4], bd64, 1.0, fin_k))
                    descs.append(([5, 6, 7], ones_bf, 1.0 / 384, fin_cq))
                    descs.append(([10], bdk32, 1.0, fin_kr))
                    descs.append(([8, 9], ones_bf, 1.0 / 256, fin_ckv))
                    pending = None
                    for di, (chs, bd_, sc_, fin_) in enumerate(descs):
                        sls = [proj(G, c) for c in chs]
                        if pending is not None:
                            p_sls, p_bd, p_sc, p_fin = pending
                            p_fin(p_sls, norm_rinv(p_sls, p_bd, 128, p_sc))
                        pending = (sls, bd_, sc_, fin_)
                        if di == 4 and G + 1 < int(os.environ.get("P1_GROUPS", NG)):
                            stage(G + 1)
                    p_sls, p_bd, p_sc, p_fin = pending
                    p_fin(p_sls, norm_rinv(p_sls, p_bd, 128, p_sc))
                    for t in range(4):
                        tile_i = G * 4 + t
                        vp = psum[7]
                        xb = xbf[G % 2]
                        for k in range(8):
                            P.op("pe", lambda e, k=k, t=t: e.matmul(vp[:, 0:128], lhsT=xb[:, k, t * 128:(t + 1) * 128], rhs=Wbf[:, k, 1536:1664],
                                                                   start=(k == 0), stop=(k == 7)),
                                 reads=[("Wbf", k), ("xbf", G % 2, k)], writes=["ps7"])
                        P.op("dve", lambda e, t=t, tile_i=tile_i: e.tensor_scalar(out=VA[:, tile_i, 0, 0:64], in0=vp[:, 0:64], scalar1=rstd_col[G % 2][:, t:t + 1],
                                                                                 scalar2=None, op0=ALU.mult),
                             reads=["ps7", ("rstdcol", G % 2)], writes=[("VA0", tile_i)])
                        P.op("dve", lambda e, t=t, tile_i=tile_i: e.tensor_scalar(out=VA[:, tile_i, 1, 64:128], in0=vp[:, 64:128], scalar1=rstd_col[G % 2][:, t:t + 1],
                                                                                 scalar2=None, op0=ALU.mult),
                             reads=["ps7", ("rstdcol", G % 2)], writes=[("VA1", tile_i)])

                NGRP = int(os.environ.get("P1_GROUPS", NG))
                if NGRP > 0:
                    stage(0)
                for G in range(NGRP):
                    group(G)
                for nm, t in (("qA", qA), ("kA", kA), ("VA", VA), ("cqn", cqn), ("ckvn", ckvn), ("KT0", KT[0])):
                    dump(P, nm, t)
                P.flush(blk)


        if stop_after >= 2:
            A.lo = KB(R_OA + 32)
            LIM = SB_END
            kAm = [A.tmp(f"kAm{i}", [128, S], BF16, LIM) for i in range(2)]
            relrep = A.tmp("relrep", [128, 8, 128], F32, LIM)
            ohp = A.tmp("ohp", [128, 640], F32, LIM)
            mka = A.tmp("mka", [128, 640], F32, LIM)
            rep = A.tmp("rep", [128, 640], F32, LIM)
            BTf = A.tmp("BTf", [128, 384], F32, LIM)
            BTb = [A.tmp(f"BTb{i}", [128, 384], BF16, LIM) for i in range(8)]
            pTa = [A.tmp(f"pTa{i}", [128, 384], BF16, LIM) for i in range(3)]
            NB = 8
            raw = [A.tmp(f"raw{i}", [128, NB, 256], F32, LIM) for i in range(2)]
            dsh2 = A.tmp("dsh2", [128, NB, 128], F32, LIM)
            with nc.Block() as blk:
                P.op("sp", lambda e: e.dma_start(out=relrep[:], in_=relb_d), writes=["relrep"], dma=True, key="p2a")
                P.op("sp", lambda e: e.dma_start(out=ohp[:], in_=ohrel_d), writes=["ohp"], dma=True, key="p2b")
                P.op("sp", lambda e: e.dma_start(out=mka[:], in_=maskadd_d), writes=["mka"], dma=True, key="p2c")
                P.op("pool", lambda e: e.memset(kAm[0][:], 0.0), writes=["kAm0"])
                P.op("pool", lambda e: e.memset(kAm[1][:], 0.0), writes=["kAm1"])
                P.op("dve", lambda e: e.tensor_copy(out=kAm[0][0:64, :], in_=kA[0:64, :]), reads=["kAm0"], writes=["kAm0"])
                P.op("dve", lambda e: e.tensor_copy(out=kAm[1][64:128, :], in_=kA[64:128, :]), reads=["kAm1"], writes=["kAm1"])
                for h in range(8):
                    for hf in range(2):
                        P.op("pe", lambda e, h=h, hf=hf: e.matmul(psum[hf][:, 0:320], lhsT=relrep[:, h, :], rhs=ohp[:, hf * 320:(hf + 1) * 320], start=True, stop=True),
                             reads=["relrep", "ohp"], writes=[f"ps{hf}"])
                        P.op("dve", lambda e, hf=hf: e.tensor_tensor(out=rep[:, hf * 320:(hf + 1) * 320], in0=psum[hf][:, 0:320], in1=mka[:, hf * 320:(hf + 1) * 320], op=ALU.add),
                             reads=[f"ps{hf}", "mka"], writes=[("rep", hf)])
                    P.op("sp", lambda e, h=h: e.dma_start(out=trep.ap()[h], in_=rep[:]), reads=[("rep", 0), ("rep", 1)], writes=[("trep", h)], dma=True, key="trepw")
                    P.op("sp", lambda e, h=h: e.dma_start(out=BTf[:], in_=bass.AP(tensor=trep, offset=h * 128 * 640 + 127, ap=[[639, 128], [1, 384]])),
                         reads=[("trep", h)], writes=["BTf"], dma=True, key="btf")
                    P.op("dve", lambda e, h=h: e.tensor_scalar(out=BTb[h][:], in0=BTf[:], scalar1=8.0, scalar2=None, op0=ALU.mult), reads=["BTf"], writes=[("BTb", h)])
                it = [0]
                for c in range(4):
                    items = [(n, hh) for n in range(NT) for hh in range(2)]
                    rmap = {}

                    def qk(n, hh, c=c):
                        head = c + 4 * hh
                        r = it[0] % 3
                        it[0] += 1
                        rmap[(n, hh)] = r
                        spb = psum[r]
                        ms_ = [(yi, m) for yi, m in enumerate((n + 1, n, n - 1)) if 0 <= m < NT]
                        y0, y1 = ms_[0][0] * 128, ms_[-1][0] * 128 + 128
                        P.op("pe", lambda e: e.matmul(spb[:, y0:y1], lhsT=ident_bf, rhs=BTb[head][:, y0:y1], start=True, stop=False),
                             reads=[("BTb", head)], writes=[f"ps{r}"])
                        for i_, (yi, m) in enumerate(ms_):
                            P.op("pe", lambda e, yi=yi, m=m, i_=i_, L=len(ms_): e.matmul(spb[:, yi * 128:(yi + 1) * 128], lhsT=kAm[hh][:, m * 128:(m + 1) * 128],
                                                                                         rhs=qA[:, c, n * 128:(n + 1) * 128], start=False, stop=(i_ == L - 1)),
                                 reads=[f"kAm{hh}"], writes=[f"ps{r}"])
                        P.op("act", lambda e: e.activation(out=pTa[r][:, y0:y1], in_=spb[:, y0:y1], func=AF.Exp, scale=0.125),
                             reads=[f"ps{r}"], writes=[("pTa", r)])

                    def pv(n, hh, c=c):
                        r = rmap[(n, hh)]
                        ab = 3 + n % 2
                        acc = psum[ab]
                        ms_ = [(yi, m) for yi, m in enumerate((n + 1, n, n - 1)) if 0 <= m < NT]
                        for i_, (yi, m) in enumerate(ms_):
                            P.op("pe", lambda e, yi=yi, m=m, i_=i_, L=len(ms_): e.matmul(acc[:, hh * 128:(hh + 1) * 128], lhsT=VA[:, m, hh, :], rhs=pTa[r][:, yi * 128:(yi + 1) * 128],
                                                                                         start=(i_ == 0), stop=(i_ == L - 1)),
                                 reads=[("pTa", r)], writes=[f"ps{ab}"])
                        if hh == 1:
                            nb_, ni = n // NB, n % NB
                            bi = (c * (NT // NB) + nb_) % 2
                            rw, rk = raw[bi], ("raw", bi)
                            P.op("dve", lambda e: e.tensor_copy(out=rw[:, ni, :], in_=acc[:, 0:256]), reads=[f"ps{ab}"], writes=[rk])
                            if ni == NB - 1:
                                ns = slice(nb_ * NB * 128, (nb_ + 1) * NB * 128)
                                P.op("pool", lambda e: e.tensor_copy(out=dsh2[0:64, :, :], in_=rw[64:128, :, 0:128]), reads=[rk], writes=["dsh2a"])
                                P.op("dve", lambda e: e.tensor_copy(out=dsh2[64:128, :, :], in_=rw[0:64, :, 128:256]), reads=[rk], writes=["dsh2b"])
                                P.op("dve", lambda e: e.tensor_scalar(out=dsh2[0:64, :, :], in0=dsh2[0:64, :, :], scalar1=esink[0:64, c:c + 1], scalar2=None, op0=ALU.add), reads=["dsh2a"], writes=["dsh2a"])
                                P.op("dve", lambda e: e.tensor_scalar(out=dsh2[64:128, :, :], in0=dsh2[64:128, :, :], scalar1=esink[64:128, c + 4:c + 5], scalar2=None, op0=ALU.add), reads=["dsh2b"], writes=["dsh2b"])
                                P.op("act", lambda e: e.activation(out=dsh2[:], in_=dsh2[:], func=AF.Ln), reads=["dsh2a", "dsh2b"], writes=["dsh2a", "dsh2b"])
                                P.op("act", lambda e: e.activation(out=dsh2[:], in_=dsh2[:], func=AF.Exp, scale=-1.0), reads=["dsh2a", "dsh2b"], writes=["dsh2a", "dsh2b"])
                                P.op("dve", lambda e: e.tensor_tensor(out=oA[0:64, c, ns].rearrange("p (a b) -> p a b", a=NB), in0=rw[0:64, :, 0:128], in1=dsh2[0:64, :, :], op=ALU.mult),
                                     reads=[rk, "dsh2a"], writes=[("oA", c, nb_, 0)])
                                P.op("dve", lambda e: e.tensor_tensor(out=oA[64:128, c, ns].rearrange("p (a b) -> p a b", a=NB), in0=rw[64:128, :, 128:256], in1=dsh2[64:128, :, :], op=ALU.mult),
                                     reads=[rk, "dsh2b"], writes=[("oA", c, nb_, 1)])

                    qk(*items[0])
                    for i in range(len(items)):
                        if i + 1 < len(items):
                            qk(*items[i + 1])
                        pv(*items[i])
                dump(P, "oA", oA)
                P.flush(blk)

        R_OB = R_OA + 32
        oB = A.at("oB", [128, 4, S], BF16, KB(R_OB))
        if stop_after >= 3:
            A.lo = KB(R_OB + 32)
            A2 = Arena(nc)
            A2.n = 5000
            A2.lo = KB(R_A)
            LIM2 = KB(R_OA)
            LIM = SB_END
            QT = [A2.tmp(f"QT{i}", [128, S], BF16, LIM2) for i in range(2)]
            VO = [A2.tmp(f"VO{i}", [128, NT, 128], BF16, LIM2) for i in range(2)]
            CSG = A2.tmp("CSG", [128, S], F32, LIM2)
            Wqb = A2.tmp("Wqb", [128, 3, 1024], BF16, LIM2)
            Wkv = A.tmp("Wkv", [128, 2, 1024], BF16, SB_END)
            pTb = [A.tmp(f"pTb{i}", [128, 512], BF16, LIM) for i in range(4)]
            sq3 = [A.tmp(f"sq3{i}", [128, 512], BF16, LIM) for i in range(3)]
            ri3 = [A.tmp(f"ri3{i}", [128, 512], F32, LIM) for i in range(3)]
            u3 = [A.tmp(f"u3{i}", [128, 512], F32, LIM) for i in range(2)]
            kt3 = A.tmp("kt3", [128, 512], BF16, LIM)
            dshb = [A.tmp(f"dshb{i}", [128, 512], F32, LIM) for i in range(2)]
            SCALE_B = 96.0 ** -0.5
            with nc.Block() as blk:
                P.op("sp", lambda e: e.dma_start(out=CSG[:], in_=csq_d), writes=["CSG"], dma=True, key="p3a")
                P.op("pool", lambda e: e.dma_start(out=Wqb[:], in_=wqb_d.rearrange("(k p) f -> p k f", p=128)), writes=["Wqb"], dma=True, key="p3b")
                P.op("pool", lambda e: e.dma_start(out=Wkv[:], in_=wkvb_d.rearrange("(k p) f -> p k f", p=128)), writes=["Wkv"], dma=True, key="p3c")
                P.op("dve", lambda e: e.tensor_scalar(out=CSG[:], in0=CSG[:], scalar1=gcols[:, GC_QB:GC_QB + 1], scalar2=None, op0=ALU.mult), reads=["CSG"], writes=["CSG"])
                P.op("pool", lambda e: e.memset(VO[0][:, :, 64:128], 1.0), writes=["VO0ones"])
                P.op("pool", lambda e: e.memset(VO[1][:, :, 0:64], 1.0), writes=["VO1ones"])
                for hp in range(4):
                    for G in range(NG):
                        gs = slice(G * 512, (G + 1) * 512)
                        kp, qp0, qp1, vp = psum[5], psum[0], psum[1], psum[7]
                        for i in range(2):
                            P.op("pe", lambda e, i=i, gs=gs, hp=hp, kp=kp: e.matmul(kp[:], lhsT=Wkv[:, i, hp * 128:(hp + 1) * 128], rhs=ckvn[:, i, gs], start=(i == 0), stop=(i == 1)),
                                 reads=["Wkv"], writes=["ps5"])
                        for hh, qp in ((0, qp0), (1, qp1)):
                            h = 2 * hp + hh
                            for i in range(3):
                                P.op("pe", lambda e, i=i, gs=gs, h=h, qp=qp: e.matmul(qp[:], lhsT=Wqb[:, i, h * 128:(h + 1) * 128], rhs=cqn[:, i, gs], start=(i == 0), stop=(i == 2)),
                                     reads=["Wqb"], writes=[f"ps{hh}"])
                        for t in range(4):
                            ti = G * 4 + t
                            for i in range(2):
                                P.op("pe", lambda e, i=i, ti=ti, t=t, hp=hp, vp=vp: e.matmul(vp[:, t * 128:(t + 1) * 128], lhsT=ckvn[:, i, ti * 128:(ti + 1) * 128], rhs=Wkv[:, i, 512 + hp * 128:512 + (hp + 1) * 128],
                                                                               start=(i == 0), stop=(i == 1)),
                                     reads=["Wkv"], writes=["ps7"])
                        srcs = ((kp, "ps5", bd64, psum[6], "ps6"), (qp0, "ps0", bdq, psum[2], "ps2"), (qp1, "ps1", bdq, psum[3], "ps3"))
                        for z_, (pp, pk, bd_, mp, mk) in enumerate(srcs):
                            P.op("act", lambda e, pp=pp, z_=z_: e.activation(out=sq3[z_][:], in_=pp[:], func=AF.Square), reads=[pk], writes=[("sq3", z_)])
                        for z_, (pp, pk, bd_, mp, mk) in enumerate(srcs):
                            P.op("pe", lambda e, bd_=bd_, mp=mp, z_=z_: e.matmul(mp[:], lhsT=bd_, rhs=sq3[z_][:], start=True, stop=True), reads=[("sq3", z_)], writes=[mk])
                        for z_, (pp, pk, bd_, mp, mk) in enumerate(srcs):
                            P.op("act", lambda e, mp=mp, z_=z_: e.activation(out=ri3[z_][:], in_=mp[:], func=AF.Ln, bias=gcols[:, GC_EPS:GC_EPS + 1]), reads=[mk], writes=[("ri3", z_)])
                            P.op("act", lambda e, z_=z_: e.activation(out=ri3[z_][:], in_=ri3[z_][:], func=AF.Exp, scale=-0.5), reads=[("ri3", z_)], writes=[("ri3", z_)])
                        vp3 = vp[:].rearrange("p (a b) -> p a b", a=4)
                        P.op("dve", lambda e, G=G, vp3=vp3: e.tensor_copy(out=VO[0][:, G * 4:(G + 1) * 4, 0:64], in_=vp3[:, :, 0:64]), reads=["ps7"], writes=[("VO0", G)])
                        P.op("dve", lambda e, G=G, vp3=vp3: e.tensor_copy(out=VO[1][:, G * 4:(G + 1) * 4, 64:128], in_=vp3[:, :, 64:128]), reads=["ps7"], writes=[("VO1", G)])
                        P.op("dve", lambda e, gs=gs, kp=kp: e.scalar_tensor_tensor(out=KT[0][0:64, gs], in0=kp[0:64, :], scalar=gcols[0:64, GC_KN:GC_KN + 1], in1=ri3[0][0:64, :], op0=ALU.mult, op1=ALU.mult),
                             reads=["ps5", ("ri3", 0)], writes=[("KT0n", G)])
                        P.op("dve", lambda e, kp=kp: e.scalar_tensor_tensor(out=kt3[64:128, :], in0=kp[64:128, :], scalar=gcols[64:128, GC_KN:GC_KN + 1], in1=ri3[0][64:128, :], op0=ALU.mult, op1=ALU.mult),
                             reads=["ps5", ("ri3", 0)], writes=["kt3"])
                        P.op("pool", lambda e, gs=gs: e.tensor_copy(out=KT[1][0:64, gs], in_=kt3[64:128, :]), reads=["kt3"], writes=[("KT1n", G)])
                        for hh, qp in ((0, qp0), (1, qp1)):
                            P.op("pool", lambda e, gs=gs, hh=hh: e.tensor_tensor(out=u3[hh][:], in0=ri3[1 + hh][:], in1=CSG[:, gs], op=ALU.mult), reads=[("ri3", 1 + hh), "CSG"], writes=[("u3", hh)])
                            P.op("dve", lambda e, gs=gs, hh=hh, qp=qp: e.tensor_tensor(out=QT[hh][:, gs], in0=qp[:], in1=u3[hh][:], op=ALU.mult), reads=[f"ps{hh}", ("u3", hh)], writes=[("QT", hh, G)])
                    for hh in range(2):
                        nr = slice(0, 64) if hh == 0 else slice(64, 128)
                        dr = slice(64, 128) if hh == 0 else slice(0, 64)
                        kdeps = [("KT0n" if hh == 0 else "KT1n", G) for G in range(NG)]
                        for qg in range(NG):
                            qs = slice(qg * 512, (qg + 1) * 512)
                            ab = 3 + qg % 2
                            acc = psum[ab]

                            def QK(kt, hh=hh, qs=qs, qg=qg):
                                b = kt % 3
                                P.op("pe", lambda e, b=b, kt=kt: e.matmul(psum[b][:], lhsT=KT[hh][:, kt * 128:(kt + 1) * 128], rhs=QT[hh][:, qs], start=True, stop=True),
                                     reads=[("KT0n" if hh == 0 else "KT1n", kt // 4), ("QT", hh, qg)], writes=[f"ps{b}"])
                                P.op("act", lambda e, b=b, kt=kt: e.activation(out=pTb[kt % 4][:], in_=psum[b][:], func=AF.Exp, scale=SCALE_B),
                                     reads=[f"ps{b}"], writes=[("pTb", kt % 4)])

                            def PV(kt, hh=hh, acc=acc, ab=ab):
                                P.op("pe", lambda e, kt=kt: e.matmul(acc[:], lhsT=VO[hh][:, kt, :], rhs=pTb[kt % 4][:], start=(kt == 0), stop=(kt == NT - 1)),
                                     reads=[("pTb", kt % 4), (f"VO{hh}", kt // 4), f"VO{hh}ones"], writes=[f"ps{ab}"])
                            QK(0)
                            QK(1)
                            for kt in range(NT):
                                if kt + 2 < NT:
                                    QK(kt + 2)
                                PV(kt)
                            d_ = dshb[qg % 2]
                            P.op("dve", lambda e, acc=acc, d_=d_, nr=nr, dr=dr: e.tensor_copy(out=d_[nr, :], in_=acc[dr, :]), reads=[f"ps{ab}"], writes=[("dshb", qg % 2)])
                            P.op("dve", lambda e, d_=d_, nr=nr: e.reciprocal(out=d_[nr, :], in_=d_[nr, :]), reads=[("dshb", qg % 2)], writes=[("dshb", qg % 2)])
                            P.op("dve", lambda e, acc=acc, d_=d_, nr=nr, qs=qs, hp=hp: e.tensor_tensor(out=oB[nr, hp, qs], in0=acc[nr, :], in1=d_[nr, :], op=ALU.mult),
                                 reads=[f"ps{ab}", ("dshb", qg % 2)], writes=[("oB", hp, qg, hh)])
                dump(P, "oB", oB)
                P.flush(blk)


        R_AF = R_C
        affT = A.at("affT", [128, S], F32, KB(R_AF))
        if stop_after >= 4:
            A.lo = KB(R_AF + 16)
            LIM = KB(R_OA)
            Wo = A.tmp("Wo", [128, 8, 1024], BF16, LIM)
            g2bc = A.tmp("g2bc", [128, 1024], F32, LIM)
            wr = A.tmp("wr", [128, 8, 128], F32, LIM)
            xt = [A.tmp(f"xt{i}", [128, 1024], F32, LIM) for i in range(2)]
            x1 = [A.tmp(f"x1{i}", [128, 1024], F32, LIM) for i in range(2)]
            h2f = [A.tmp(f"h2f{i}", [128, 1024], F32, LIM) for i in range(2)]
            h2b = [A.tmp(f"h2b{i}", [128, 1024], BF16, LIM) for i in range(2)]
            h2Tb_ = [A.tmp(f"h2T{i}", [128, 8, 128], F32, LIM) for i in range(2)]
            sqA = A.tmp("sqA", [128, 4, 128], BF16, LIM)
            sqB = A.tmp("sqB", [128, 4, 128], BF16, LIM)
            afp = [A.tmp(f"afp{i}", [128, 128], F32, LIM) for i in range(2)]
            sm = A.tmp("sm", [128, 16], F32, LIM)
            with nc.Block() as blk:
                P.op("pool", lambda e: e.dma_start(out=Wo[:], in_=wo_d.rearrange("(k p) f -> p k f", p=128)), writes=["Wo"], dma=True, key="p4a")
                P.op("sp", lambda e: e.dma_start(out=g2bc[:], in_=ln2_d.partition_broadcast(128)), writes=["g2bc"], dma=True, key="p4b")
                P.op("sp", lambda e: e.dma_start(out=wr[:], in_=wr_d.rearrange("(k p) f -> p k f", p=128)), writes=["wr"], dma=True, key="p4c")
                for c in range(8):
                    P.op("dve", lambda e, c=c: e.tensor_scalar(out=Wo[:, c, :], in0=Wo[:, c, :], scalar1=gcols[:, GC_OA + c:GC_OA + c + 1], scalar2=None, op0=ALU.mult),
                         reads=["Wo"], writes=["Wo"])
                P.op("pool", lambda e: e.memset(afp[0][:], 0.0), writes=[("afp", 0)])
                P.op("pool", lambda e: e.memset(afp[1][:], 0.0), writes=[("afp", 1)])
                def stA(t):
                    ts_ = slice(t * 128, (t + 1) * 128)
                    b = t % 2
                    P.op("sp", lambda e, b=b, ts_=ts_: e.dma_start(out=xt[b][:], in_=x[ts_, :]), writes=[("xt", b)], dma=True, key=f"xt{b}")
                    P.op("act", lambda e, ts_=ts_: e.activation(out=sqA[:], in_=oA[:, :, ts_], func=AF.Square), writes=["sqA"])
                    P.op("act", lambda e, ts_=ts_: e.activation(out=sqB[:], in_=oB[:, :, ts_], func=AF.Square), writes=["sqB"])
                    ss = psum[6]
                    for c in range(4):
                        P.op("pe", lambda e, c=c: e.matmul(ss[:, 0:128], lhsT=sqA[:, c, :], rhs=ones_bf, start=(c == 0), stop=(c == 3)), reads=["sqA"], writes=["ps6"])
                    for c in range(4):
                        P.op("pe", lambda e, c=c: e.matmul(ss[:, 128:256], lhsT=sqB[:, c, :], rhs=ones_bf, start=(c == 0), stop=(c == 3)), reads=["sqB"], writes=["ps6"])
                    yield
                    P.op("act", lambda e: e.activation(out=sm[:, 0:2], in_=ss[:, 0:256:128], func=AF.Sqrt, scale=1.0 / 512, bias=gcols[:, GC_EPS:GC_EPS + 1]), reads=["ps6"], writes=["sm01"])
                    P.op("dve", lambda e: e.reciprocal(out=sm[:, 2:4], in_=sm[:, 0:2]), reads=["sm01"], writes=["sm23"])
                    yield
                    for hf in range(2):
                        hs = slice(hf * 512, (hf + 1) * 512)
                        for c in range(4):
                            P.op("pe", lambda e, c=c, hs=hs, hf=hf, ts_=ts_: e.matmul(psum[hf][:], lhsT=oA[:, c, ts_], rhs=Wo[:, c, hs], start=(c == 0), stop=(c == 3)), reads=["Wo"], writes=[f"ps{hf}"])
                        for c in range(4):
                            P.op("pe", lambda e, c=c, hs=hs, hf=hf, ts_=ts_: e.matmul(psum[2 + hf][:], lhsT=oB[:, c, ts_], rhs=Wo[:, 4 + c, hs], start=(c == 0), stop=(c == 3)), reads=["Wo"], writes=[f"ps{2 + hf}"])
                        P.op("dve", lambda e, hs=hs, hf=hf, b=b: e.scalar_tensor_tensor(out=x1[b][:, hs], in0=psum[hf][:], scalar=sm[:, 2:3], in1=xt[b][:, hs], op0=ALU.mult, op1=ALU.add),
                             reads=[f"ps{hf}", "sm23", ("xt", b)], writes=[("x1", b, hf)])
                        P.op("dve", lambda e, hs=hs, hf=hf, b=b: e.scalar_tensor_tensor(out=x1[b][:, hs], in0=psum[2 + hf][:], scalar=sm[:, 3:4], in1=x1[b][:, hs], op0=ALU.mult, op1=ALU.add),
                             reads=[f"ps{2 + hf}", "sm23", ("x1", b, hf)], writes=[("x1", b, hf)])
                        yield
                    X1 = [("x1", b, 0), ("x1", b, 1)]
                    P.op("sp", lambda e, b=b, ts_=ts_: e.dma_start(out=out[ts_, :], in_=x1[b][:]), reads=X1, dma=True, key=f"o{b}")

                def stB1(t):
                    ts_ = slice(t * 128, (t + 1) * 128)
                    b = t % 2
                    h2T = h2Tb_[b]
                    X1 = [("x1", b, 0), ("x1", b, 1)]
                    P.op("dve", lambda e, b=b: e.tensor_tensor(out=h2f[b][:], in0=x1[b][:], in1=x1[b][:], op=ALU.mult), reads=X1, writes=[("h2f", b)])
                    P.op("dve", lambda e, b=b: e.reduce_sum(out=sm[:, 4:5], in_=h2f[b][:], axis=mybir.AxisListType.X), reads=[("h2f", b)], writes=["sm4"])
                    yield
                    P.op("act", lambda e: e.activation(out=sm[:, 5:6], in_=sm[:, 4:5], func=AF.Sqrt, scale=1.0 / D, bias=gcols[:, GC_EPS:GC_EPS + 1]), reads=["sm4"], writes=["sm5"])
                    P.op("dve", lambda e: e.reciprocal(out=sm[:, 6:7], in_=sm[:, 5:6]), reads=["sm5"], writes=["sm6"])
                    yield
                    P.op("dve", lambda e, b=b: e.scalar_tensor_tensor(out=h2f[b][:], in0=x1[b][:], scalar=sm[:, 6:7], in1=g2bc[:], op0=ALU.mult, op1=ALU.mult),
                         reads=X1 + ["sm6", "g2bc", ("h2f", b)], writes=[("h2f", b)])
                    P.op("pool", lambda e, b=b: e.tensor_copy(out=h2b[b][:], in_=h2f[b][:]), reads=[("h2f", b)], writes=[("h2b", b)])
                    P.op("sp", lambda e, b=b, ts_=ts_: e.dma_start(out=h2_dram[ts_, :], in_=h2b[b][:]), reads=[("h2b", b)], dma=True, key=f"h2o{b}")
                    yield
                    for k in range(8):
                        pb = 4 + k // 4
                        P.op("pe", lambda e, k=k, pb=pb, b=b: e.transpose(psum[pb][:, (k % 4) * 128:(k % 4 + 1) * 128], h2f[b][:, k * 128:(k + 1) * 128], identf[:]),
                             reads=[("h2f", b), "identf"], writes=[f"ps{pb}"])
                    yield
                    P.op("dve", lambda e: e.tensor_copy(out=h2T[:, 0:4, :], in_=psum[4][:].rearrange("p (a b) -> p a b", a=4)), reads=["ps4"], writes=[("h2Ta", b)])
                    P.op("dve", lambda e: e.tensor_copy(out=h2T[:, 4:8, :], in_=psum[5][:].rearrange("p (a b) -> p a b", a=4)), reads=["ps5"], writes=[("h2Tb", b)])

                def stB2(t):
                    ts_ = slice(t * 128, (t + 1) * 128)
                    b = t % 2
                    h2T = h2Tb_[b]
                    lg = psum[7]
                    for k in range(8):
                        P.op("pe", lambda e, k=k: e.matmul(lg[:, 0:128], lhsT=h2T[:, k, :], rhs=wr[:, k, :], start=(k == 0), stop=(k == 7)),
                             reads=[("h2Ta", b), ("h2Tb", b), "wr"], writes=["ps7"])
                    af = afp[b]
                    yield
                    P.op("dve", lambda e: e.reduce_max(out=sm[:, 7:8], in_=lg[:, 0:16], axis=mybir.AxisListType.X), reads=["ps7"], writes=["sm7"])
                    P.op("dve", lambda e: e.tensor_scalar(out=sm[:, 8:9], in0=sm[:, 7:8], scalar1=-1.0, scalar2=None, op0=ALU.mult), reads=["sm7"], writes=["sm8"])
                    yield
                    P.op("act", lambda e, af=af: e.activation(out=af[:, 0:16], in_=lg[:, 0:16], func=AF.Exp, bias=sm[:, 8:9]), reads=["ps7", "sm8"], writes=[("afp", b)])
                    yield
                    P.op("dve", lambda e, af=af: e.reduce_sum(out=sm[:, 9:10], in_=af[:, 0:16], axis=mybir.AxisListType.X), reads=[("afp", b)], writes=["sm9"])
                    P.op("dve", lambda e: e.reciprocal(out=sm[:, 10:11], in_=sm[:, 9:10]), reads=["sm9"], writes=["sm10"])
                    P.op("dve", lambda e, af=af: e.tensor_scalar(out=af[:, 0:16], in0=af[:, 0:16], scalar1=sm[:, 10:11], scalar2=None, op0=ALU.mult), reads=["sm10", ("afp", b)], writes=[("afp", b)])
                    P.op("sp", lambda e, af=af, ts_=ts_: e.dma_start(out=aff_dram[ts_, :], in_=af[:, 0:16]), reads=[("afp", b)], dma=True, key=f"afo{b}")
                    yield
                    P.op("pe", lambda e, af=af: e.transpose(lg[:, 128:256], af[:], identf[:]), reads=[("afp", b)], writes=["ps7"])
                    yield
                    P.op("dve", lambda e, ts_=ts_: e.tensor_copy(out=affT[:, ts_], in_=lg[:, 128:256]), reads=["ps7"], writes=[("affT", t)])


                def run_rr(gens):
                    gens = list(gens)
                    while gens:
                        for g_ in list(gens):
                            try:
                                next(g_)
                            except StopIteration:
                                gens.remove(g_)

                for t in range(NT + 2):
                    gl = []
                    if t < NT:
                        gl.append(stA(t))
                    if 1 <= t <= NT:
                        gl.append(stB1(t - 1))
                    if 2 <= t:
                        gl.append(stB2(t - 2))
                    run_rr(gl)
                dump(P, "affT", affT)
                P.flush(blk)

        NWB = 7
        WM = [A.at(f"WM{i}", [128, 8, 1024], BF16, KB(20 + 16 * i)) for i in range(NWB)]
        wsrc = (wg_d, wu_d, wd_d)

        def wload_mat(i):
            if i >= 3 * NE:
                return
            ex_, kind, sl = i // 3, i % 3, i % NWB
            P.op("pool", lambda e: e.dma_start(out=WM[sl][:], in_=wsrc[kind][ex_].rearrange("(k p) f -> p k f", p=128)), writes=[("WM", sl)], dma=True, key=f"wm{sl}")

        if stop_after >= 5:
            A.lo = KB(132)
            LIM = SB_END
            junk = A.tmp("junk", [128, S], BF16, LIM)
            mask = A.tmp("mask", [128, S], F32, LIM)
            cum = A.tmp("cum", [128, S], F32, LIM)
            cumh = A.tmp("cumh", [128, S], F16, LIM)
            bs = A.tmp("bs", [128, 8], F32, LIM)
            with nc.Block() as blk:
                if stop_after >= 6:
                    for i in range(NWB):
                        wload_mat(i)
                P.op("dve", lambda e: e.memset(bs[:, 0:1], 0.0), writes=["bs"])
                P.op("dve", lambda e: e.memset(bs[:, 1:2], 1.0), reads=["bs"], writes=["bs"])
                P.op("dve", lambda e: e.memset(bs[:, 2:3], 0.5), reads=["bs"], writes=["bs"])
                for it_ in range(30):
                    hn = 2.0 ** -(it_ + 2)
                    P.op("dve", lambda e: e.tensor_tensor(out=bs[:, 5:6], in0=bs[:, 2:3], in1=bs[:, 0:1], op=ALU.subtract), reads=["bs"], writes=["bs"])
                    P.op("dve", lambda e: e.tensor_scalar(out=junk[:], in0=affT[:], scalar1=bs[:, 2:3], scalar2=None, op0=ALU.is_ge, op1=ALU.add, accum_out=bs[:, 3:4]),
                         reads=["bs"], writes=["bs", "junk"])
                    P.op("dve", lambda e: e.tensor_single_scalar(out=bs[:, 4:5], in_=bs[:, 3:4], scalar=CAP - 0.5, op=ALU.is_ge), reads=["bs"], writes=["bs"])
                    P.op("dve", lambda e: e.scalar_tensor_tensor(out=bs[:, 0:1], in0=bs[:, 5:6], scalar=bs[:, 4:5], in1=bs[:, 0:1], op0=ALU.mult, op1=ALU.add), reads=["bs"], writes=["bs"])
                    P.op("dve", lambda e, hn=hn: e.tensor_scalar(out=bs[:, 2:3], in0=bs[:, 0:1], scalar1=hn, scalar2=None, op0=ALU.add), reads=["bs"], writes=["bs"])
                P.op("dve", lambda e: e.tensor_scalar(out=mask[:], in0=affT[:], scalar1=bs[:, 0:1], scalar2=None, op0=ALU.is_ge), reads=["bs"], writes=["mask"])
                P.op("dve", lambda e: e.tensor_tensor_scan(out=cum[:], data0=mask[:], data1=mask[:], initial=0.0, op0=ALU.add, op1=ALU.max), reads=["mask"], writes=["cum"])
                P.op("dve", lambda e: e.tensor_scalar(out=cumh[:], in0=cum[:], scalar1=1000.0, scalar2=None, op0=ALU.min), reads=["cum"], writes=["cumh"])
                P.op("sp", lambda e: e.dma_start(out=cum_dram, in_=cumh[0:16, :]), reads=["cumh"], dma=True, key="p5a")
                dump(P, "cum", cum)
                P.flush(blk)

        if stop_after >= 6:
            A.lo = KB(132)
            LIM = SB_END
            cbc = A.tmp("cbc", [128, S], F16, LIM)
            junk6 = A.tmp("junk6", [128, S], BF16, LIM)
            xg = [[A.tmp(f"xg{b}{i}", [128, 1024], BF16, LIM) for i in range(4)] for b in range(2)]
            xeT = [A.tmp(f"xeT{b}", [128, 8, 512], BF16, LIM) for b in range(2)]
            actT = A.tmp("actT", [128, 8, 512], BF16, LIM)
            sg = [A.tmp(f"sg{i}", [128, 512], F32, LIM) for i in range(2)]
            ye = [A.tmp(f"ye{i}", [128, 1024], F32, LIM) for i in range(2)]
            gt = [[A.tmp(f"gt{b}{i}", [128, 16], F32, LIM) for i in range(4)] for b in range(2)]
            idxf = A.tmp("idxf", [128, 4], F32, LIM)
            idxi = [A.tmp(f"idxi{i}", [128, 4], I32, LIM) for i in range(2)]
            with nc.Block() as blk:
                prev_sc = [[]]

                def s1_cbc(ex):
                    P.op("sp", lambda e: e.dma_start(out=cbc[:], in_=cum_dram[ex:ex + 1, :].partition_broadcast(128)), writes=["cbc"], dma=True, key="cbc")

                def s1_cmp(ex, j):
                    P.op("dve", lambda e: e.tensor_scalar(out=junk6[:], in0=cbc[:], scalar1=ccol[:, j:j + 1], scalar2=None, op0=ALU.is_le, op1=ALU.add, accum_out=idxf[:, j:j + 1]),
                         reads=["cbc"], writes=["junk6", ("idxf", j)])

                def s1_gather(ex):
                    b = ex % 2
                    ii = idxi[b]
                    IDF = [("idxf", j) for j in range(4)]
                    P.op("dve", lambda e: e.tensor_scalar(out=idxf[:], in0=idxf[:], scalar1=float(S - 1), scalar2=None, op0=ALU.min), reads=IDF, writes=IDF)
                    P.op("dve", lambda e: e.tensor_copy(out=ii[:], in_=idxf[:]), reads=IDF, writes=[("idxi", b)])
                    for j in range(4):
                        P.op("pool", lambda e, j=j: e.indirect_dma_start(out=xg[b][j][:], out_offset=None, in_=h2_dram, in_offset=bass.IndirectOffsetOnAxis(ap=ii[:, j:j + 1], axis=0)),
                             reads=[("idxi", b)], writes=[("xg", b, j)], dma=True, key=f"xg{b}{j}")
                        P.op("pool", lambda e, j=j: e.indirect_dma_start(out=gt[b][j][:], out_offset=None, in_=aff_dram, in_offset=bass.IndirectOffsetOnAxis(ap=ii[:, j:j + 1], axis=0)),
                             reads=[("idxi", b)], writes=[("gt", b, j)], dma=True, key=f"gt{b}{j}")

                def s1_tr(ex):
                    b = ex % 2
                    for j in range(4):
                        tpb = psum[j % 2][:].bitcast(BF16)
                        for k in range(8):
                            P.op("pe", lambda e, j=j, k=k, tpb=tpb: e.transpose(tpb[:, k * 128:(k + 1) * 128], xg[b][j][:, k * 128:(k + 1) * 128], ident_bf),
                                 reads=[("xg", b, j)], writes=[f"ps{j % 2}"])
                        P.op("dve", lambda e, j=j, tpb=tpb: e.tensor_copy(out=xeT[b][:, :, j * 128:(j + 1) * 128], in_=tpb.rearrange("p (a b) -> p a b", a=8)),
                             reads=[f"ps{j % 2}"], writes=[("xeT", b, j)])

                def s2_gu(ex, fc):
                    b = ex % 2
                    sg_, su_ = (3 * ex) % NWB, (3 * ex + 1) % NWB
                    XE = [("xeT", b, j) for j in range(4)]
                    gb, ub = 2 + fc % 2, 4 + fc % 2
                    for k in range(8):
                        P.op("pe", lambda e, k=k: e.matmul(psum[gb][:], lhsT=WM[sg_][:, k, fc * 128:(fc + 1) * 128], rhs=xeT[b][:, k, :], start=(k == 0), stop=(k == 7)),
                             reads=XE + [("WM", sg_)], writes=[f"ps{gb}"])
                    for k in range(8):
                        P.op("pe", lambda e, k=k: e.matmul(psum[ub][:], lhsT=WM[su_][:, k, fc * 128:(fc + 1) * 128], rhs=xeT[b][:, k, :], start=(k == 0), stop=(k == 7)),
                             reads=XE + [("WM", su_)], writes=[f"ps{ub}"])
                    P.op("act", lambda e: e.activation(out=sg[fc % 2][:], in_=psum[gb][:], func=AF.Silu), reads=[f"ps{gb}"], writes=[("sg", fc % 2)])
                    P.op("dve", lambda e: e.tensor_tensor(out=actT[:, fc, :], in0=psum[ub][:], in1=sg[fc % 2][:], op=ALU.mult),
                         reads=[f"ps{ub}", ("sg", fc % 2)], writes=[("actT", fc)])

                def s2_down(ex):
                    b = ex % 2
                    sd_ = (3 * ex + 2) % NWB
                    ii = idxi[b]
                    AC = [("actT", fc) for fc in range(8)]
                    for j in range(4):
                        yb_ = ye[j % 2]
                        for hf in range(2):
                            pb = 6 + hf
                            for fc in range(8):
                                P.op("pe", lambda e, fc=fc, j=j, hf=hf, pb=pb: e.matmul(psum[pb][:], lhsT=actT[:, fc, j * 128:(j + 1) * 128], rhs=WM[sd_][:, fc, hf * 512:(hf + 1) * 512],
                                                                                      start=(fc == 0), stop=(fc == 7)),
                                     reads=AC + [("WM", sd_)], writes=[f"ps{pb}"])
                            P.op("dve", lambda e, j=j, hf=hf, pb=pb, yb_=yb_: e.tensor_scalar(out=yb_[:, hf * 512:(hf + 1) * 512], in0=psum[pb][:], scalar1=gt[b][j][:, ex:ex + 1], scalar2=None, op0=ALU.mult),
                                 reads=[f"ps{pb}", ("gt", b, j)], writes=[("ye", j % 2, hf)])
                        o_ = P.op("pool", lambda e, j=j, yb_=yb_: e.indirect_dma_start(out=out, out_offset=bass.IndirectOffsetOnAxis(ap=ii[:, j:j + 1], axis=0), in_=yb_[:], in_offset=None, compute_op=ALU.add),
                                  reads=[("ye", j % 2, 0), ("ye", j % 2, 1), ("idxi", b)], deps=list(prev_sc[0]), dma=True, key=f"sc{j % 2}")
                        prev_sc[0] = [o_]

                s1_cbc(0)
                for j in range(4):
                    s1_cmp(0, j)
                s1_gather(0)
                s1_tr(0)
                for ex in range(NE):
                    nx = ex + 1
                    if nx < NE:
                        s1_cbc(nx)
                    for fc in range(8):
                        s2_gu(ex, fc)
                        if nx < NE and fc < 4:
                            s1_cmp(nx, fc)
                        if nx < NE and fc == 3:
                            s1_gather(nx)
                    wload_mat(3 * ex + NWB)
                    wload_mat(3 * ex + 1 + NWB)
                    if nx < NE:
                        s1_tr(nx)
                    s2_down(ex)
                    wload_mat(3 * ex + 2 + NWB)
                P.flush(blk)

    return nc, dbg


def t5_bucket_np(rel):
    half, max_exact = 16, 8
    base = np.where(rel > 0, half, 0)
    n = np.abs(rel)
    nf = np.maximum(n, 1).astype(np.float32)
    large = max_exact + (np.log(nf / max_exact) / math.log(128 / max_exact) * (half - max_exact)).astype(np.int32)
    large = np.minimum(large, half - 1)
    return base + np.where(n < max_exact, n, large)


def host_constants():
    c = {}
    cm = np.zeros((128, 6, 128), np.float32)
    cm[:, 0, :] = np.eye(128)
    cm[:, 1, :] = 1.0
    cm[0:64, 2, 0:64] = 1 / 64
    cm[64:128, 2, 64:128] = 1 / 64
    cm[0:64, 3, 0:64] = 1 / 64
    cm[64:96, 3, 64:96] = 1 / 32
    cm[96:128, 3, 96:128] = 1 / 32
    cm[0:32, 4, 0:32] = 1 / 32
    cm[32:64, 4, 32:64] = 1 / 32
    for j in range(64):
        for i in range(64, 128):
            if (j % 32) == ((i - 64) % 32):
                cm[j, 5, i] = 1.0
    c["cmat"] = cm
    pos = np.arange(S, dtype=np.float32)
    freqs = (np.float32(10000.0) ** (-np.arange(0, 32, 2, dtype=np.float32) / np.float32(32))).astype(np.float32)
    ang = pos[:, None] * freqs[None, :]
    cos, sin = np.cos(ang).astype(np.float32).T, np.sin(ang).astype(np.float32).T
    cs_main = np.concatenate([cos, cos], 0)
    cs_sw = np.concatenate([-sin, sin], 0)
    c["csk"] = np.ascontiguousarray(np.concatenate([cs_main, cs_sw, np.zeros((64, S), np.float32)], 0))
    c["csq"] = np.ascontiguousarray(np.concatenate([np.ones((64, S), np.float32), cs_main, cs_sw], 0))
    i = np.arange(640)
    rel = 255 - i
    valid = np.abs(rel) <= 128
    bk = t5_bucket_np(rel)
    oh = np.zeros((128, 640), np.float32)
    oh[bk[valid], i[valid]] = 1.0
    c["ohrel"] = oh
    c["maskadd"] = np.tile(np.where(valid, 0.0, -30000.0).astype(np.float32)[None, :], (128, 1))
    c["ccol"] = (np.arange(4)[None, :] * 128 + np.arange(128)[:, None]).astype(np.float32)
    return c


def host_layout(inp):
    f = lambda a: np.ascontiguousarray(np.asarray(a, dtype=np.float32))
    w_in = f(inp["w_in"])[0]
    sw = np.concatenate([np.arange(16, 32), np.arange(0, 16)])
    qperm = np.concatenate([np.concatenate([np.arange(c * 64, c * 64 + 64), np.arange((c + 4) * 64, (c + 4) * 64 + 64)]) for c in range(4)])
    cols = [w_in[:, 0:512][:, qperm], w_in[:, 512:640], w_in[:, 768:1152], w_in[:, 1152:1408],
            w_in[:, 1408:1440], w_in[:, 1408:1440][:, sw], np.zeros((D, 64), np.float32), np.zeros((D, 128), np.float32),
            w_in[:, 640:768]]
    w_aug = np.concatenate(cols, 1)
    assert w_aug.shape[1] == 1664, w_aug.shape
    g = np.zeros((128, NGC), np.float32)
    g[:, GC_Q2] = np.tile(f(inp["a_q_norm_g"])[0], 2)
    g[:, GC_K2] = np.tile(f(inp["a_k_norm_g"])[0], 2)
    g[:, GC_CQ:GC_CQ + 3] = f(inp["cq_norm_g"])[0].reshape(3, 128).T
    g[:, GC_CKV:GC_CKV + 2] = f(inp["ckv_norm_g"])[0].reshape(2, 128).T
    kr = f(inp["b_kr_g"])[0]
    g[0:32, GC_KR] = kr
    g[32:64, GC_KR] = kr[sw]
    qr = f(inp["b_qr_g"])[0]
    g[:, GC_QB] = np.concatenate([f(inp["b_qn_g"])[0], qr, qr[sw]])
    g[:, GC_KN] = np.tile(f(inp["b_kn_g"])[0], 2)
    g[:, GC_LN1:GC_LN1 + 8] = f(inp["ln1_g"])[0].reshape(8, 128).T
    g[:, GC_OA:GC_OA + 4] = f(inp["out_a_g"])[0][qperm].reshape(4, 128).T
    g[:, GC_OB:GC_OB + 4] = f(inp["out_b_g"])[0].reshape(4, 128).T
    g[:, GC_EPS] = EPS
    w_qb = f(inp["w_qb"])[0]
    wq_cols = []
    for h in range(8):
        b = h * 96
        wq_cols += [w_qb[:, b:b + 64], w_qb[:, b + 64:b + 96], w_qb[:, b + 64:b + 96][:, sw]]
    w_kvb = f(inp["w_kvb"])[0]
    kn = np.concatenate([w_kvb[:, h * 128:h * 128 + 64] for h in range(8)], 1)
    vv = np.concatenate([w_kvb[:, h * 128 + 64:h * 128 + 128] for h in range(8)], 1)
    w_o = f(inp["w_o"])[0]
    wo_p = np.concatenate([w_o[0:512][qperm], w_o[512:1024]], 0)
    shared = {
        "w_aug": np.ascontiguousarray(w_aug), "gcols": g,
        "relb_rep": np.ascontiguousarray(np.concatenate([np.repeat(f(inp["rel_bias"])[:, :, None], 128, axis=2), np.zeros((96, 8, 128), np.float32)], 0)), "a_sink": f(inp["a_sink"]),
        "wqb_aug": np.ascontiguousarray(np.concatenate(wq_cols, 1)),
        "wkvb_aug": np.ascontiguousarray(np.concatenate([kn, vv], 1)),
        "wo_p": np.ascontiguousarray(wo_p), "ln2_g": f(inp["ln2_g"]),
        "w_router": np.ascontiguousarray(np.concatenate([f(inp["w_router"])[0], np.zeros((D, 112), np.float32)], 1)),
        "w_gate": f(inp["w_gate"])[0], "w_up": f(inp["w_up"])[0], "w_down": f(inp["w_down"])[0],
    }
    shared.update(host_constants())
    return shared


_CACHE = {}


def kernel(**inputs):
    x = np.asarray(inputs["x"], dtype=np.float32)
    shared = host_layout(inputs)
    if "nc" not in _CACHE:
        _CACHE["nc"] = build_program()[0]
    nc = _CACHE["nc"]
    in_maps = []
    for b in range(8):
        m = dict(shared)
        m["x"] = np.ascontiguousarray(x[b])
        m["xT"] = np.ascontiguousarray(x[b].T)
        in_maps.append(m)
    res = run_bass_kernel_spmd(nc, in_maps, core_ids=list(range(8)))
    return np.stack([np.asarray(r["out"], dtype=np.float32) for r in res.results], 0)
```

```python
import math
import os
from contextlib import ExitStack

import numpy as np
import ml_dtypes

import concourse.bass as bass
import concourse.mybir as mybir
from concourse.bass_utils import run_bass_kernel_spmd

F32 = mybir.dt.float32
BF16 = mybir.dt.bfloat16
F16 = mybir.dt.float16
I32 = mybir.dt.int32
ALU = mybir.AluOpType
AF = mybir.ActivationFunctionType

S = 4096
D = 1024
NT = 32
NG = 8
EPS = 1e-6
NE = 16
CAP = 512
SB_BASE = 16512
SB_END = 229376

GC_Q2, GC_K2, GC_CQ, GC_CKV, GC_KR, GC_QB, GC_KN, GC_LN1, GC_OA, GC_OB, GC_EPS = 0, 1, 2, 5, 7, 8, 9, 10, 18, 22, 26
NGC = 28


class Op:
    __slots__ = ("eng", "fn", "deps", "signal", "sem", "val", "is_dma", "key", "idx")

    def __init__(self, eng, fn, deps, is_dma, key):
        self.eng, self.fn, self.deps, self.is_dma, self.key = eng, fn, deps, is_dma, key
        self.signal = False
        self.sem = None
        self.val = 0


class Prog:
    ENGS = ("pe", "act", "dve", "pool", "sp")
    LIMIT = 30000

    def __init__(self, nc, sems):
        self.nc = nc
        self.free_sems = list(sems)
        self.ops = {e: [] for e in self.ENGS}
        self.lastw = {}
        self.readers = {}
        self.eng_sem = {e: None for e in self.ENGS}
        self.eng_cnt = {e: 0 for e in self.ENGS}
        self.dma_sem = {}
        self.dma_cnt = {}
        self.waited = {e: {} for e in self.ENGS}
        self.phase_dma = []

    def op(self, eng, fn, reads=(), writes=(), dma=False, key=None, deps=()):
        d = []
        for r in reads:
            w = self.lastw.get(r)
            if w is not None:
                d.append(w)
        for w_ in writes:
            w = self.lastw.get(w_)
            if w is not None:
                d.append(w)
            d.extend(self.readers.get(w_, ()))
        d.extend(deps)
        self.nops = getattr(self, "nops", 0) + 1
        if self.nops > int(os.environ.get("P_MAXOPS", 10 ** 9)) and fn is not None:
            return Op(eng, None, [], False, None)
        o = Op(eng, fn, d, dma, key)
        for r in reads:
            self.readers.setdefault(r, []).append(o)
        for w_ in writes:
            self.lastw[w_] = o
            self.readers[w_] = []
        self.ops[eng].append(o)
        if dma:
            if key not in self.dma_sem:
                self.dma_sem[key] = self.free_sems.pop()
                self.dma_cnt[key] = 0
            self.dma_cnt[key] += 16
            o.sem, o.val = self.dma_sem[key], self.dma_cnt[key]
            self.phase_dma.append(o)
        return o

    def flush(self, blk):
        if self.phase_dma:
            last = {}
            for o in self.phase_dma:
                last[o.sem] = o
            self.op("sp", None, deps=list(last.values()))
        for e in self.ENGS:
            for o in self.ops[e]:
                for dd in o.deps:
                    if dd.is_dma:
                        continue
                    if dd.eng == o.eng and o.eng == "pe":
                        continue
                    dd.signal = True
        for e in self.ENGS:
            for o in self.ops[e]:
                if o.is_dma or not o.signal:
                    continue
                if self.eng_sem[e] is None or self.eng_cnt[e] >= self.LIMIT:
                    self.eng_sem[e] = self.free_sems.pop()
                    self.eng_cnt[e] = 0
                self.eng_cnt[e] += 1
                o.sem, o.val = self.eng_sem[e], self.eng_cnt[e]
        starters = {"pe": blk.tensor, "act": blk.scalar, "dve": blk.vector, "pool": blk.gpsimd, "sp": blk.sync}
        for e in self.ENGS:
            ops = self.ops[e]
            if not ops:
                continue

            def body(h, ops=ops, e=e):
                waited = self.waited[e]
                for o in ops:
                    need = {}
                    for dd in o.deps:
                        if not dd.is_dma and dd.eng == e and e == "pe":
                            continue
                        if dd.sem is None:
                            continue
                        if need.get(dd.sem, (0, None))[0] < dd.val:
                            need[dd.sem] = (dd.val, dd.sem)
                    for sem, (val, _) in need.items():
                        if waited.get(sem, 0) < val:
                            h.wait_ge(sem, val)
                            waited[sem] = val
                    if o.fn is None:
                        continue
                    ins = o.fn(h)
                    if o.is_dma:
                        ins.then_inc(o.sem, 16)
                    elif o.signal:
                        ins.then_inc(o.sem, 1)

            starters[e](body)
        self.ops = {e: [] for e in self.ENGS}
        self.lastw = {}
        self.readers = {}
        self.phase_dma = []


class Arena:
    def __init__(self, nc):
        self.nc = nc
        self.n = 0
        self.lo = SB_BASE

    def at(self, name, shape, dtype, off):
        self.n += 1
        return self.nc.alloc_sbuf_tensor_at(f"{name}_{self.n}", list(shape), dtype, offset=off)

    def tmp(self, name, shape, dtype, limit):
        nbytes = int(np.prod(shape[1:])) * mybir.dt.size(dtype)
        nbytes = (nbytes + 31) // 32 * 32
        off = self.lo
        self.lo += nbytes
        assert self.lo <= limit, f"SBUF overflow allocating {name}: {self.lo} > {limit}"
        return self.at(name, shape, dtype, off)


def KB(x):
    return SB_BASE + int(x * 1024)


def build_program(debug=(), stop_after=99):
    nc = bass.Bass("TRN2", target_bir_lowering=False)
    dbg = {}

    def din(name, shape, dt=F32):
        return nc.dram_tensor(name, list(shape), dt, kind="ExternalInput").ap()

    xT = din("xT", [D, S])
    x = din("x", [S, D])
    w_aug = din("w_aug", [D, 1664])
    gcols_d = din("gcols", [128, NGC])
    cmat_d = din("cmat", [128, 6, 128])
    csk_d = din("csk", [128, S])
    csq_d = din("csq", [128, S])
    ohrel_d = din("ohrel", [128, 640])
    maskadd_d = din("maskadd", [128, 640])
    relb_d = din("relb_rep", [128, 8, 128])
    sink_d = din("a_sink", [1, 8])
    wqb_d = din("wqb_aug", [384, 1024])
    wkvb_d = din("wkvb_aug", [256, 1024])
    wo_d = din("wo_p", [D, D])
    ln2_d = din("ln2_g", [1, D])
    wr_d = din("w_router", [D, 128])
    wg_d = din("w_gate", [NE, D, D])
    wu_d = din("w_up", [NE, D, D])
    wd_d = din("w_down", [NE, D, D])
    ccol_d = din("ccol", [128, 4])
    out = nc.dram_tensor("out", [S, D], F32, kind="ExternalOutput").ap()
    trep = nc.dram_tensor("trep", [8, 128, 640], F32, kind="ExternalOutput")
    h2_dram = nc.dram_tensor("h2_dram", [S, D], BF16, kind="ExternalOutput").ap()
    aff_dram = nc.dram_tensor("aff_dram", [S, NE], F32, kind="ExternalOutput").ap()
    cum_dram = nc.dram_tensor("cum_dram", [NE, S], F16, kind="ExternalOutput").ap()

    def dump(P, name, t, eng="sp"):
        if name not in debug:
            return
        shp = list(t.shape)
        d = nc.dram_tensor("dbg_" + name, shp, t.dtype, kind="ExternalOutput").ap()
        dbg[name] = d
        lastops = [P.ops[en][-1] for en in ("pe", "act", "dve", "pool") if P.ops[en]]
        lo_p = 64 if name.startswith("KT") else 0
        P.op(eng, lambda e: e.dma_start(out=d[lo_p:], in_=t[lo_p:]), deps=lastops + list(P.phase_dma), dma=True, key="dbg_" + name)

    with ExitStack() as es:
        sems = [es.enter_context(nc.semaphore(f"s{i}")) for i in range(100)]
        P = Prog(nc, sems)
        A = Arena(nc)
        psum = [es.enter_context(nc.psum_tensor(f"ps{i}", [128, 512], F32)) for i in range(8)]

        cmat = A.at("cmat", [128, 6, 128], BF16, KB(0))
        identf = A.at("identf", [128, 128], F32, KB(1.5))
        gcols = A.at("gcols", [128, NGC], F32, KB(2))
        esink = A.at("esink", [128, 8], F32, KB(2.25))
        ccol = A.at("ccol", [128, 4], F32, KB(2.5))
        CONST_END = 4
        ident_bf = cmat[:, 0, :]
        ones_bf = cmat[:, 1, :]
        bd64 = cmat[:, 2, :]
        bdq = cmat[:, 3, :]
        bdk32 = cmat[:, 4, :]
        fold = cmat[:, 5, :]

        R_C = CONST_END
        R_A = R_C + 56
        R_OA = R_A + 56
        cqn = A.at("cqn", [128, 3, S], BF16, KB(R_C))
        ckvn = A.at("ckvn", [128, 2, S], BF16, KB(R_C + 24))
        KT = [A.at(f"KT{i}", [128, S], BF16, KB(R_C + 40 + 8 * i)) for i in range(2)]
        qA = A.at("qA", [128, 4, S], BF16, KB(R_A))
        kA = A.at("kA", [128, S], BF16, KB(R_A + 32))
        VA = A.at("VA", [128, NT, 2, 128], BF16, KB(R_A + 40))
        oA = A.at("oA", [128, 4, S], BF16, KB(R_OA))

        if stop_after >= 1:
            A.lo = KB(R_OA)
            LIM = SB_END
            Wbf = A.tmp("Wbf", [128, 8, 1664], BF16, LIM)
            Wst = A.tmp("Wst", [128, 1664], F32, LIM)
            xst = [A.tmp(f"xst{i}", [128, 512], F32, LIM) for i in range(3)]
            xbf = [A.tmp(f"xbf{i}", [128, 8, 512], BF16, LIM) for i in range(2)]
            xsq = A.tmp("xsq", [128, 8, 512], BF16, LIM)
            zt = [A.tmp(f"zt{i}", [128, 512], F32, LIM) for i in range(4)]
            sqb = [A.tmp(f"sqb{i}", [128, 512], BF16, LIM) for i in range(3)]
            rs = [A.tmp(f"rs{i}", [128, 512], F32, LIM) for i in range(2)]
            rinv = [A.tmp(f"rinv{i}", [128, 512], F32, LIM) for i in range(2)]
            rstd_bc = [A.tmp(f"rstdbc{i}", [128, 512], F32, LIM) for i in range(2)]
            rsc = A.tmp("rsc", [128, 4], F32, LIM)
            rstd_col = [A.tmp(f"rstdcol{i}", [128, 4], F32, LIM) for i in range(2)]
            wk = A.tmp("wk", [128, 512], BF16, LIM)
            csk = [A.tmp(f"csk{i}", [128, 512], F32, LIM) for i in range(2)]
            sinkt = A.tmp("sinkt", [128, 8], F32, LIM)

            with nc.Block() as blk:
                P.op("sp", lambda e: e.dma_start(out=Wst[:, 0:768], in_=cmat_d.rearrange("p a b -> p (a b)")), writes=["Wst"], dma=True, key="wst")
                P.op("sp", lambda e: e.dma_start(out=identf[:], in_=cmat_d[:, 0, :]), writes=["identf"], dma=True, key="c11")
                P.op("sp", lambda e: e.dma_start(out=gcols[:], in_=gcols_d), writes=["gcols"], dma=True, key="c12")
                P.op("sp", lambda e: e.dma_start(out=ccol[:], in_=ccol_d), writes=["ccol"], dma=True, key="c13")
                P.op("sp", lambda e: e.dma_start(out=sinkt[:], in_=sink_d.partition_broadcast(128)), writes=["sinkt"], dma=True, key="c14")
                P.op("dve", lambda e: e.tensor_copy(out=cmat[:].rearrange("p a b -> p (a b)"), in_=Wst[:, 0:768]), reads=["Wst"], writes=["cmat"])
                P.op("act", lambda e: e.activation(out=esink[:], in_=sinkt[:], func=AF.Exp), reads=["sinkt"], writes=["esink"])
                P.op("pool", lambda e: e.memset(VA[:, :, 0, 64:128], 1.0), writes=["VAones0"])
                P.op("pool", lambda e: e.memset(VA[:, :, 1, 0:64], 1.0), writes=["VAones1"])
                for k in range(8):
                    P.op("sp", lambda e, k=k: e.dma_start(out=Wst[:], in_=w_aug[k * 128:(k + 1) * 128, :]),
                         writes=["Wst"], dma=True, key="wst")
                    P.op("dve", lambda e, k=k: e.tensor_scalar(out=Wbf[:, k, :], in0=Wst[:], scalar1=gcols[:, GC_LN1 + k:GC_LN1 + k + 1],
                                                               scalar2=None, op0=ALU.mult),
                         reads=["Wst", "gcols"], writes=[("Wbf", k)])
                WB = [("Wbf", k) for k in range(8)]

                def stage(G):
                    xb = xbf[G % 2]
                    P.op("sp", lambda e: e.dma_start(out=csk[G % 2][:], in_=csk_d[:, G * 512:(G + 1) * 512]),
                         writes=[("csk", G % 2)], dma=True, key=f"csk{G % 2}")
                    for k in range(8):
                        r = k % 3
                        P.op("sp", lambda e, k=k, r=r: e.dma_start(out=xst[r][:], in_=xT[k * 128:(k + 1) * 128, G * 512:(G + 1) * 512]),
                             writes=[("xst", r)], dma=True, key=f"xst{r}")
                        P.op("act", lambda e, k=k, r=r: e.activation(out=xsq[:, k, :], in_=xst[r][:], func=AF.Square),
                             reads=[("xst", r)], writes=[("xsq", k)])
                        P.op("dve", lambda e, k=k, r=r: e.tensor_copy(out=xb[:, k, :], in_=xst[r][:]),
                             reads=[("xst", r)], writes=[("xbf", G % 2, k)])

                def stats(G):
                    ss = psum[0]
                    for k in range(8):
                        P.op("pe", lambda e, k=k: e.matmul(ss[:], lhsT=ones_bf, rhs=xsq[:, k, :], start=(k == 0), stop=(k == 7)),
                             reads=[("xsq", k), "cmat"], writes=["ps0"])
                    P.op("act", lambda e: e.activation(out=rs[0][:], in_=ss[:], func=AF.Ln, scale=1.0 / D, bias=gcols[:, GC_EPS:GC_EPS + 1]),
                         reads=["ps0", "gcols"], writes=["rs0"])
                    P.op("act", lambda e: e.activation(out=rstd_bc[G % 2][:], in_=rs[0][:], func=AF.Exp, scale=-0.5), reads=["rs0"], writes=[("rstdbc", G % 2)])
                    sc = psum[1]
                    for t in range(4):
                        for k in range(8):
                            P.op("pe", lambda e, k=k, t=t: e.matmul(sc[:, t * 128:(t + 1) * 128], lhsT=xsq[:, k, t * 128:(t + 1) * 128], rhs=ones_bf,
                                                                   start=(k == 0), stop=(k == 7)),
                                 reads=[("xsq", k), "cmat"], writes=["ps1"])
                    P.op("act", lambda e: e.activation(out=rsc[:], in_=sc[:, 0:512:128], func=AF.Ln, scale=1.0 / D, bias=gcols[:, GC_EPS:GC_EPS + 1]),
                         reads=["ps1", "gcols"], writes=["rsc"])
                    P.op("act", lambda e: e.activation(out=rstd_col[G % 2][:], in_=rsc[:], func=AF.Exp, scale=-0.5), reads=["rsc"], writes=[("rstdcol", G % 2)])

                zring = [0]

                def proj(G, c):
                    slot = zring[0] % 4
                    zring[0] += 1
                    pb = 2 + (slot % 3)
                    zp = psum[pb]
                    xb = xbf[G % 2]
                    for k in range(8):
                        P.op("pe", lambda e, k=k: e.matmul(zp[:], lhsT=Wbf[:, k, c * 128:(c + 1) * 128], rhs=xb[:, k, :], start=(k == 0), stop=(k == 7)),
                             reads=[("Wbf", k), ("xbf", G % 2, k)], writes=[f"ps{pb}"])
                    P.op("dve", lambda e: e.tensor_tensor(out=zt[slot][:], in0=zp[:], in1=rstd_bc[G % 2][:], op=ALU.mult),
                         reads=[f"ps{pb}", ("rstdbc", G % 2)], writes=[("zt", slot)])
                    return slot

                nring = [0]

                def norm_rinv(slots, bd, nrows, scale):
                    j = nring[0] % 2
                    nring[0] += 1
                    msp = psum[5 + j]
                    for i, sl in enumerate(slots):
                        sb_ = sqb[(nring[0] * 3 + i) % 3] if False else sqb[i]
                        P.op("act", lambda e, sl=sl, sb_=sb_: e.activation(out=sb_[0:nrows, :], in_=zt[sl][0:nrows, :], func=AF.Square),
                             reads=[("zt", sl)], writes=[("sqb", i)])
                        P.op("pe", lambda e, sb_=sb_, i=i: e.matmul(msp[0:nrows, :], lhsT=bd[0:nrows, 0:nrows], rhs=sb_[0:nrows, :],
                                                                   start=(i == 0), stop=(i == len(slots) - 1)),
                             reads=[("sqb", i), "cmat"], writes=[f"ps{5 + j}"])
                    P.op("act", lambda e: e.activation(out=rs[1][0:nrows, :], in_=msp[0:nrows, :], func=AF.Ln, scale=scale, bias=gcols[0:nrows, GC_EPS:GC_EPS + 1]),
                         reads=[f"ps{5 + j}", "gcols"], writes=["rs1"])
                    P.op("act", lambda e: e.activation(out=rinv[j][0:nrows, :], in_=rs[1][0:nrows, :], func=AF.Exp, scale=-0.5), reads=["rs1"], writes=[("rinv", j)])
                    return j

                def finish(slot, j, gc, dst, dkey, nrows=128):
                    P.op("dve", lambda e: e.scalar_tensor_tensor(out=dst, in0=zt[slot][0:nrows, :], scalar=gcols[0:nrows, gc:gc + 1],
                                                                 in1=rinv[j][0:nrows, :], op0=ALU.mult, op1=ALU.mult),
                         reads=[("zt", slot), ("rinv", j), "gcols"], writes=[dkey])

                def group(G):
                    gs = slice(G * 512, (G + 1) * 512)
                    stats(G)

                    def fin_q(c):
                        return lambda sls, j: finish(sls[0], j, GC_Q2, qA[:, c, gs], ("qA", c, G))

                    def fin_k(sls, j):
                        finish(sls[0], j, GC_K2, kA[:, gs], ("kA", G))

                    def fin_cq(sls, j):
                        for i in range(3):
                            finish(sls[i], j, GC_CQ + i, cqn[:, i, gs], ("cqn", i, G))

                    def fin_ckv(sls, j):
                        for i in range(2):
                            finish(sls[i], j, GC_CKV + i, ckvn[:, i, gs], ("ckvn", i, G))

                    def fin_kr(sls, j):
                        sl = sls[0]
                        finish(sl, j, GC_KR, zt[sl][:, :], ("zt", sl))
                        P.op("dve", lambda e: e.tensor_tensor(out=wk[:], in0=zt[sl][:, :], in1=csk[G % 2][:], op=ALU.mult),
                             reads=[("zt", sl), ("csk", G % 2)], writes=["wk"])
                        kp = psum[7]
                        P.op("pe", lambda e: e.matmul(kp[:], lhsT=fold, rhs=wk[:], start=True, stop=True),
                             reads=["wk", "cmat"], writes=["ps7"])
                        P.op("dve", lambda e: e.tensor_copy(out=KT[0][64:128, gs], in_=kp[64:128, :]), reads=["ps7"], writes=[("KT0r", G)])
                        P.op("dve", lambda e: e.tensor_copy(out=KT[1][64:128, gs], in_=kp[64:128, :]), reads=["ps7"], writes=[("KT1r", G)])

                    descs = [([c], bd64, 1.0, fin_q(c)) for c in range(4)]
                    descs.append(([4], bd64, 1.0, fin_k))
                    descs.append(([5, 6, 7], ones_bf, 1.0 / 384, fin_cq))
                    descs.append(([10], bdk32, 1.0, fin_kr))
                    descs.append(([8, 9], ones_bf, 1.0 / 256, fin_ckv))
                    pending = None
                    for di, (chs, bd_, sc_, fin_) in enumerate(descs):
                        sls = [proj(G, c) for c in chs]
                        if pending is not None:
                            p_sls, p_bd, p_sc, p_fin = pending
                            p_fin(p_sls, norm_rinv(p_sls, p_bd, 128, p_sc))
                        pending = (sls, bd_, sc_, fin_)
                        if di == 4 and G + 1 < int(os.environ.get("P1_GROUPS", NG)):
                            stage(G + 1)
                    p_sls, p_bd, p_sc, p_fin = pending
                    p_fin(p_sls, norm_rinv(p_sls, p_bd, 128, p_sc))
                    for t in range(4):
                        tile_i = G * 4 + t
                        vp = psum[7]
                        xb = xbf[G % 2]
                        for k in range(8):
                            P.op("pe", lambda e, k=k, t=t: e.matmul(vp[:, 0:128], lhsT=xb[:, k, t * 128:(t + 1) * 128], rhs=Wbf[:, k, 1536:1664],
                                                                   start=(k == 0), stop=(k == 7)),
                                 reads=[("Wbf", k), ("xbf", G % 2, k)], writes=["ps7"])
                        P.op("dve", lambda e, t=t, tile_i=tile_i: e.tensor_scalar(out=VA[:, tile_i, 0, 0:64], in0=vp[:, 0:64], scalar1=rstd_col[G % 2][:, t:t + 1],
                                                                                 scalar2=None, op0=ALU.mult),
                             reads=["ps7", ("rstdcol", G % 2)], writes=[("VA0", tile_i)])
                        P.op("dve", lambda e, t=t, tile_i=tile_i: e.tensor_scalar(out=VA[:, tile_i, 1, 64:128], in0=vp[:, 64:128], scalar1=rstd_col[G % 2][:, t:t + 1],
                                                                                 scalar2=None, op0=ALU.mult),
                             reads=["ps7", ("rstdcol", G % 2)], writes=[("VA1", tile_i)])

                NGRP = int(os.environ.get("P1_GROUPS", NG))
                if NGRP > 0:
                    stage(0)
                for G in range(NGRP):
                    group(G)
                for nm, t in (("qA", qA), ("kA", kA), ("VA", VA), ("cqn", cqn), ("ckvn", ckvn), ("KT0", KT[0])):
                    dump(P, nm, t)
                P.flush(blk)


        if stop_after >= 2:
            A.lo = KB(R_OA + 32)
            LIM = SB_END
            kAm = [A.tmp(f"kAm{i}", [128, S], BF16, LIM) for i in range(2)]
            relrep = A.tmp("relrep", [128, 8, 128], F32, LIM)
            ohp = A.tmp("ohp", [128, 640], F32, LIM)
            mka = A.tmp("mka", [128, 640], F32, LIM)
            rep = A.tmp("rep", [128, 640], F32, LIM)
            BTf = A.tmp("BTf", [128, 384], F32, LIM)
            BTb = [A.tmp(f"BTb{i}", [128, 384], BF16, LIM) for i in range(8)]
            pTa = [A.tmp(f"pTa{i}", [128, 384], BF16, LIM) for i in range(3)]
            NB = 8
            raw = [A.tmp(f"raw{i}", [128, NB, 256], F32, LIM) for i in range(2)]
            dsh2 = A.tmp("dsh2", [128, NB, 128], F32, LIM)
            with nc.Block() as blk:
                P.op("sp", lambda e: e.dma_start(out=relrep[:], in_=relb_d), writes=["relrep"], dma=True, key="p2a")
                P.op("sp", lambda e: e.dma_start(out=ohp[:], in_=ohrel_d), writes=["ohp"], dma=True, key="p2b")
                P.op("sp", lambda e: e.dma_start(out=mka[:], in_=maskadd_d), writes=["mka"], dma=True, key="p2c")
                P.op("pool", lambda e: e.memset(kAm[0][:], 0.0), writes=["kAm0"])
                P.op("pool", lambda e: e.memset(kAm[1][:], 0.0), writes=["kAm1"])
                P.op("dve", lambda e: e.tensor_copy(out=kAm[0][0:64, :], in_=kA[0:64, :]), reads=["kAm0"], writes=["kAm0"])
                P.op("dve", lambda e: e.tensor_copy(out=kAm[1][64:128, :], in_=kA[64:128, :]), reads=["kAm1"], writes=["kAm1"])
                for h in range(8):
                    for hf in range(2):
                        P.op("pe", lambda e, h=h, hf=hf: e.matmul(psum[hf][:, 0:320], lhsT=relrep[:, h, :], rhs=ohp[:, hf * 320:(hf + 1) * 320], start=True, stop=True),
                             reads=["relrep", "ohp"], writes=[f"ps{hf}"])
                        P.op("dve", lambda e, hf=hf: e.tensor_tensor(out=rep[:, hf * 320:(hf + 1) * 320], in0=psum[hf][:, 0:320], in1=mka[:, hf * 320:(hf + 1) * 320], op=ALU.add),
                             reads=[f"ps{hf}", "mka"], writes=[("rep", hf)])
                    P.op("sp", lambda e, h=h: e.dma_start(out=trep.ap()[h], in_=rep[:]), reads=[("rep", 0), ("rep", 1)], writes=[("trep", h)], dma=True, key="trepw")
                    P.op("sp", lambda e, h=h: e.dma_start(out=BTf[:], in_=bass.AP(tensor=trep, offset=h * 128 * 640 + 127, ap=[[639, 128], [1, 384]])),
                         reads=[("trep", h)], writes=["BTf"], dma=True, key="btf")
                    P.op("dve", lambda e, h=h: e.tensor_scalar(out=BTb[h][:], in0=BTf[:], scalar1=8.0, scalar2=None, op0=ALU.mult), reads=["BTf"], writes=[("BTb", h)])
                it = [0]
                for c in range(4):
                    items = [(n, hh) for n in range(NT) for hh in range(2)]
                    rmap = {}

                    def qk(n, hh, c=c):
                        head = c + 4 * hh
                        r = it[0] % 3
                        it[0] += 1
                        rmap[(n, hh)] = r
                        spb = psum[r]
                        ms_ = [(yi, m) for yi, m in enumerate((n + 1, n, n - 1)) if 0 <= m < NT]
                        y0, y1 = ms_[0][0] * 128, ms_[-1][0] * 128 + 128
                        P.op("pe", lambda e: e.matmul(spb[:, y0:y1], lhsT=ident_bf, rhs=BTb[head][:, y0:y1], start=True, stop=False),
                             reads=[("BTb", head)], writes=[f"ps{r}"])
                        for i_, (yi, m) in enumerate(ms_):
                            P.op("pe", lambda e, yi=yi, m=m, i_=i_, L=len(ms_): e.matmul(spb[:, yi * 128:(yi + 1) * 128], lhsT=kAm[hh][:, m * 128:(m + 1) * 128],
                                                                                         rhs=qA[:, c, n * 128:(n + 1) * 128], start=False, stop=(i_ == L - 1)),
                                 reads=[f"kAm{hh}"], writes=[f"ps{r}"])
                        P.op("act", lambda e: e.activation(out=pTa[r][:, y0:y1], in_=spb[:, y0:y1], func=AF.Exp, scale=0.125),
                             reads=[f"ps{r}"], writes=[("pTa", r)])

                    def pv(n, hh, c=c):
                        r = rmap[(n, hh)]
                        ab = 3 + n % 2
                        acc = psum[ab]
                        ms_ = [(yi, m) for yi, m in enumerate((n + 1, n, n - 1)) if 0 <= m < NT]
                        for i_, (yi, m) in enumerate(ms_):
                            P.op("pe", lambda e, yi=yi, m=m, i_=i_, L=len(ms_): e.matmul(acc[:, hh * 128:(hh + 1) * 128], lhsT=VA[:, m, hh, :], rhs=pTa[r][:, yi * 128:(yi + 1) * 128],
                                                                                         start=(i_ == 0), stop=(i_ == L - 1)),
                                 reads=[("pTa", r)], writes=[f"ps{ab}"])
                        if hh == 1:
                            nb_, ni = n // NB, n % NB
                            bi = (c * (NT // NB) + nb_) % 2
                            rw, rk = raw[bi], ("raw", bi)
                            P.op("dve", lambda e: e.tensor_copy(out=rw[:, ni, :], in_=acc[:, 0:256]), reads=[f"ps{ab}"], writes=[rk])
                            if ni == NB - 1:
                                ns = slice(nb_ * NB * 128, (nb_ + 1) * NB * 128)
                                P.op("pool", lambda e: e.tensor_copy(out=dsh2[0:64, :, :], in_=rw[64:128, :, 0:128]), reads=[rk], writes=["dsh2a"])
                                P.op("dve", lambda e: e.tensor_copy(out=dsh2[64:128, :, :], in_=rw[0:64, :, 128:256]), reads=[rk], writes=["dsh2b"])
                                P.op("dve", lambda e: e.tensor_scalar(out=dsh2[0:64, :, :], in0=dsh2[0:64, :, :], scalar1=esink[0:64, c:c + 1], scalar2=None, op0=ALU.add), reads=["dsh2a"], writes=["dsh2a"])
                                P.op("dve", lambda e: e.tensor_scalar(out=dsh2[64:128, :, :], in0=dsh2[64:128, :, :], scalar1=esink[64:128, c + 4:c + 5], scalar2=None, op0=ALU.add), reads=["dsh2b"], writes=["dsh2b"])
                                P.op("act", lambda e: e.activation(out=dsh2[:], in_=dsh2[:], func=AF.Ln), reads=["dsh2a", "dsh2b"], writes=["dsh2a", "dsh2b"])
                                P.op("act", lambda e: e.activation(out=dsh2[:], in_=dsh2[:], func=AF.Exp, scale=-1.0), reads=["dsh2a", "dsh2b"], writes=["dsh2a", "dsh2b"])
                                P.op("dve", lambda e: e.tensor_tensor(out=oA[0:64, c, ns].rearrange("p (a b) -> p a b", a=NB), in0=rw[0:64, :, 0:128], in1=dsh2[0:64, :, :], op=ALU.mult),
                                     reads=[rk, "dsh2a"], writes=[("oA", c, nb_, 0)])
                                P.op("dve", lambda e: e.tensor_tensor(out=oA[64:128, c, ns].rearrange("p (a b) -> p a b", a=NB), in0=rw[64:128, :, 128:256], in1=dsh2[64:128, :, :], op=ALU.mult),
                                     reads=[rk, "dsh2b"], writes=[("oA", c, nb_, 1)])

                    qk(*items[0])
                    qk(*items[1])
                    for i in range(len(items)):
                        if i + 2 < len(items):
                            qk(*items[i + 2])
                        pv(*items[i])
                dump(P, "oA", oA)
                P.flush(blk)

        R_OB = R_OA + 32
        oB = A.at("oB", [128, 4, S], BF16, KB(R_OB))
        if stop_after >= 3:
            A.lo = KB(R_OB + 32)
            A2 = Arena(nc)
            A2.n = 5000
            A2.lo = KB(R_A)
            LIM2 = KB(R_OA)
            LIM = SB_END
            QT = [A2.tmp(f"QT{i}", [128, S], BF16, LIM2) for i in range(2)]
            VO = [A2.tmp(f"VO{i}", [128, NT, 128], BF16, LIM2) for i in range(2)]
            CSG = A2.tmp("CSG", [128, S], F32, LIM2)
            Wqb = A2.tmp("Wqb", [128, 3, 1024], BF16, LIM2)
            Wkv = A.tmp("Wkv", [128, 2, 1024], BF16, SB_END)
            pTb = [A.tmp(f"pTb{i}", [128, 512], BF16, LIM) for i in range(4)]
            sq3 = [A.tmp(f"sq3{i}", [128, 512], BF16, LIM) for i in range(3)]
            ri3 = [A.tmp(f"ri3{i}", [128, 512], F32, LIM) for i in range(3)]
            u3 = [A.tmp(f"u3{i}", [128, 512], F32, LIM) for i in range(2)]
            kt3 = A.tmp("kt3", [128, 512], BF16, LIM)
            dshb = [A.tmp(f"dshb{i}", [128, 512], F32, LIM) for i in range(2)]
            SCALE_B = 96.0 ** -0.5
            with nc.Block() as blk:
                P.op("sp", lambda e: e.dma_start(out=CSG[:], in_=csq_d), writes=["CSG"], dma=True, key="p3a")
                P.op("pool", lambda e: e.dma_start(out=Wqb[:], in_=wqb_d.rearrange("(k p) f -> p k f", p=128)), writes=["Wqb"], dma=True, key="p3b")
                P.op("pool", lambda e: e.dma_start(out=Wkv[:], in_=wkvb_d.rearrange("(k p) f -> p k f", p=128)), writes=["Wkv"], dma=True, key="p3c")
                P.op("dve", lambda e: e.tensor_scalar(out=CSG[:], in0=CSG[:], scalar1=gcols[:, GC_QB:GC_QB + 1], scalar2=None, op0=ALU.mult), reads=["CSG"], writes=["CSG"])
                P.op("pool", lambda e: e.memset(VO[0][:, :, 64:128], 1.0), writes=["VO0ones"])
                P.op("pool", lambda e: e.memset(VO[1][:, :, 0:64], 1.0), writes=["VO1ones"])
                for hp in range(4):
                    for G in range(NG):
                        gs = slice(G * 512, (G + 1) * 512)
                        kp, qp0, qp1, vp = psum[5], psum[0], psum[1], psum[7]
                        for i in range(2):
                            P.op("pe", lambda e, i=i, gs=gs, hp=hp, kp=kp: e.matmul(kp[:], lhsT=Wkv[:, i, hp * 128:(hp + 1) * 128], rhs=ckvn[:, i, gs], start=(i == 0), stop=(i == 1)),
                                 reads=["Wkv"], writes=["ps5"])
                        for hh, qp in ((0, qp0), (1, qp1)):
                            h = 2 * hp + hh
                            for i in range(3):
                                P.op("pe", lambda e, i=i, gs=gs, h=h, qp=qp: e.matmul(qp[:], lhsT=Wqb[:, i, h * 128:(h + 1) * 128], rhs=cqn[:, i, gs], start=(i == 0), stop=(i == 2)),
                                     reads=["Wqb"], writes=[f"ps{hh}"])
                        for t in range(4):
                            ti = G * 4 + t
                            for i in range(2):
                                P.op("pe", lambda e, i=i, ti=ti, t=t, hp=hp, vp=vp: e.matmul(vp[:, t * 128:(t + 1) * 128], lhsT=ckvn[:, i, ti * 128:(ti + 1) * 128], rhs=Wkv[:, i, 512 + hp * 128:512 + (hp + 1) * 128],
                                                                               start=(i == 0), stop=(i == 1)),
                                     reads=["Wkv"], writes=["ps7"])
                        srcs = ((kp, "ps5", bd64, psum[6], "ps6"), (qp0, "ps0", bdq, psum[2], "ps2"), (qp1, "ps1", bdq, psum[3], "ps3"))
                        for z_, (pp, pk, bd_, mp, mk) in enumerate(srcs):
                            P.op("act", lambda e, pp=pp, z_=z_: e.activation(out=sq3[z_][:], in_=pp[:], func=AF.Square), reads=[pk], writes=[("sq3", z_)])
                        for z_, (pp, pk, bd_, mp, mk) in enumerate(srcs):
                            P.op("pe", lambda e, bd_=bd_, mp=mp, z_=z_: e.matmul(mp[:], lhsT=bd_, rhs=sq3[z_][:], start=True, stop=True), reads=[("sq3", z_)], writes=[mk])
                        for z_, (pp, pk, bd_, mp, mk) in enumerate(srcs):
                            P.op("act", lambda e, mp=mp, z_=z_: e.activation(out=ri3[z_][:], in_=mp[:], func=AF.Ln, bias=gcols[:, GC_EPS:GC_EPS + 1]), reads=[mk], writes=[("ri3", z_)])
                            P.op("act", lambda e, z_=z_: e.activation(out=ri3[z_][:], in_=ri3[z_][:], func=AF.Exp, scale=-0.5), reads=[("ri3", z_)], writes=[("ri3", z_)])
                        vp3 = vp[:].rearrange("p (a b) -> p a b", a=4)
                        P.op("dve", lambda e, G=G, vp3=vp3: e.tensor_copy(out=VO[0][:, G * 4:(G + 1) * 4, 0:64], in_=vp3[:, :, 0:64]), reads=["ps7"], writes=[("VO0", G)])
                        P.op("dve", lambda e, G=G, vp3=vp3: e.tensor_copy(out=VO[1][:, G * 4:(G + 1) * 4, 64:128], in_=vp3[:, :, 64:128]), reads=["ps7"], writes=[("VO1", G)])
                        P.op("dve", lambda e, gs=gs, kp=kp: e.scalar_tensor_tensor(out=KT[0][0:64, gs], in0=kp[0:64, :], scalar=gcols[0:64, GC_KN:GC_KN + 1], in1=ri3[0][0:64, :], op0=ALU.mult, op1=ALU.mult),
                             reads=["ps5", ("ri3", 0)], writes=[("KT0n", G)])
                        P.op("dve", lambda e, kp=kp: e.scalar_tensor_tensor(out=kt3[64:128, :], in0=kp[64:128, :], scalar=gcols[64:128, GC_KN:GC_KN + 1], in1=ri3[0][64:128, :], op0=ALU.mult, op1=ALU.mult),
                             reads=["ps5", ("ri3", 0)], writes=["kt3"])
                        P.op("pool", lambda e, gs=gs: e.tensor_copy(out=KT[1][0:64, gs], in_=kt3[64:128, :]), reads=["kt3"], writes=[("KT1n", G)])
                        for hh, qp in ((0, qp0), (1, qp1)):
                            P.op("pool", lambda e, gs=gs, hh=hh: e.tensor_tensor(out=u3[hh][:], in0=ri3[1 + hh][:], in1=CSG[:, gs], op=ALU.mult), reads=[("ri3", 1 + hh), "CSG"], writes=[("u3", hh)])
                            P.op("dve", lambda e, gs=gs, hh=hh, qp=qp: e.tensor_tensor(out=QT[hh][:, gs], in0=qp[:], in1=u3[hh][:], op=ALU.mult), reads=[f"ps{hh}", ("u3", hh)], writes=[("QT", hh, G)])
                    for hh in range(2):
                        nr = slice(0, 64) if hh == 0 else slice(64, 128)
                        dr = slice(64, 128) if hh == 0 else slice(0, 64)
                        kdeps = [("KT0n" if hh == 0 else "KT1n", G) for G in range(NG)]
                        for qg in range(NG):
                            qs = slice(qg * 512, (qg + 1) * 512)
                            ab = 3 + qg % 2
                            acc = psum[ab]

                            def QK(kt, hh=hh, qs=qs, qg=qg):
                                b = kt % 3
                                P.op("pe", lambda e, b=b, kt=kt: e.matmul(psum[b][:], lhsT=KT[hh][:, kt * 128:(kt + 1) * 128], rhs=QT[hh][:, qs], start=True, stop=True),
                                     reads=[("KT0n" if hh == 0 else "KT1n", kt // 4), ("QT", hh, qg)], writes=[f"ps{b}"])
                                P.op("act", lambda e, b=b, kt=kt: e.activation(out=pTb[kt % 4][:], in_=psum[b][:], func=AF.Exp, scale=SCALE_B),
                                     reads=[f"ps{b}"], writes=[("pTb", kt % 4)])

                            def PV(kt, hh=hh, acc=acc, ab=ab):
                                P.op("pe", lambda e, kt=kt: e.matmul(acc[:], lhsT=VO[hh][:, kt, :], rhs=pTb[kt % 4][:], start=(kt == 0), stop=(kt == NT - 1)),
                                     reads=[("pTb", kt % 4), (f"VO{hh}", kt // 4), f"VO{hh}ones"], writes=[f"ps{ab}"])
                            QK(0)
                            QK(1)
                            for kt in range(NT):
                                if kt + 2 < NT:
                                    QK(kt + 2)
                                PV(kt)
                            d_ = dshb[qg % 2]
                            P.op("dve", lambda e, acc=acc, d_=d_, nr=nr, dr=dr: e.tensor_copy(out=d_[nr, :], in_=acc[dr, :]), reads=[f"ps{ab}"], writes=[("dshb", qg % 2)])
                            P.op("dve", lambda e, d_=d_, nr=nr: e.reciprocal(out=d_[nr, :], in_=d_[nr, :]), reads=[("dshb", qg % 2)], writes=[("dshb", qg % 2)])
                            P.op("dve", lambda e, acc=acc, d_=d_, nr=nr, qs=qs, hp=hp: e.tensor_tensor(out=oB[nr, hp, qs], in0=acc[nr, :], in1=d_[nr, :], op=ALU.mult),
                                 reads=[f"ps{ab}", ("dshb", qg % 2)], writes=[("oB", hp, qg, hh)])
                dump(P, "oB", oB)
                P.flush(blk)


        R_AF = R_C
        affT = A.at("affT", [128, S], F32, KB(R_AF))
        if stop_after >= 4:
            A.lo = KB(R_AF + 16)
            LIM = KB(R_OA)
            Wo = A.tmp("Wo", [128, 8, 1024], BF16, LIM)
            g2bc = A.tmp("g2bc", [128, 1024], F32, LIM)
            wr = A.tmp("wr", [128, 8, 128], F32, LIM)
            xt = [A.tmp(f"xt{i}", [128, 1024], F32, LIM) for i in range(2)]
            x1 = [A.tmp(f"x1{i}", [128, 1024], F32, LIM) for i in range(2)]
            h2f = [A.tmp(f"h2f{i}", [128, 1024], F32, LIM) for i in range(2)]
            h2b = [A.tmp(f"h2b{i}", [128, 1024], BF16, LIM) for i in range(2)]
            h2Tb_ = [A.tmp(f"h2T{i}", [128, 8, 128], F32, LIM) for i in range(2)]
            sqA = A.tmp("sqA", [128, 4, 128], BF16, LIM)
            sqB = A.tmp("sqB", [128, 4, 128], BF16, LIM)
            afp = [A.tmp(f"afp{i}", [128, 128], F32, LIM) for i in range(2)]
            sm = A.tmp("sm", [128, 16], F32, LIM)
            with nc.Block() as blk:
                P.op("pool", lambda e: e.dma_start(out=Wo[:], in_=wo_d.rearrange("(k p) f -> p k f", p=128)), writes=["Wo"], dma=True, key="p4a")
                P.op("sp", lambda e: e.dma_start(out=g2bc[:], in_=ln2_d.partition_broadcast(128)), writes=["g2bc"], dma=True, key="p4b")
                P.op("sp", lambda e: e.dma_start(out=wr[:], in_=wr_d.rearrange("(k p) f -> p k f", p=128)), writes=["wr"], dma=True, key="p4c")
                for c in range(8):
                    P.op("dve", lambda e, c=c: e.tensor_scalar(out=Wo[:, c, :], in0=Wo[:, c, :], scalar1=gcols[:, GC_OA + c:GC_OA + c + 1], scalar2=None, op0=ALU.mult),
                         reads=["Wo"], writes=["Wo"])
                P.op("pool", lambda e: e.memset(afp[0][:], 0.0), writes=[("afp", 0)])
                P.op("pool", lambda e: e.memset(afp[1][:], 0.0), writes=[("afp", 1)])
                def stA(t):
                    ts_ = slice(t * 128, (t + 1) * 128)
                    b = t % 2
                    P.op("sp", lambda e, b=b, ts_=ts_: e.dma_start(out=xt[b][:], in_=x[ts_, :]), writes=[("xt", b)], dma=True, key=f"xt{b}")
                    P.op("act", lambda e, ts_=ts_: e.activation(out=sqA[:], in_=oA[:, :, ts_], func=AF.Square), writes=["sqA"])
                    P.op("act", lambda e, ts_=ts_: e.activation(out=sqB[:], in_=oB[:, :, ts_], func=AF.Square), writes=["sqB"])
                    ss = psum[6]
                    for c in range(4):
                        P.op("pe", lambda e, c=c: e.matmul(ss[:, 0:128], lhsT=sqA[:, c, :], rhs=ones_bf, start=(c == 0), stop=(c == 3)), reads=["sqA"], writes=["ps6"])
                    for c in range(4):
                        P.op("pe", lambda e, c=c: e.matmul(ss[:, 128:256], lhsT=sqB[:, c, :], rhs=ones_bf, start=(c == 0), stop=(c == 3)), reads=["sqB"], writes=["ps6"])
                    yield
                    P.op("act", lambda e: e.activation(out=sm[:, 0:2], in_=ss[:, 0:256:128], func=AF.Sqrt, scale=1.0 / 512, bias=gcols[:, GC_EPS:GC_EPS + 1]), reads=["ps6"], writes=["sm01"])
                    P.op("dve", lambda e: e.reciprocal(out=sm[:, 2:4], in_=sm[:, 0:2]), reads=["sm01"], writes=["sm23"])
                    yield
                    for hf in range(2):
                        hs = slice(hf * 512, (hf + 1) * 512)
                        for c in range(4):
                            P.op("pe", lambda e, c=c, hs=hs, hf=hf, ts_=ts_: e.matmul(psum[hf][:], lhsT=oA[:, c, ts_], rhs=Wo[:, c, hs], start=(c == 0), stop=(c == 3)), reads=["Wo"], writes=[f"ps{hf}"])
                        for c in range(4):
                            P.op("pe", lambda e, c=c, hs=hs, hf=hf, ts_=ts_: e.matmul(psum[2 + hf][:], lhsT=oB[:, c, ts_], rhs=Wo[:, 4 + c, hs], start=(c == 0), stop=(c == 3)), reads=["Wo"], writes=[f"ps{2 + hf}"])
                        P.op("dve", lambda e, hs=hs, hf=hf, b=b: e.scalar_tensor_tensor(out=x1[b][:, hs], in0=psum[hf][:], scalar=sm[:, 2:3], in1=xt[b][:, hs], op0=ALU.mult, op1=ALU.add),
                             reads=[f"ps{hf}", "sm23", ("xt", b)], writes=[("x1", b, hf)])
                        P.op("dve", lambda e, hs=hs, hf=hf, b=b: e.scalar_tensor_tensor(out=x1[b][:, hs], in0=psum[2 + hf][:], scalar=sm[:, 3:4], in1=x1[b][:, hs], op0=ALU.mult, op1=ALU.add),
                             reads=[f"ps{2 + hf}", "sm23", ("x1", b, hf)], writes=[("x1", b, hf)])
                        yield
                    X1 = [("x1", b, 0), ("x1", b, 1)]
                    P.op("sp", lambda e, b=b, ts_=ts_: e.dma_start(out=out[ts_, :], in_=x1[b][:]), reads=X1, dma=True, key=f"o{b}")

                def stB1(t):
                    ts_ = slice(t * 128, (t + 1) * 128)
                    b = t % 2
                    h2T = h2Tb_[b]
                    X1 = [("x1", b, 0), ("x1", b, 1)]
                    P.op("dve", lambda e, b=b: e.tensor_tensor(out=h2f[b][:], in0=x1[b][:], in1=x1[b][:], op=ALU.mult), reads=X1, writes=[("h2f", b)])
                    P.op("dve", lambda e, b=b: e.reduce_sum(out=sm[:, 4:5], in_=h2f[b][:], axis=mybir.AxisListType.X), reads=[("h2f", b)], writes=["sm4"])
                    yield
                    P.op("act", lambda e: e.activation(out=sm[:, 5:6], in_=sm[:, 4:5], func=AF.Sqrt, scale=1.0 / D, bias=gcols[:, GC_EPS:GC_EPS + 1]), reads=["sm4"], writes=["sm5"])
                    P.op("dve", lambda e: e.reciprocal(out=sm[:, 6:7], in_=sm[:, 5:6]), reads=["sm5"], writes=["sm6"])
                    yield
                    P.op("dve", lambda e, b=b: e.scalar_tensor_tensor(out=h2f[b][:], in0=x1[b][:], scalar=sm[:, 6:7], in1=g2bc[:], op0=ALU.mult, op1=ALU.mult),
                         reads=X1 + ["sm6", "g2bc", ("h2f", b)], writes=[("h2f", b)])
                    P.op("pool", lambda e, b=b: e.tensor_copy(out=h2b[b][:], in_=h2f[b][:]), reads=[("h2f", b)], writes=[("h2b", b)])
                    P.op("sp", lambda e, b=b, ts_=ts_: e.dma_start(out=h2_dram[ts_, :], in_=h2b[b][:]), reads=[("h2b", b)], dma=True, key=f"h2o{b}")
                    yield
                    for k in range(8):
                        pb = 4 + k // 4
                        P.op("pe", lambda e, k=k, pb=pb, b=b: e.transpose(psum[pb][:, (k % 4) * 128:(k % 4 + 1) * 128], h2f[b][:, k * 128:(k + 1) * 128], identf[:]),
                             reads=[("h2f", b), "identf"], writes=[f"ps{pb}"])
                    yield
                    P.op("dve", lambda e: e.tensor_copy(out=h2T[:, 0:4, :], in_=psum[4][:].rearrange("p (a b) -> p a b", a=4)), reads=["ps4"], writes=[("h2Ta", b)])
                    P.op("dve", lambda e: e.tensor_copy(out=h2T[:, 4:8, :], in_=psum[5][:].rearrange("p (a b) -> p a b", a=4)), reads=["ps5"], writes=[("h2Tb", b)])

                def stB2(t):
                    ts_ = slice(t * 128, (t + 1) * 128)
                    b = t % 2
                    h2T = h2Tb_[b]
                    lg = psum[7]
                    for k in range(8):
                        P.op("pe", lambda e, k=k: e.matmul(lg[:, 0:128], lhsT=h2T[:, k, :], rhs=wr[:, k, :], start=(k == 0), stop=(k == 7)),
                             reads=[("h2Ta", b), ("h2Tb", b), "wr"], writes=["ps7"])
                    af = afp[b]
                    yield
                    P.op("dve", lambda e: e.reduce_max(out=sm[:, 7:8], in_=lg[:, 0:16], axis=mybir.AxisListType.X), reads=["ps7"], writes=["sm7"])
                    P.op("dve", lambda e: e.tensor_scalar(out=sm[:, 8:9], in0=sm[:, 7:8], scalar1=-1.0, scalar2=None, op0=ALU.mult), reads=["sm7"], writes=["sm8"])
                    yield
                    P.op("act", lambda e, af=af: e.activation(out=af[:, 0:16], in_=lg[:, 0:16], func=AF.Exp, bias=sm[:, 8:9]), reads=["ps7", "sm8"], writes=[("afp", b)])
                    yield
                    P.op("dve", lambda e, af=af: e.reduce_sum(out=sm[:, 9:10], in_=af[:, 0:16], axis=mybir.AxisListType.X), reads=[("afp", b)], writes=["sm9"])
                    P.op("dve", lambda e: e.reciprocal(out=sm[:, 10:11], in_=sm[:, 9:10]), reads=["sm9"], writes=["sm10"])
                    P.op("dve", lambda e, af=af: e.tensor_scalar(out=af[:, 0:16], in0=af[:, 0:16], scalar1=sm[:, 10:11], scalar2=None, op0=ALU.mult), reads=["sm10", ("afp", b)], writes=[("afp", b)])
                    P.op("sp", lambda e, af=af, ts_=ts_: e.dma_start(out=aff_dram[ts_, :], in_=af[:, 0:16]), reads=[("afp", b)], dma=True, key=f"afo{b}")
                    yield
                    P.op("pe", lambda e, af=af: e.transpose(lg[:, 128:256], af[:], identf[:]), reads=[("afp", b)], writes=["ps7"])
                    yield
                    P.op("dve", lambda e, ts_=ts_: e.tensor_copy(out=affT[:, ts_], in_=lg[:, 128:256]), reads=["ps7"], writes=[("affT", t)])


                def run_rr(gens):
                    gens = list(gens)
                    while gens:
                        for g_ in list(gens):
                            try:
                                next(g_)
                            except StopIteration:
                                gens.remove(g_)

                for t in range(NT + 2):
                    gl = []
                    if t < NT:
                        gl.append(stA(t))
                    if 1 <= t <= NT:
                        gl.append(stB1(t - 1))
                    if 2 <= t:
                        gl.append(stB2(t - 2))
                    run_rr(gl)
                dump(P, "affT", affT)
                P.flush(blk)

        NWB = 7
        WM = [A.at(f"WM{i}", [128, 8, 1024], BF16, KB(20 + 16 * i)) for i in range(NWB)]
        wsrc = (wg_d, wu_d, wd_d)

        def wload_mat(i):
            if i >= 3 * NE:
                return
            ex_, kind, sl = i // 3, i % 3, i % NWB
            P.op("pool", lambda e: e.dma_start(out=WM[sl][:], in_=wsrc[kind][ex_].rearrange("(k p) f -> p k f", p=128)), writes=[("WM", sl)], dma=True, key=f"wm{sl}")

        if stop_after >= 5:
            A.lo = KB(132)
            LIM = SB_END
            junk = A.tmp("junk", [128, S], BF16, LIM)
            mask = A.tmp("mask", [128, S], F32, LIM)
            cum = A.tmp("cum", [128, S], F32, LIM)
            cumh = A.tmp("cumh", [128, S], F16, LIM)
            bs = A.tmp("bs", [128, 8], F32, LIM)
            with nc.Block() as blk:
                if stop_after >= 6:
                    for i in range(NWB):
                        wload_mat(i)
                P.op("dve", lambda e: e.memset(bs[:, 0:1], 0.0), writes=["bs"])
                P.op("dve", lambda e: e.memset(bs[:, 1:2], 1.0), reads=["bs"], writes=["bs"])
                P.op("dve", lambda e: e.memset(bs[:, 2:3], 0.5), reads=["bs"], writes=["bs"])
                for it_ in range(30):
                    hn = 2.0 ** -(it_ + 2)
                    P.op("dve", lambda e: e.tensor_tensor(out=bs[:, 5:6], in0=bs[:, 2:3], in1=bs[:, 0:1], op=ALU.subtract), reads=["bs"], writes=["bs"])
                    P.op("dve", lambda e: e.tensor_scalar(out=junk[:], in0=affT[:], scalar1=bs[:, 2:3], scalar2=None, op0=ALU.is_ge, op1=ALU.add, accum_out=bs[:, 3:4]),
                         reads=["bs"], writes=["bs", "junk"])
                    P.op("dve", lambda e: e.tensor_single_scalar(out=bs[:, 4:5], in_=bs[:, 3:4], scalar=CAP - 0.5, op=ALU.is_ge), reads=["bs"], writes=["bs"])
                    P.op("dve", lambda e: e.scalar_tensor_tensor(out=bs[:, 0:1], in0=bs[:, 5:6], scalar=bs[:, 4:5], in1=bs[:, 0:1], op0=ALU.mult, op1=ALU.add), reads=["bs"], writes=["bs"])
                    P.op("dve", lambda e, hn=hn: e.tensor_scalar(out=bs[:, 2:3], in0=bs[:, 0:1], scalar1=hn, scalar2=None, op0=ALU.add), reads=["bs"], writes=["bs"])
                P.op("dve", lambda e: e.tensor_scalar(out=mask[:], in0=affT[:], scalar1=bs[:, 0:1], scalar2=None, op0=ALU.is_ge), reads=["bs"], writes=["mask"])
                P.op("dve", lambda e: e.tensor_tensor_scan(out=cum[:], data0=mask[:], data1=mask[:], initial=0.0, op0=ALU.add, op1=ALU.max), reads=["mask"], writes=["cum"])
                P.op("dve", lambda e: e.tensor_scalar(out=cumh[:], in0=cum[:], scalar1=1000.0, scalar2=None, op0=ALU.min), reads=["cum"], writes=["cumh"])
                P.op("sp", lambda e: e.dma_start(out=cum_dram, in_=cumh[0:16, :]), reads=["cumh"], dma=True, key="p5a")
                dump(P, "cum", cum)
                P.flush(blk)

        if stop_after >= 6:
            A.lo = KB(132)
            LIM = SB_END
            cbc = A.tmp("cbc", [128, S], F16, LIM)
            junk6 = A.tmp("junk6", [128, S], BF16, LIM)
            xg = [[A.tmp(f"xg{b}{i}", [128, 1024], BF16, LIM) for i in range(4)] for b in range(2)]
            xeT = [A.tmp(f"xeT{b}", [128, 8, 512], BF16, LIM) for b in range(2)]
            actT = A.tmp("actT", [128, 8, 512], BF16, LIM)
            sg = [A.tmp(f"sg{i}", [128, 512], F32, LIM) for i in range(2)]
            ye = [A.tmp(f"ye{i}", [128, 1024], F32, LIM) for i in range(2)]
            gt = [[A.tmp(f"gt{b}{i}", [128, 16], F32, LIM) for i in range(4)] for b in range(2)]
            idxf = A.tmp("idxf", [128, 4], F32, LIM)
            idxi = [A.tmp(f"idxi{i}", [128, 4], I32, LIM) for i in range(2)]
            with nc.Block() as blk:
                prev_sc = [[]]

                def s1_cbc(ex):
                    P.op("sp", lambda e: e.dma_start(out=cbc[:], in_=cum_dram[ex:ex + 1, :].partition_broadcast(128)), writes=["cbc"], dma=True, key="cbc")

                def s1_cmp(ex, j):
                    P.op("dve", lambda e: e.tensor_scalar(out=junk6[:], in0=cbc[:], scalar1=ccol[:, j:j + 1], scalar2=None, op0=ALU.is_le, op1=ALU.add, accum_out=idxf[:, j:j + 1]),
                         reads=["cbc"], writes=["junk6", ("idxf", j)])

                def s1_gather(ex):
                    b = ex % 2
                    ii = idxi[b]
                    IDF = [("idxf", j) for j in range(4)]
                    P.op("dve", lambda e: e.tensor_scalar(out=idxf[:], in0=idxf[:], scalar1=float(S - 1), scalar2=None, op0=ALU.min), reads=IDF, writes=IDF)
                    P.op("dve", lambda e: e.tensor_copy(out=ii[:], in_=idxf[:]), reads=IDF, writes=[("idxi", b)])
                    for j in range(4):
                        P.op("pool", lambda e, j=j: e.indirect_dma_start(out=xg[b][j][:], out_offset=None, in_=h2_dram, in_offset=bass.IndirectOffsetOnAxis(ap=ii[:, j:j + 1], axis=0)),
                             reads=[("idxi", b)], writes=[("xg", b, j)], dma=True, key=f"xg{b}{j}")
                        P.op("pool", lambda e, j=j: e.indirect_dma_start(out=gt[b][j][:], out_offset=None, in_=aff_dram, in_offset=bass.IndirectOffsetOnAxis(ap=ii[:, j:j + 1], axis=0)),
                             reads=[("idxi", b)], writes=[("gt", b, j)], dma=True, key=f"gt{b}{j}")

                def s1_tr(ex):
                    b = ex % 2
                    for j in range(4):
                        tpb = psum[j % 2][:].bitcast(BF16)
                        for k in range(8):
                            P.op("pe", lambda e, j=j, k=k, tpb=tpb: e.transpose(tpb[:, k * 128:(k + 1) * 128], xg[b][j][:, k * 128:(k + 1) * 128], ident_bf),
                                 reads=[("xg", b, j)], writes=[f"ps{j % 2}"])
                        P.op("dve", lambda e, j=j, tpb=tpb: e.tensor_copy(out=xeT[b][:, :, j * 128:(j + 1) * 128], in_=tpb.rearrange("p (a b) -> p a b", a=8)),
                             reads=[f"ps{j % 2}"], writes=[("xeT", b, j)])

                def s2_gu(ex, fc):
                    b = ex % 2
                    sg_, su_ = (3 * ex) % NWB, (3 * ex + 1) % NWB
                    XE = [("xeT", b, j) for j in range(4)]
                    gb, ub = 2 + fc % 2, 4 + fc % 2
                    for k in range(8):
                        P.op("pe", lambda e, k=k: e.matmul(psum[gb][:], lhsT=WM[sg_][:, k, fc * 128:(fc + 1) * 128], rhs=xeT[b][:, k, :], start=(k == 0), stop=(k == 7)),
                             reads=XE + [("WM", sg_)], writes=[f"ps{gb}"])
                    for k in range(8):
                        P.op("pe", lambda e, k=k: e.matmul(psum[ub][:], lhsT=WM[su_][:, k, fc * 128:(fc + 1) * 128], rhs=xeT[b][:, k, :], start=(k == 0), stop=(k == 7)),
                             reads=XE + [("WM", su_)], writes=[f"ps{ub}"])
                    P.op("act", lambda e: e.activation(out=sg[fc % 2][:], in_=psum[gb][:], func=AF.Silu), reads=[f"ps{gb}"], writes=[("sg", fc % 2)])
                    P.op("dve", lambda e: e.tensor_tensor(out=actT[:, fc, :], in0=psum[ub][:], in1=sg[fc % 2][:], op=ALU.mult),
                         reads=[f"ps{ub}", ("sg", fc % 2)], writes=[("actT", fc)])

                def s2_down(ex):
                    b = ex % 2
                    sd_ = (3 * ex + 2) % NWB
                    ii = idxi[b]
                    AC = [("actT", fc) for fc in range(8)]
                    for j in range(4):
                        yb_ = ye[j % 2]
                        for hf in range(2):
                            pb = 6 + hf
                            for fc in range(8):
                                P.op("pe", lambda e, fc=fc, j=j, hf=hf, pb=pb: e.matmul(psum[pb][:], lhsT=actT[:, fc, j * 128:(j + 1) * 128], rhs=WM[sd_][:, fc, hf * 512:(hf + 1) * 512],
                                                                                      start=(fc == 0), stop=(fc == 7)),
                                     reads=AC + [("WM", sd_)], writes=[f"ps{pb}"])
                            P.op("dve", lambda e, j=j, hf=hf, pb=pb, yb_=yb_: e.tensor_scalar(out=yb_[:, hf * 512:(hf + 1) * 512], in0=psum[pb][:], scalar1=gt[b][j][:, ex:ex + 1], scalar2=None, op0=ALU.mult),
                                 reads=[f"ps{pb}", ("gt", b, j)], writes=[("ye", j % 2, hf)])
                        o_ = P.op("pool", lambda e, j=j, yb_=yb_: e.indirect_dma_start(out=out, out_offset=bass.IndirectOffsetOnAxis(ap=ii[:, j:j + 1], axis=0), in_=yb_[:], in_offset=None, compute_op=ALU.add),
                                  reads=[("ye", j % 2, 0), ("ye", j % 2, 1), ("idxi", b)], deps=list(prev_sc[0]), dma=True, key=f"sc{j % 2}")
                        prev_sc[0] = [o_]

                s1_cbc(0)
                for j in range(4):
                    s1_cmp(0, j)
                s1_gather(0)
                s1_tr(0)
                for ex in range(NE):
                    nx = ex + 1
                    if nx < NE:
                        s1_cbc(nx)
                    for fc in range(8):
                        s2_gu(ex, fc)
                        if nx < NE and fc < 4:
                            s1_cmp(nx, fc)
                        if nx < NE and fc == 3:
                            s1_gather(nx)
                    wload_mat(3 * ex + NWB)
                    wload_mat(3 * ex + 1 + NWB)
                    if nx < NE:
                        s1_tr(nx)
                    s2_down(ex)
                    wload_mat(3 * ex + 2 + NWB)
                P.flush(blk)

    return nc, dbg


def t5_bucket_np(rel):
    half, max_exact = 16, 8
    base = np.where(rel > 0, half, 0)
    n = np.abs(rel)
    nf = np.maximum(n, 1).astype(np.float32)
    large = max_exact + (np.log(nf / max_exact) / math.log(128 / max_exact) * (half - max_exact)).astype(np.int32)
    large = np.minimum(large, half - 1)
    return base + np.where(n < max_exact, n, large)


def host_constants():
    c = {}
    cm = np.zeros((128, 6, 128), np.float32)
    cm[:, 0, :] = np.eye(128)
    cm[:, 1, :] = 1.0
    cm[0:64, 2, 0:64] = 1 / 64
    cm[64:128, 2, 64:128] = 1 / 64
    cm[0:64, 3, 0:64] = 1 / 64
    cm[64:96, 3, 64:96] = 1 / 32
    cm[96:128, 3, 96:128] = 1 / 32
    cm[0:32, 4, 0:32] = 1 / 32
    cm[32:64, 4, 32:64] = 1 / 32
    for j in range(64):
        for i in range(64, 128):
            if (j % 32) == ((i - 64) % 32):
                cm[j, 5, i] = 1.0
    c["cmat"] = cm
    pos = np.arange(S, dtype=np.float32)
    freqs = (np.float32(10000.0) ** (-np.arange(0, 32, 2, dtype=np.float32) / np.float32(32))).astype(np.float32)
    ang = pos[:, None] * freqs[None, :]
    cos, sin = np.cos(ang).astype(np.float32).T, np.sin(ang).astype(np.float32).T
    cs_main = np.concatenate([cos, cos], 0)
    cs_sw = np.concatenate([-sin, sin], 0)
    c["csk"] = np.ascontiguousarray(np.concatenate([cs_main, cs_sw, np.zeros((64, S), np.float32)], 0))
    c["csq"] = np.ascontiguousarray(np.concatenate([np.ones((64, S), np.float32), cs_main, cs_sw], 0))
    i = np.arange(640)
    rel = 255 - i
    valid = np.abs(rel) <= 128
    bk = t5_bucket_np(rel)
    oh = np.zeros((128, 640), np.float32)
    oh[bk[valid], i[valid]] = 1.0
    c["ohrel"] = oh
    c["maskadd"] = np.tile(np.where(valid, 0.0, -30000.0).astype(np.float32)[None, :], (128, 1))
    c["ccol"] = (np.arange(4)[None, :] * 128 + np.arange(128)[:, None]).astype(np.float32)
    return c


def host_layout(inp):
    f = lambda a: np.ascontiguousarray(np.asarray(a, dtype=np.float32))
    w_in = f(inp["w_in"])[0]
    sw = np.concatenate([np.arange(16, 32), np.arange(0, 16)])
    qperm = np.concatenate([np.concatenate([np.arange(c * 64, c * 64 + 64), np.arange((c + 4) * 64, (c + 4) * 64 + 64)]) for c in range(4)])
    cols = [w_in[:, 0:512][:, qperm], w_in[:, 512:640], w_in[:, 768:1152], w_in[:, 1152:1408],
            w_in[:, 1408:1440], w_in[:, 1408:1440][:, sw], np.zeros((D, 64), np.float32), np.zeros((D, 128), np.float32),
            w_in[:, 640:768]]
    w_aug = np.concatenate(cols, 1)
    assert w_aug.shape[1] == 1664, w_aug.shape
    g = np.zeros((128, NGC), np.float32)
    g[:, GC_Q2] = np.tile(f(inp["a_q_norm_g"])[0], 2)
    g[:, GC_K2] = np.tile(f(inp["a_k_norm_g"])[0], 2)
    g[:, GC_CQ:GC_CQ + 3] = f(inp["cq_norm_g"])[0].reshape(3, 128).T
    g[:, GC_CKV:GC_CKV + 2] = f(inp["ckv_norm_g"])[0].reshape(2, 128).T
    kr = f(inp["b_kr_g"])[0]
    g[0:32, GC_KR] = kr
    g[32:64, GC_KR] = kr[sw]
    qr = f(inp["b_qr_g"])[0]
    g[:, GC_QB] = np.concatenate([f(inp["b_qn_g"])[0], qr, qr[sw]])
    g[:, GC_KN] = np.tile(f(inp["b_kn_g"])[0], 2)
    g[:, GC_LN1:GC_LN1 + 8] = f(inp["ln1_g"])[0].reshape(8, 128).T
    g[:, GC_OA:GC_OA + 4] = f(inp["out_a_g"])[0][qperm].reshape(4, 128).T
    g[:, GC_OB:GC_OB + 4] = f(inp["out_b_g"])[0].reshape(4, 128).T
    g[:, GC_EPS] = EPS
    w_qb = f(inp["w_qb"])[0]
    wq_cols = []
    for h in range(8):
        b = h * 96
        wq_cols += [w_qb[:, b:b + 64], w_qb[:, b + 64:b + 96], w_qb[:, b + 64:b + 96][:, sw]]
    w_kvb = f(inp["w_kvb"])[0]
    kn = np.concatenate([w_kvb[:, h * 128:h * 128 + 64] for h in range(8)], 1)
    vv = np.concatenate([w_kvb[:, h * 128 + 64:h * 128 + 128] for h in range(8)], 1)
    w_o = f(inp["w_o"])[0]
    wo_p = np.concatenate([w_o[0:512][qperm], w_o[512:1024]], 0)
    shared = {
        "w_aug": np.ascontiguousarray(w_aug), "gcols": g,
        "relb_rep": np.ascontiguousarray(np.concatenate([np.repeat(f(inp["rel_bias"])[:, :, None], 128, axis=2), np.zeros((96, 8, 128), np.float32)], 0)), "a_sink": f(inp["a_sink"]),
        "wqb_aug": np.ascontiguousarray(np.concatenate(wq_cols, 1)),
        "wkvb_aug": np.ascontiguousarray(np.concatenate([kn, vv], 1)),
        "wo_p": np.ascontiguousarray(wo_p), "ln2_g": f(inp["ln2_g"]),
        "w_router": np.ascontiguousarray(np.concatenate([f(inp["w_router"])[0], np.zeros((D, 112), np.float32)], 1)),
        "w_gate": f(inp["w_gate"])[0], "w_up": f(inp["w_up"])[0], "w_down": f(inp["w_down"])[0],
    }
    shared.update(host_constants())
    return shared


_CACHE = {}


def kernel(**inputs):
    x = np.asarray(inputs["x"], dtype=np.float32)
    shared = host_layout(inputs)
    if "nc" not in _CACHE:
        _CACHE["nc"] = build_program()[0]
    nc = _CACHE["nc"]
    in_maps = []
    for b in range(8):
        m = dict(shared)
        m["x"] = np.ascontiguousarray(x[b])
        m["xT"] = np.ascontiguousarray(x[b].T)
        in_maps.append(m)
    res = run_bass_kernel_spmd(nc, in_maps, core_ids=list(range(8)))
    return np.stack([np.asarray(r["out"], dtype=np.float32) for r in res.results], 0)
```
